# Optimizing a Trainium2 kernel written in Bass

```python
import numpy as np
import jax
import jax.numpy as jnp
from jax import lax

D_MODEL = 1024
BATCH = 16
SEQ = 2048
DEPTH = 2

HEAD_DIM = 64
N_HEADS = D_MODEL // HEAD_DIM
SWA_Q_HEADS = 3 * N_HEADS // 8
NSA_Q_HEADS = SWA_Q_HEADS
FOX_HEADS = N_HEADS - SWA_Q_HEADS - NSA_Q_HEADS
KV_HEADS = 2
GQA_GROUP = SWA_Q_HEADS // KV_HEADS
Q_BLOCK = 128
SWA_WINDOW = 128
CMP_BLOCK = 32
CMP_STRIDE = 16
SEL_BLOCK = 64
SEL_TOP_N = 8
NSA_WINDOW = 512
SEL_Q_CHUNK = 32
SEL_FORCE = 1.0e4
FORGET_BIAS_INIT = 3.0
N_EXPERTS = 32
TOP_K = 4
D_EXPERT = D_MODEL
SWIGLU_LIMIT = 7.0
SWIGLU_ALPHA = 1.702
MOE_BLOCK = 128
LN_EPS = 1e-5

FOX_W = FOX_HEADS * HEAD_DIM
SWA_QW = SWA_Q_HEADS * HEAD_DIM
NSA_QW = NSA_Q_HEADS * HEAD_DIM
KV_W = KV_HEADS * HEAD_DIM
COL_SIZES = (FOX_W, FOX_W, FOX_W, FOX_HEADS,
             SWA_QW, KV_W, KV_W,
             NSA_QW, KV_W, KV_W, KV_W, KV_W, KV_W, KV_W, 3 * NSA_Q_HEADS)
N_IN_COLS = sum(COL_SIZES)
FOX_F_OFFSET = 3 * FOX_W

kernel_name = "hybrid_fox_swa_nsa_moe_deepnorm"


def _layer_norm(x, g, b):
    xf = x.astype(jnp.float32)
    mu = jnp.mean(xf, axis=-1, keepdims=True)
    var = jnp.mean(jnp.square(xf - mu), axis=-1, keepdims=True)
    return ((xf - mu) * lax.rsqrt(var + LN_EPS) * g + b).astype(x.dtype)


def _masked_softmax(s, mask):
    m = jnp.max(jnp.where(mask, s, -jnp.inf), axis=-1, keepdims=True)
    m = jnp.where(jnp.isfinite(m), m, 0.0)
    e = jnp.where(mask, jnp.exp(s - m), 0.0)
    d = jnp.sum(e, axis=-1, keepdims=True)
    return e / jnp.where(d > 0, d, 1.0)


def _alibi_slopes():
    n = SWA_Q_HEADS + NSA_Q_HEADS
    s = 2.0 ** (-8.0 * np.arange(1, n + 1) / n)
    swa = jnp.asarray(s[0::2].reshape(KV_HEADS, GQA_GROUP), jnp.float32)
    nsa = jnp.asarray(s[1::2].reshape(KV_HEADS, GQA_GROUP), jnp.float32)
    return swa, nsa


def _heads(t, *heads):
    return t.reshape(t.shape[0], t.shape[1], *heads, HEAD_DIM)


def _forgetting_attention(q, k, v, f_logit):
    S = q.shape[1]
    scale = HEAD_DIM ** -0.5
    c = jnp.cumsum(jax.nn.log_sigmoid(f_logit.astype(jnp.float32)), axis=1)
    c = jnp.transpose(c, (0, 2, 1))
    outs = []
    for i in range(S // Q_BLOCK):
        q0, q1 = i * Q_BLOCK, (i + 1) * Q_BLOCK
        s = jnp.einsum('bqhd,bkhd->bhqk', q[:, q0:q1], k[:, :q1],
                       preferred_element_type=jnp.float32) * scale
        s = s + c[:, :, q0:q1, None] - c[:, :, None, :q1]
        causal = jnp.arange(q1)[None, :] <= jnp.arange(q0, q1)[:, None]
        p = jax.nn.softmax(jnp.where(causal, s, -jnp.inf), axis=-1)
        outs.append(jnp.einsum('bhqk,bkhd->bqhd', p.astype(v.dtype), v[:, :q1]))
    return jnp.concatenate(outs, axis=1)


def _banded_gqa(q, k, v, window, slopes, sinks=None):
    B_, S, KV, G, Dh = q.shape
    scale = Dh ** -0.5
    span = window + Q_BLOCK
    pad = ((0, 0), (window, 0), (0, 0), (0, 0))
    kp = jnp.pad(k, pad)
    vp = jnp.pad(v, pad)
    qi = jnp.arange(Q_BLOCK)[:, None]
    kj = jnp.arange(span)[None, :]
    dist = qi + window - kj
    in_band = (dist >= 0) & (dist < window)
    bias = -slopes[:, :, None, None] * dist.astype(jnp.float32)

    def block(i):
        qb = lax.dynamic_slice_in_dim(q, i * Q_BLOCK, Q_BLOCK, axis=1)
        kb = lax.dynamic_slice_in_dim(kp, i * Q_BLOCK, span, axis=1)
        vb = lax.dynamic_slice_in_dim(vp, i * Q_BLOCK, span, axis=1)
        valid = in_band & (kj + i * Q_BLOCK - window >= 0)
        s = jnp.einsum('bqhgd,bkhd->bhgqk', qb, kb,
                       preferred_element_type=jnp.float32) * scale + bias
        if sinks is None:
            p = _masked_softmax(s, valid)
        else:
            s = jnp.where(valid, s, -jnp.inf)
            sink = sinks.astype(jnp.float32)[None, :, :, None, None]
            m = jnp.maximum(jnp.max(s, axis=-1, keepdims=True), sink)
            e = jnp.exp(s - m)
            p = e / (jnp.sum(e, axis=-1, keepdims=True) + jnp.exp(sink - m))
        return jnp.einsum('bhgqk,bkhd->bqhgd', p.astype(v.dtype), vb)

    out = lax.map(block, jnp.arange(S // Q_BLOCK))
    return jnp.moveaxis(out, 0, 1).reshape(B_, S, KV, G, Dh)


def _compress(kv, pe, w1, w2):
    B_, S, KV, Dh = kv.shape
    nc = (S - CMP_BLOCK) // CMP_STRIDE + 1
    idx = np.arange(nc)[:, None] * CMP_STRIDE + np.arange(CMP_BLOCK)[None, :]
    blocks = kv[:, idx] + pe[None, None, :, None, :]
    blocks = jnp.moveaxis(blocks, 3, 2).reshape(B_, nc, KV, CMP_BLOCK * Dh)
    return jax.nn.gelu(blocks @ w1) @ w2


def _nsa(q, k_cmp, v_cmp, k_sel, v_sel, k_win, v_win, gate_logit, slopes, pe, w1, w2):
    B_, S, KV, G, Dh = q.shape
    scale = Dh ** -0.5
    t = jnp.arange(S)

    kc = _compress(k_cmp, pe[0], w1[0], w2[0])
    vc = _compress(v_cmp, pe[1], w1[1], w2[1])
    nc = kc.shape[1]
    cmp_start = np.arange(nc) * CMP_STRIDE
    cmp_end = cmp_start + CMP_BLOCK - 1
    cdist = (t[:, None] - cmp_end[None, :]).astype(jnp.float32)
    s = jnp.einsum('bthgd,bchd->bthgc', q, kc, preferred_element_type=jnp.float32) * scale
    s = s - slopes[None, None, :, :, None] * cdist[None, :, None, None, :]
    cmask = (cmp_end[None, :] <= t[:, None])[None, :, None, None, :]
    p_cmp = _masked_softmax(s, cmask)
    o_cmp = jnp.einsum('bthgc,bchd->bthgd', p_cmp.astype(vc.dtype), vc)

    ns = S // SEL_BLOCK
    sel_start = np.arange(ns) * SEL_BLOCK
    ov = np.clip(np.minimum(cmp_start[:, None] + CMP_BLOCK, sel_start[None, :] + SEL_BLOCK)
                 - np.maximum(cmp_start[:, None], sel_start[None, :]), 0, None) / CMP_BLOCK
    p_slc = jnp.einsum('bthgc,cn->bthn', p_cmp, jnp.asarray(ov, jnp.float32))
    cur = t // SEL_BLOCK
    j = jnp.arange(ns)
    forced = (j[None, :] == 0) | (j[None, :] == cur[:, None]) | (j[None, :] == cur[:, None] - 1)
    future = j[None, :] > cur[:, None]
    score = jnp.where(future[:, None, :], -1.0,
                      jnp.where(forced[:, None, :], SEL_FORCE, p_slc))
    n_sel = min(SEL_TOP_N, ns)
    _, sel_idx = lax.top_k(score, n_sel)

    ksb = jnp.transpose(k_sel.reshape(B_, ns, SEL_BLOCK, KV, Dh), (0, 3, 1, 2, 4))
    vsb = jnp.transpose(v_sel.reshape(B_, ns, SEL_BLOCK, KV, Dh), (0, 3, 1, 2, 4))
    nch = S // SEL_Q_CHUNK
    qc = jnp.transpose(q.reshape(B_, nch, SEL_Q_CHUNK, KV, G, Dh), (1, 0, 3, 2, 4, 5))
    ic = jnp.transpose(sel_idx.reshape(B_, nch, SEL_Q_CHUNK, KV, n_sel), (1, 0, 3, 2, 4))
    bi = jnp.arange(B_)[:, None, None, None]
    hi = jnp.arange(KV)[None, :, None, None]

    def chunk(args):
        qx, ix, c0 = args
        kg = ksb[bi, hi, ix]
        vg = vsb[bi, hi, ix]
        s = jnp.einsum('bhqgd,bhqnkd->bhqgnk', qx, kg,
                       preferred_element_type=jnp.float32) * scale
        tpos = c0 + jnp.arange(SEL_Q_CHUNK)
        spos = ix[..., None] * SEL_BLOCK + jnp.arange(SEL_BLOCK)
        dist = tpos[None, None, :, None, None] - spos
        s = s - slopes[None, :, None, :, None, None] * dist[:, :, :, None].astype(jnp.float32)
        s = s.reshape(B_, KV, SEL_Q_CHUNK, G, n_sel * SEL_BLOCK)
        valid = (dist >= 0).reshape(B_, KV, SEL_Q_CHUNK, 1, n_sel * SEL_BLOCK)
        p = _masked_softmax(s, valid)
        return jnp.einsum('bhqgm,bhqmd->bhqgd', p.astype(vg.dtype),
                          vg.reshape(B_, KV, SEL_Q_CHUNK, n_sel * SEL_BLOCK, Dh))

    o_sel = lax.map(chunk, (qc, ic, jnp.arange(nch) * SEL_Q_CHUNK))
    o_sel = jnp.transpose(o_sel, (1, 0, 3, 2, 4, 5)).reshape(B_, S, KV, G, Dh)

    o_win = _banded_gqa(q, k_win, v_win, NSA_WINDOW, slopes)

    g = jax.nn.sigmoid(gate_logit.reshape(B_, S, KV, G, 3))
    return g[..., 0:1] * o_cmp + g[..., 1:2] * o_sel + g[..., 2:3] * o_win


def _moe(x, router_w, router_b, w_gu, b_gu, w_dn, b_dn):
    B_, S, D = x.shape
    N = B_ * S
    NK = N * TOP_K
    xf = x.reshape(N, D)
    logits = (xf @ router_w + router_b).astype(jnp.float32)
    top_v, top_e = lax.top_k(logits, TOP_K)
    gate_w = jax.nn.softmax(top_v, axis=-1).astype(x.dtype)
    e_flat = top_e.reshape(NK)
    w_flat = gate_w.reshape(NK)
    tok = jnp.arange(NK, dtype=jnp.int32) // TOP_K
    counts = jnp.bincount(e_flat, length=N_EXPERTS)
    padded = (counts + MOE_BLOCK - 1) // MOE_BLOCK * MOE_BLOCK
    pad_end = jnp.cumsum(padded)
    pad_start = pad_end - padded
    grp_start = jnp.cumsum(counts) - counts
    order = jnp.argsort(e_flat)
    se = e_flat[order]
    dest = pad_start[se] + jnp.arange(NK) - grp_start[se]
    nblk = -(-NK // MOE_BLOCK) + N_EXPERTS
    P = nblk * MOE_BLOCK
    buf_tok = jnp.full((P,), N, jnp.int32).at[dest].set(tok[order])
    buf_w = jnp.zeros((P,), x.dtype).at[dest].set(w_flat[order])
    blk_e = jnp.minimum(jnp.searchsorted(pad_end, jnp.arange(nblk) * MOE_BLOCK, side='right'),
                        N_EXPERTS - 1)
    x_pad = jnp.concatenate([xf, jnp.zeros((1, D), x.dtype)], axis=0)

    def expert_block(args):
        e, tk = args
        h = x_pad[tk] @ w_gu[e] + b_gu[e]
        gate, up = h[:, 0::2], h[:, 1::2]
        gate = jnp.minimum(gate, SWIGLU_LIMIT)
        up = jnp.clip(up, -SWIGLU_LIMIT, SWIGLU_LIMIT)
        glu = gate * jax.nn.sigmoid(gate * SWIGLU_ALPHA)
        return ((up + 1.0) * glu) @ w_dn[e] + b_dn[e]

    y = lax.map(expert_block, (blk_e, buf_tok.reshape(nblk, MOE_BLOCK)))
    y = y.reshape(P, D) * buf_w[:, None]
    out = jnp.zeros((N + 1, D), x.dtype).at[buf_tok].add(y)[:N]
    return out.reshape(B_, S, D)


def setup_inputs(seed: int = 0) -> dict:
    key = jax.random.key(seed)
    ks = jax.random.split(key, 18)
    beta = (8.0 * DEPTH) ** -0.25
    L, D = DEPTH, D_MODEL

    def nrm(k, shape, scale):
        return scale * jax.random.normal(k, shape, jnp.float32)

    x = nrm(ks[0], (BATCH, SEQ, D), 1.0)
    w_in = nrm(ks[1], (L, D, N_IN_COLS), D ** -0.5)
    b_in = nrm(ks[2], (L, N_IN_COLS), 0.01).at[:, FOX_F_OFFSET:FOX_F_OFFSET + FOX_HEADS].add(FORGET_BIAS_INIT)
    sinks = nrm(ks[3], (L, SWA_Q_HEADS), 0.5)
    cmp_pe = nrm(ks[4], (L, 2, CMP_BLOCK, HEAD_DIM), 0.02)
    cmp_w1 = nrm(ks[5], (L, 2, CMP_BLOCK * HEAD_DIM, HEAD_DIM), (CMP_BLOCK * HEAD_DIM) ** -0.5)
    cmp_w2 = nrm(ks[6], (L, 2, HEAD_DIM, HEAD_DIM), HEAD_DIM ** -0.5)
    w_out = nrm(ks[7], (L, D, D), beta * D ** -0.5)
    ln1_g = 1.0 + nrm(ks[8], (L, D), 0.02)
    ln1_b = nrm(ks[9], (L, D), 0.02)
    router_w = nrm(ks[10], (L, D, N_EXPERTS), D ** -0.5)
    router_b = nrm(ks[11], (L, N_EXPERTS), 0.01)
    w_gate_up = nrm(ks[12], (L, N_EXPERTS, D, 2 * D_EXPERT), D ** -0.5)
    b_gate_up = nrm(ks[13], (L, N_EXPERTS, 2 * D_EXPERT), 0.01)
    w_down = nrm(ks[14], (L, N_EXPERTS, D_EXPERT, D), beta * D_EXPERT ** -0.5)
    b_down = nrm(ks[15], (L, N_EXPERTS, D), 0.01)
    ln2_g = 1.0 + nrm(ks[16], (L, D), 0.02)
    ln2_b = nrm(ks[17], (L, D), 0.02)
    return {"x": x, "w_in": w_in, "b_in": b_in, "sinks": sinks,
            "cmp_pe": cmp_pe, "cmp_w1": cmp_w1, "cmp_w2": cmp_w2, "w_out": w_out,
            "ln1_g": ln1_g, "ln1_b": ln1_b, "router_w": router_w, "router_b": router_b,
            "w_gate_up": w_gate_up, "b_gate_up": b_gate_up, "w_down": w_down,
            "b_down": b_down, "ln2_g": ln2_g, "ln2_b": ln2_b}


def reference(x, w_in, b_in, sinks, cmp_pe, cmp_w1, cmp_w2, w_out, ln1_g, ln1_b,
              router_w, router_b, w_gate_up, b_gate_up, w_down, b_down, ln2_g, ln2_b):
    alpha = (2.0 * DEPTH) ** 0.25
    B_, S, _ = x.shape
    slopes_swa, slopes_nsa = _alibi_slopes()
    splits = np.cumsum(COL_SIZES)[:-1].tolist()
    for l in range(DEPTH):
        h = x @ w_in[l] + b_in[l]
        (fq, fk, fv, ff, sq, sk, sv, nq, nkc, nvc, nks, nvs, nkw, nvw, ng) = jnp.split(h, splits, axis=-1)
        o_fox = _forgetting_attention(_heads(fq, FOX_HEADS), _heads(fk, FOX_HEADS),
                                      _heads(fv, FOX_HEADS), ff)
        o_swa = _banded_gqa(_heads(sq, KV_HEADS, GQA_GROUP), _heads(sk, KV_HEADS),
                            _heads(sv, KV_HEADS), SWA_WINDOW, slopes_swa,
                            sinks[l].reshape(KV_HEADS, GQA_GROUP))
        o_nsa = _nsa(_heads(nq, KV_HEADS, GQA_GROUP), _heads(nkc, KV_HEADS), _heads(nvc, KV_HEADS),
                     _heads(nks, KV_HEADS), _heads(nvs, KV_HEADS), _heads(nkw, KV_HEADS),
                     _heads(nvw, KV_HEADS), ng, slopes_nsa, cmp_pe[l], cmp_w1[l], cmp_w2[l])
        mix = jnp.concatenate([o_fox.reshape(B_, S, FOX_W), o_swa.reshape(B_, S, SWA_QW),
                               o_nsa.reshape(B_, S, NSA_QW)], axis=-1) @ w_out[l]
        x = _layer_norm(alpha * x + mix, ln1_g[l], ln1_b[l])
        ffn = _moe(x, router_w[l], router_b[l], w_gate_up[l], b_gate_up[l], w_down[l], b_down[l])
        x = _layer_norm(alpha * x + ffn, ln2_g[l], ln2_b[l])
    return x
```

```python
import contextlib
import numpy as np
import ml_dtypes
import concourse.bass as bass
import concourse.mybir as mybir
from concourse.bass_utils import run_bass_kernel_spmd

F32 = mybir.dt.float32
BF16 = mybir.dt.bfloat16
I32 = mybir.dt.int32
ALU = mybir.AluOpType
AF = mybir.ActivationFunctionType
AX = mybir.AxisListType

S = 2048
D = 1024
NT = 16
NEXP = 32
CAP = 768
NSLOT = 1 + NEXP * CAP
NCOLS = 2582
NTC = 15
NTOKC = 662
ALPHA = 4.0 ** 0.25
LN_EPS = 1e-5
NEG = -30000.0
SCALE = 0.125


class Buf:
    __slots__ = ("name", "w", "r")

    def __init__(self, name=""):
        self.name = name
        self.w = None
        self.r = {}


class _Eng:
    def __init__(self, name, eng, sem):
        self.name = name
        self.eng = eng
        self.sem = sem
        self.count = 0
        self.seen = {}


class Tracker:
    def __init__(self, nc, stack, n_dma_sems=24):
        self.nc = nc
        self.e = {}
        for name, eng in (("pe", nc.tensor), ("act", nc.scalar), ("dve", nc.vector),
                          ("pool", nc.gpsimd), ("sp", nc.sync)):
            sem = stack.enter_context(nc.semaphore("prog_" + name))
            self.e[name] = _Eng(name, eng, sem)
        self.dsems = []
        for i in range(n_dma_sems):
            sem = stack.enter_context(nc.semaphore("dma_%d" % i))
            self.dsems.append([sem, 0])
        self.dnext = 0

    def _wait(self, E, tok):
        sem, val = tok
        k = id(sem)
        if E.seen.get(k, 0) >= val:
            return
        E.eng.wait_ge(sem, val)
        E.seen[k] = val

    def _deps(self, E, reads, writes, skip_self):
        toks = []
        for b in reads:
            if b.w is not None:
                toks.append(b.w)
        for b in writes:
            if b.w is not None:
                toks.append(b.w)
            toks.extend(b.r.values())
        for t in toks:
            if skip_self and t[0] is E.sem:
                continue
            self._wait(E, t)

    def _commit(self, tok, reads, writes):
        for b in writes:
            b.w = tok
            b.r = {}
        for b in reads:
            k = id(tok[0])
            if k not in b.r or b.r[k][1] < tok[1]:
                b.r[k] = tok

    def op(self, ename, fn, reads=(), writes=()):
        E = self.e[ename]
        self._deps(E, reads, writes, skip_self=(ename == "pe"))
        ins = fn(E.eng)
        E.count += 1
        ins.then_inc(E.sem, 1)
        tok = (E.sem, E.count)
        self._commit(tok, reads, writes)
        return tok

    def dma(self, qname, fn, reads=(), writes=()):
        E = self.e[qname]
        self._deps(E, reads, writes, skip_self=False)
        slot = self.dsems[self.dnext]
        self.dnext = (self.dnext + 1) % len(self.dsems)
        if slot[1] > 0:
            self._wait(E, (slot[0], slot[1]))
        ins = fn(E.eng)
        slot[1] += 16
        ins.then_inc(slot[0], 16)
        tok = (slot[0], slot[1])
        self._commit(tok, reads, writes)
        return tok

    def barrier(self):
        sp = self.e["sp"]
        for name, E in self.e.items():
            if E is not sp and E.count > 0:
                self._wait(sp, (E.sem, E.count))
        for slot in self.dsems:
            if slot[1] > 0:
                self._wait(sp, (slot[0], slot[1]))
        ins = sp.eng.nop()
        sp.count += 1
        ins.then_inc(sp.sem, 1)
        tok = (sp.sem, sp.count)
        for name, E in self.e.items():
            if E is not sp:
                self._wait(E, tok)
        for name, E in self.e.items():
            for name2, E2 in self.e.items():
                E.seen[id(E2.sem)] = E2.count
            for slot in self.dsems:
                E.seen[id(slot[0])] = slot[1]


def _slopes():
    n = 12
    s = 2.0 ** (-8.0 * np.arange(1, n + 1) / n)
    return s[0::2].astype(np.float64), s[1::2].astype(np.float64)


def _col_perm():
    FOXW, SWAQ, KVW = 256, 384, 128
    off = {}
    names = ["fq", "fk", "fv", "ff", "sq", "sk", "sv", "nq", "nkc", "nvc", "nks", "nvs", "nkw", "nvw", "ng"]
    sizes = [256, 256, 256, 4, 384, 128, 128, 384, 128, 128, 128, 128, 128, 128, 18]
    o = 0
    for n_, s_ in zip(names, sizes):
        off[n_] = o
        o += s_
    cols = []
    r = lambda name, a, b: list(range(off[name] + a, off[name] + b))
    cols += r("fq", 0, 256)
    cols += r("fk", 0, 256)
    for g in range(3):
        cols += r("sq", (0 * 3 + g) * 64, (0 * 3 + g) * 64 + 64) + r("sq", (3 + g) * 64, (3 + g) * 64 + 64)
    cols += r("sk", 0, 128)
    for g in range(3):
        cols += r("nq", (0 * 3 + g) * 64, (0 * 3 + g) * 64 + 64) + r("nq", (3 + g) * 64, (3 + g) * 64 + 64)
    cols += r("nkc", 0, 128)
    cols += r("nvc", 0, 128)
    cols += r("nks", 0, 128)
    cols += r("nkw", 0, 128)
    assert len(cols) == NTC * 128
    cols += r("fv", 0, 256) + r("sv", 0, 128) + r("nvs", 0, 128)
    cols += r("nvw", 0, 128) + r("ff", 0, 4) + r("ng", 0, 18)
    assert len(cols) == NCOLS and len(set(cols)) == NCOLS
    return np.array(cols)


def make_consts():
    c = {}
    bf = ml_dtypes.bfloat16
    swa, nsa = _slopes()
    j = np.arange(128)
    c["ident_bf"] = np.eye(128, dtype=np.float32).astype(bf)
    c["ident_f"] = np.eye(128, dtype=np.float32)
    mC = np.where(j[:, None] > j[None, :], NEG, 0.0).astype(np.float32)
    mW = np.where(j[:, None] <= j[None, :], NEG, 0.0).astype(np.float32)
    c["maskC"] = np.tile(mC, (1, 3)).astype(bf)
    c["maskW"] = np.tile(mW, (1, 3)).astype(bf)
    def alibi(sl, nd):
        t = np.zeros((128, 6, nd), np.float32)
        for h in range(6):
            for d in range(nd):
                t[:, h, d] = sl[h] * (j - 128.0 * d)
        return t
    c["al_swa"] = alibi(swa, 2)
    c["al_win"] = alibi(nsa, 5)
    c["al_sel"] = alibi(nsa, 16)
    c["al_i"] = (swa[None, :] * j[:, None]).astype(np.float32)
    cb = np.zeros((128, 6, 16), np.float32)
    cc = np.arange(128)
    for h in range(6):
        for qb in range(16):
            cb[:, h, qb] = nsa[h] * (16.0 * cc + 31 - 128.0 * qb)
    c["cbias"] = cb
    Z = np.zeros((8, 256), np.float32)
    for r in range(8):
        Z[r, 120 + r] = 1.0
    c["Zc"] = Z.astype(bf)
    R = np.zeros((8, 128), np.float32)
    for r in range(8):
        R[r, :] = np.where(16 * r + 15 > j, NEG, 0.0)
    c["Rc"] = np.tile(R, (1, 3)).astype(bf)
    nf = np.zeros((128, 16, 32), np.float32)
    ad = np.zeros((128, 16, 32), np.float32)
    jb = np.arange(32)
    for qb in range(16):
        t = qb * 128 + j
        cur = t // 64
        forced = (jb[None, :] == 0) | (jb[None, :] == cur[:, None]) | (jb[None, :] == cur[:, None] - 1)
        future = jb[None, :] > cur[:, None]
        nf[:, qb, :] = np.where(future | forced, 0.0, 1.0)
        ad[:, qb, :] = np.where(future, -1.0, np.where(forced, 1.0e4, 0.0))
    c["sel_nf"] = nf
    c["sel_ad"] = ad
    ex = np.zeros((32, 16, 128), np.float32)
    for kb in range(16):
        ex[2 * kb, kb, 0:64] = 1.0
        ex[2 * kb + 1, kb, 64:128] = 1.0
    c["expand"] = ex.astype(bf)
    ncmp = 127
    cs = np.arange(ncmp) * 16
    ss = np.arange(32) * 64
    ov = np.clip(np.minimum(cs[:, None] + 32, ss[None, :] + 64) - np.maximum(cs[:, None], ss[None, :]), 0, None) / 32.0
    ovp = np.zeros((128, 32), np.float32)
    ovp[:127] = ov
    c["ov"] = ovp.astype(bf)
    c["ltri"] = (j[:, None] < j[None, :]).astype(np.float32).astype(bf)
    c["ones_bf"] = np.ones((128, 128), np.float32).astype(bf)
    c["utri_f"] = (j[:, None] <= j[None, :]).astype(np.float32)
    c["ones_f"] = np.ones((128, 128), np.float32)
    c["iota_e"] = np.tile((1.0 + np.arange(32) * CAP)[None, :], (128, 1)).astype(np.float32)
    c["tokid"] = (np.arange(32)[None, :] * 128 + j[:, None]).astype(np.int32)
    meta = np.zeros((NSLOT, 2), np.float32)
    meta[:, 0] = np.array([4096], np.int32).view(np.float32)[0]
    c["meta_init"] = meta
    return c


CONST_DT = {"ident_bf": BF16, "maskC": BF16, "maskW": BF16, "Zc": BF16, "Rc": BF16, "expand": BF16,
            "ov": BF16, "ltri": BF16, "ones_bf": BF16, "tokid": I32}


def build(nseq=2, nlayers=2, dbg=()):
    nc = bass.Bass("TRN2", target_bir_lowering=False)
    NTOK = nseq * S
    NGT = nseq * NT
    L = 2

    def din(name, shape, dt=F32):
        return nc.dram_tensor(name, list(shape), dt, kind="ExternalInput").ap()

    def dscr(name, shape, dt=F32):
        kind = "ExternalOutput" if name in dbg else "Internal"
        return nc.dram_tensor(name, list(shape), dt, kind=kind).ap()

    x_d = din("x", [NTOK, D])
    w_in_d = din("w_in", [L, D, NCOLS])
    bT_d = din("bT", [L, 128, NTC])
    bV_d = din("bV", [L, 1, NTOKC])
    sinks_d = din("sinks", [L, 1, 6])
    peT_d = din("peT", [L, 64, 2, 32])
    w1_d = din("cmp_w1", [L, 2, 2048, 64])
    w2_d = din("cmp_w2", [L, 2, 64, 64])
    wout_d = din("w_out", [L, D, D])
    ln1g_d = din("ln1_g", [L, 1, D]); ln1b_d = din("ln1_b", [L, 1, D])
    ln2g_d = din("ln2_g", [L, 1, D]); ln2b_d = din("ln2_b", [L, 1, D])
    rw_d = din("router_w", [L, D, NEXP]); rb_d = din("router_b", [L, 1, NEXP])
    wgu_d = din("w_gate_up", [L, NEXP, D, 2 * D])
    bgu_d = din("b_gu", [L, NEXP, 128, 16])
    wdn_d = din("w_down", [L, NEXP, D, D])
    bdn_d = din("b_down", [L, NEXP, D])
    consts_np = make_consts()
    cd = {k: din("c_" + k, v.shape, CONST_DT.get(k, F32)) for k, v in consts_np.items()}
    out_d = nc.dram_tensor("out", [NTOK, D], F32, kind="ExternalOutput").ap()

    attn_d = dscr("attn_s", [NTOK, D], BF16)
    x1_d = dscr("x1_s", [NTOK, D])
    xcur_d = dscr("xcur_s", [NTOK, D])
    yacc_d = [dscr("yacc_s%d" % l, [NTOK + 1, D]) for l in range(L)]
    xg_d = dscr("xg_s", [NSLOT, D], BF16)
    meta_d = [dscr("meta_s%d" % l, [NSLOT, 2]) for l in range(L)]

    with contextlib.ExitStack() as gst:
        T = Tracker(nc, gst)

        uid = [0]

        def sbt(st, name, shape, dt=F32):
            uid[0] += 1
            return st.enter_context(nc.sbuf_tensor("s%d_%s" % (uid[0], name), list(shape), dt))

        def pst(st, name, shape, dt=F32):
            uid[0] += 1
            return st.enter_context(nc.psum_tensor("p%d_%s" % (uid[0], name), list(shape), dt))

        def V(fn, r=(), w=()):
            return T.op("dve", fn, r, w)

        def A(fn, r=(), w=()):
            return T.op("act", fn, r, w)

        def G(fn, r=(), w=()):
            return T.op("pool", fn, r, w)

        def M(fn, r=(), w=()):
            return T.op("pe", fn, r, w)

        def LD(out, in_, r=(), w=(), q="sp"):
            return T.dma(q, lambda e: e.dma_start(out=out, in_=in_), r, w)

        C = {}
        for k, v in consts_np.items():
            if k == "meta_init":
                continue
            C[k] = sbt(gst, "k_" + k, v.shape, CONST_DT.get(k, F32))
            LD(C[k][:], cd[k])
        acum = sbt(gst, "acum", [128, NEXP], BF16)
        B_acum = Buf()
        zero_t = sbt(gst, "zero_t", [128, D])
        V(lambda e: e.memset(zero_t[:], 0.0))
        T.barrier()

        for l in range(nlayers):
            x_src = x_d if l == 0 else xcur_d
            x_dst = out_d if l == nlayers - 1 else xcur_d
            G(lambda e: e.memset(acum[:], 0.0), w=[B_acum])
            for r0 in range(0, NTOK + 1, 128):
                rows = min(128, NTOK + 1 - r0)
                LD(yacc_d[l][r0:r0 + rows, :], zero_t[0:rows, :])
            T.dma("pool", lambda e: e.dma_start(out=meta_d[l], in_=cd["meta_init"]))
            T.barrier()

            for b in range(nseq):
                with contextlib.ExitStack() as sst:
                    hT = sbt(sst, "hT", [128, NTC, S], BF16)
                    vaug = sbt(sst, "vaug", [128, NT, 10, 65], BF16)
                    fg = sbt(sst, "fg", [128, NT, 22])
                    phase_A(nc, T, C, l, b, x_src, w_in_d, bT_d, bV_d, hT, vaug, fg, sbt, pst, V, A, G, M, LD)
                    T.barrier()
                    phase_B(nc, T, C, l, b, hT, vaug, fg, sinks_d, peT_d, w1_d, w2_d, attn_d,
                            sbt, pst, V, A, G, M, LD)
                    T.barrier()
                phase_C(nc, T, C, l, b, x_src, attn_d, wout_d, ln1g_d, ln1b_d, rw_d, rb_d, x1_d, xg_d, meta_d[l],
                        acum, B_acum, sbt, pst, V, A, G, M, LD)
                T.barrier()
            phase_D(nc, T, C, l, wgu_d, bgu_d, wdn_d, bdn_d, xg_d, meta_d[l], yacc_d[l], sbt, pst, V, A, G, M, LD)
            T.barrier()
            phase_E(nc, T, C, l, NGT, x1_d, yacc_d[l], ln2g_d, ln2b_d, x_dst, sbt, pst, V, A, G, M, LD)
            T.barrier()
    return nc, consts_np


def phase_A(nc, T, C, l, b, x_src, w_in_d, bT_d, bV_d, hT, vaug, fg, sbt, pst, V, A, G, M, LD):
    with contextlib.ExitStack() as st:
        wbf = sbt(st, "wbf", [128, 8, NCOLS], BF16)
        xT = sbt(st, "xT", [128, 8, S], BF16)
        bT = sbt(st, "bT", [128, NTC])
        bV = sbt(st, "bV", [128, NTOKC])
        HW = NCOLS // 2
        wst = [sbt(st, "wst%d" % i, [128, HW]) for i in range(2)]
        xst = [sbt(st, "xst%d" % i, [128, D]) for i in range(2)]
        xbf = [sbt(st, "xbf%d" % i, [128, D], BF16) for i in range(2)]
        psT = [pst(st, "psTa%d" % i, [128, D], BF16) for i in range(2)]
        psA = [pst(st, "psA%d" % i, [128, 512]) for i in range(2)]
        psV0 = pst(st, "psV0", [128, 512])
        psV1 = pst(st, "psV1", [128, 512])
        B_wst = [Buf(), Buf()]; B_wbf = Buf(); B_xst = [Buf(), Buf()]; B_xbf = [Buf(), Buf()]
        B_psT = [Buf(), Buf()]; B_xT = Buf(); B_psA = [Buf(), Buf()]; B_bias = Buf()
        B_pV = Buf(); B_h = Buf()
        LD(bT[:], bT_d[l], w=[B_bias])
        LD(bV[:], bV_d[l].partition_broadcast(128), w=[B_bias])
        G(lambda e: e.memset(vaug[:, :, :, 64:65], 1.0), w=[B_h])
        wv = w_in_d[l].rearrange("(k p) n -> p k n", p=128)
        i = 0
        for k in range(8):
            for hh in range(2):
                s_ = i % 2
                LD(wst[s_][:], wv[:, k, hh * HW:(hh + 1) * HW], w=[B_wst[s_]])
                fn = lambda e, s_=s_, k=k, hh=hh: e.tensor_copy(out=wbf[:, k, hh * HW:(hh + 1) * HW], in_=wst[s_][:])
                (G if i % 2 else V)(fn, r=[B_wst[s_]], w=[B_wbf])
                i += 1
        for tt in range(NT):
            s_ = tt % 2
            r0 = b * S + tt * 128
            LD(xst[s_][:], x_src[r0:r0 + 128, :], w=[B_xst[s_]])
            (V if tt % 2 else G)(lambda e, s_=s_: e.tensor_copy(out=xbf[s_][:], in_=xst[s_][:]), r=[B_xst[s_]], w=[B_xbf[s_]])
            for k in range(8):
                M(lambda e, s_=s_, k=k: e.transpose(out=psT[s_][:, k * 128:(k + 1) * 128], in_=xbf[s_][:, k * 128:(k + 1) * 128],
                                                     identity=C["ident_bf"][:]), r=[B_xbf[s_]], w=[B_psT[s_]])
            fn = lambda e, s_=s_, tt=tt: e.tensor_copy(out=xT[:, :, tt * 128:(tt + 1) * 128],
                                                       in_=psT[s_][:].rearrange("p (k t) -> p k t", k=8))
            (V if tt % 2 == 0 else A)(fn if tt % 2 == 0 else (lambda e, s_=s_, tt=tt: e.copy(out=xT[:, :, tt * 128:(tt + 1) * 128],
                                       in_=psT[s_][:].rearrange("p (k t) -> p k t", k=8))), r=[B_psT[s_]], w=[B_xT])
        i = 0
        for c in range(NTC):
            for tq in range(4):
                s_ = i % 2
                for k in range(8):
                    M(lambda e, s_=s_, k=k, c=c, tq=tq: e.matmul(psA[s_][:], lhsT=wbf[:, k, c * 128:(c + 1) * 128],
                                                                rhs=xT[:, k, tq * 512:(tq + 1) * 512], start=(k == 0), stop=(k == 7)),
                      r=[B_wbf, B_xT], w=[B_psA[s_]])
                if i % 2 == 0:
                    A(lambda e, s_=s_, c=c, tq=tq: e.activation(out=hT[:, c, tq * 512:(tq + 1) * 512], in_=psA[s_][:], func=AF.Identity,
                                                               bias=bT[:, c:c + 1], scale=1.0), r=[B_psA[s_], B_bias], w=[B_h])
                else:
                    V(lambda e, s_=s_, c=c, tq=tq: e.tensor_scalar(out=hT[:, c, tq * 512:(tq + 1) * 512], in0=psA[s_][:],
                                                                  scalar1=bT[:, c:c + 1], scalar2=None, op0=ALU.add),
                      r=[B_psA[s_], B_bias], w=[B_h])
                i += 1
        for tt in range(NT):
            for k in range(8):
                M(lambda e, k=k, tt=tt: e.matmul(psV0[:], lhsT=xT[:, k, tt * 128:(tt + 1) * 128], rhs=wbf[:, k, 1920:2432],
                                                 start=(k == 0), stop=(k == 7)), r=[B_wbf, B_xT], w=[B_pV])
            for k in range(8):
                M(lambda e, k=k, tt=tt: e.matmul(psV1[:, 0:150], lhsT=xT[:, k, tt * 128:(tt + 1) * 128], rhs=wbf[:, k, 2432:2582],
                                                 start=(k == 0), stop=(k == 7)), r=[B_wbf, B_xT], w=[B_pV])
            V(lambda e, tt=tt: e.tensor_tensor(out=vaug[:, tt, 0:8, 0:64], in0=psV0[:].rearrange("p (h d) -> p h d", h=8),
                                               in1=bV[:, 0:512].rearrange("p (h d) -> p h d", h=8), op=ALU.add),
              r=[B_pV, B_bias], w=[B_h])
            V(lambda e, tt=tt: e.tensor_tensor(out=vaug[:, tt, 8:10, 0:64], in0=psV1[:, 0:128].rearrange("p (h d) -> p h d", h=2),
                                               in1=bV[:, 512:640].rearrange("p (h d) -> p h d", h=2), op=ALU.add),
              r=[B_pV, B_bias], w=[B_h])
            V(lambda e, tt=tt: e.tensor_tensor(out=fg[:, tt, :], in0=psV1[:, 128:150], in1=bV[:, 640:662], op=ALU.add),
              r=[B_pV, B_bias], w=[B_h])


def phase_B(nc, T, C, l, b, hT, vaug, fg, sinks_d, peT_d, w1_d, w2_d, attn_d, sbt, pst, V, A, G, M, LD):
    with contextlib.ExitStack() as st:
        w1st = sbt(st, "w1st", [128, 2, 32, 64])
        w1bf = sbt(st, "w1bf", [128, 2, 32, 64], BF16)
        w2st = sbt(st, "w2st", [64, 2, 64])
        w2bf = sbt(st, "w2bf", [64, 2, 64], BF16)
        peT = sbt(st, "peT", [64, 2, 32])
        peTb = sbt(st, "peTb", [64, 2, 32], BF16)
        cbc = sbt(st, "cbc", [64, 2])
        sinkb = sbt(st, "sinkb", [128, 6])
        sinkf = sbt(st, "sinkf", [128, 6])
        sgate = sbt(st, "sgate", [128, NT, 18])
        lpos = sbt(st, "lpos", [128, NT, 4])
        tot = sbt(st, "tot", [128, NT, 4])
        Lpre = sbt(st, "Lpre", [128, NT, 4])
        Lc = sbt(st, "Lc", [128, NT, 4])
        KcT = sbt(st, "KcT", [128, 128], BF16)
        VcA = sbt(st, "VcA", [128, 2, 97], BF16)
        gel = [sbt(st, "gel%d" % i, [64, 128]) for i in range(4)]
        Gt = sbt(st, "Gt", [64, 128], BF16)
        psS = [pst(st, "psS%d" % i, [128, 512]) for i in range(2)]
        acc = [pst(st, "acc%d" % i, [128, 512]) for i in range(3)]
        psO = pst(st, "psO", [128, 3, 128])
        psX = pst(st, "psX", [128, 512])
        B_psS = [Buf(), Buf()]; B_acc = [Buf(), Buf(), Buf()]; B_psO = Buf(); B_psX = Buf()
        B0 = Buf()
        NE = 6
        Et = [sbt(st, "Et%d" % i, [128, 128], BF16) for i in range(NE)]
        B_Et = [Buf() for _ in range(NE)]
        ei = [0]
        fb = [sbt(st, "fb%d" % i, [128, NT]) for i in range(2)]
        B_fb = [Buf(), Buf()]
        attn_t = [sbt(st, "attn_t%d" % i, [128, D], BF16) for i in range(2)]
        B_at = [Buf(), Buf()]
        sm = [sbt(st, "sm%d" % i, [128, 16]) for i in range(4)]
        B_sm = [Buf() for _ in range(4)]
        smi = [0]
        onsa = [sbt(st, "onsa%d" % i, [128, 64]) for i in range(3)]
        B_on = [Buf(), Buf(), Buf()]
        pslc = sbt(st, "pslc", [128, 32]); score = sbt(st, "score", [128, 32]); m8 = sbt(st, "m8", [128, 8])
        nsel = sbt(st, "nsel", [128, 32]); nselT = sbt(st, "nselT", [32, 3, 128], BF16)
        rdc = sbt(st, "rdc", [128, 4]); gco = sbt(st, "gco", [128, 4])
        B_sel = Buf(); B_nselT = Buf(); B_rdc = Buf()

        w1v = w1_d[l].rearrange("w (l d) j -> d w l j", d=64)
        LD(w1st[0:64], w1v, w=[B0])
        LD(w1st[64:128], w1v, w=[B0])
        LD(w2st[:], w2_d[l].rearrange("w j k -> j w k"), w=[B0])
        LD(peT[:], peT_d[l], w=[B0])
        LD(sinkb[:], sinks_d[l].partition_broadcast(128), w=[B0])
        V(lambda e: e.tensor_copy(out=w1bf[:], in_=w1st[:]), r=[B0], w=[B0])
        V(lambda e: e.tensor_copy(out=w2bf[:], in_=w2st[:]), r=[B0], w=[B0])
        V(lambda e: e.tensor_copy(out=peTb[:], in_=peT[:]), r=[B0], w=[B0])
        V(lambda e: e.tensor_tensor(out=sinkb[:], in0=sinkb[:], in1=C["al_i"][:], op=ALU.add), r=[B0], w=[B0])
        A(lambda e: e.activation(out=sinkf[:], in_=sinkb[:], func=AF.Exp), r=[B0], w=[B0])
        A(lambda e: e.activation(out=sgate[:], in_=fg[:, :, 4:22], func=AF.Sigmoid), r=[B0], w=[B0])
        A(lambda e: e.activation(out=lpos[:], in_=fg[:, :, 0:4], func=AF.Exp, scale=-1.0), r=[B0], w=[B0])
        A(lambda e: e.activation(out=lpos[:], in_=lpos[:], func=AF.Ln, bias=1.0, scale=1.0), r=[B0], w=[B0])
        lp2 = lpos[:].rearrange("p a h -> p (a h)")
        M(lambda e: e.matmul(psX[:, 0:64], lhsT=C["utri_f"][:], rhs=lp2, start=True, stop=True), r=[B0], w=[B_psX])
        M(lambda e: e.matmul(psX[:, 64:128], lhsT=C["ones_f"][:], rhs=lp2, start=True, stop=True), r=[B0], w=[B_psX])
        V(lambda e: e.tensor_copy(out=tot[:].rearrange("p a h -> p (a h)"), in_=psX[:, 64:128]), r=[B_psX], w=[B0])
        V(lambda e: e.memset(Lpre[:, 0, :], 0.0), r=[B0], w=[B0])
        for tt in range(1, NT):
            V(lambda e, tt=tt: e.tensor_tensor(out=Lpre[:, tt, :], in0=Lpre[:, tt - 1, :], in1=tot[:, tt - 1, :], op=ALU.add), r=[B0], w=[B0])
        V(lambda e: e.tensor_tensor(out=Lc[:].rearrange("p a h -> p (a h)"), in0=psX[:, 0:64],
                                    in1=Lpre[:].rearrange("p a h -> p (a h)"), op=ALU.add), r=[B_psX, B0], w=[B0])
        for wh in range(2):
            for ll in range(32):
                M(lambda e, wh=wh, ll=ll: e.matmul(psX[0:64, 200 + wh:201 + wh], lhsT=w1bf[0:64, wh, ll, :], rhs=peTb[0:64, wh, ll:ll + 1],
                                                   start=(ll == 0), stop=(ll == 31)), r=[B0], w=[B_psX])
        V(lambda e: e.tensor_copy(out=cbc[:], in_=psX[0:64, 200:202]), r=[B_psX], w=[B0])
        V(lambda e: e.tensor_copy(out=VcA[:, 0, 65:97], in_=C["ov"][:]), r=[B0], w=[B0])
        V(lambda e: e.tensor_copy(out=VcA[:, 1, 65:97], in_=C["ov"][:]), r=[B0], w=[B0])
        V(lambda e: e.memset(VcA[:, :, 64:65], 1.0), r=[B0], w=[B0])
        for kvh in range(2):
            P = slice(kvh * 64, kvh * 64 + 64)
            for wh in range(2):
                for ll in range(32):
                    M(lambda e, wh=wh, ll=ll, P=P: e.matmul(psX[0:64, 0:127], lhsT=w1bf[P, wh, ll, :],
                                                          rhs=hT[P, 11 + wh, ll:ll + 2017:16], start=(ll == 0), stop=(ll == 31)),
                      r=[B0], w=[B_psX])
                u, x2, inner, sg = gel
                A(lambda e, wh=wh: e.activation(out=u[:, 0:127], in_=psX[0:64, 0:127], func=AF.Identity, bias=cbc[:, wh:wh + 1], scale=1.0),
                  r=[B_psX, B0], w=[B0])
                V(lambda e: e.tensor_tensor(out=x2[:, 0:127], in0=u[:, 0:127], in1=u[:, 0:127], op=ALU.mult), r=[B0], w=[B0])
                V(lambda e: e.tensor_scalar(out=x2[:, 0:127], in0=x2[:, 0:127], scalar1=0.044715, scalar2=1.0, op0=ALU.mult, op1=ALU.add), r=[B0], w=[B0])
                V(lambda e: e.tensor_tensor(out=inner[:, 0:127], in0=x2[:, 0:127], in1=u[:, 0:127], op=ALU.mult), r=[B0], w=[B0])
                A(lambda e: e.activation(out=sg[:, 0:127], in_=inner[:, 0:127], func=AF.Sigmoid, scale=1.5957691216057308), r=[B0], w=[B0])
                V(lambda e: e.tensor_tensor(out=Gt[:, 0:127], in0=u[:, 0:127], in1=sg[:, 0:127], op=ALU.mult), r=[B0], w=[B0])
                if wh == 0:
                    M(lambda e, P=P: e.matmul(psX[P, 256:383], lhsT=w2bf[0:64, 0, :], rhs=Gt[0:64, 0:127], start=True, stop=True),
                      r=[B0], w=[B_psX])
                    V(lambda e, P=P: e.tensor_copy(out=KcT[P, 0:127], in_=psX[P, 256:383]), r=[B_psX], w=[B0])
                else:
                    M(lambda e: e.matmul(psX[0:127, 384:448], lhsT=Gt[0:64, 0:127], rhs=w2bf[0:64, 1, :], start=True, stop=True),
                      r=[B0], w=[B_psX])
                    V(lambda e, kvh=kvh: e.tensor_copy(out=VcA[0:127, kvh, 0:64], in_=psX[0:127, 384:448]), r=[B_psX], w=[B0])

        def new_sm():
            i_ = smi[0] % 4
            smi[0] += 1
            return sm[i_], B_sm[i_]

        def exp_pv(ps_ap, Bps, bias_ap, acc_i, v_ap, first, last, kparts=128, n_out=65, acc_ap=None, rb=()):
            i_ = ei[0] % NE
            ei[0] += 1
            A(lambda e: e.activation(out=Et[i_][0:kparts, :], in_=ps_ap, func=AF.Exp, bias=bias_ap, scale=SCALE),
              r=[Bps, B0] + list(rb), w=[B_Et[i_]])
            oap = acc[acc_i][:, 0:n_out] if acc_ap is None else acc_ap
            M(lambda e: e.matmul(oap, lhsT=Et[i_][0:kparts, :], rhs=v_ap, start=first, stop=last),
              r=[B_Et[i_], B0], w=[B_acc[acc_i]] if acc_ap is None else [B_psO])

        si = [0]

        def score_mm(lhsT_ap, rhs_ap, ncol, extra, mparts=128):
            s_ = si[0] % 2
            si[0] += 1
            nmm = 1 + len(extra)
            M(lambda e: e.matmul(psS[s_][0:mparts, 0:ncol], lhsT=lhsT_ap, rhs=rhs_ap, start=True, stop=(nmm == 1)),
              r=[B0, B_nselT], w=[B_psS[s_]])
            for j_, (la, ra) in enumerate(extra):
                M(lambda e, la=la, ra=ra, j_=j_: e.matmul(psS[s_][0:mparts, 0:ncol], lhsT=la, rhs=ra, start=False, stop=(j_ == nmm - 2)),
                  r=[B0, B_nselT], w=[B_psS[s_]])
            return s_

        for qb in range(NT):
            at = attn_t[qb % 2]
            Bat = B_at[qb % 2]
            qs = slice(qb * 128, (qb + 1) * 128)
            for h in range(4):
                P = slice((h % 2) * 64, (h % 2) * 64 + 64)
                qc, kc = h // 2, 2 + h // 2
                f_ = fb[h % 2]; Bf = B_fb[h % 2]
                V(lambda e, h=h, f_=f_: e.tensor_scalar(out=f_[:, 0:qb + 1], in0=Lc[:, 0:qb + 1, h], scalar1=Lpre[:, qb, h:h + 1],
                                                        scalar2=None, op0=ALU.subtract), r=[B0], w=[Bf])
                ai = h % 3
                for kb in range(qb + 1):
                    ks = slice(kb * 128, (kb + 1) * 128)
                    extra = [(C["ident_bf"][:], C["maskC"][:, 0:128])] if kb == qb else []
                    s_ = score_mm(hT[P, kc, ks], hT[P, qc, qs], 128, extra)
                    exp_pv(psS[s_][:, 0:128], B_psS[s_], f_[:, kb:kb + 1], ai, vaug[:, kb, h, :], kb == 0, kb == qb, rb=[Bf])
                t_, Bt = new_sm()
                V(lambda e, t_=t_, ai=ai: e.reciprocal(out=t_[:, 0:1], in_=acc[ai][:, 64:65]), r=[B_acc[ai]], w=[Bt])
                V(lambda e, t_=t_, ai=ai, h=h: e.tensor_scalar(out=at[:, h * 64:(h + 1) * 64], in0=acc[ai][:, 0:64], scalar1=t_[:, 0:1],
                                                             scalar2=None, op0=ALU.mult), r=[B_acc[ai], Bt], w=[Bat])
            for kvh in range(2):
                P = slice(kvh * 64, kvh * 64 + 64)
                kbs = ([(qb - 1, "W")] if qb >= 1 else []) + [(qb, "C")]
                for n_, (kb, mk) in enumerate(kbs):
                    ks = slice(kb * 128, (kb + 1) * 128)
                    s_ = score_mm(hT[P, 7, ks], hT[P, 4:7, qs], 384, [(C["ident_bf"][:], C["mask" + mk][:])])
                    for g in range(3):
                        hh = kvh * 3 + g
                        exp_pv(psS[s_][:, g * 128:(g + 1) * 128], B_psS[s_], C["al_swa"][:, hh, qb - kb:qb - kb + 1], g,
                               vaug[:, kb, 4 + kvh, :], n_ == 0, n_ == len(kbs) - 1)
                for g in range(3):
                    hh = kvh * 3 + g
                    t_, Bt = new_sm()
                    V(lambda e, t_=t_, g=g, hh=hh: e.tensor_tensor(out=t_[:, 0:1], in0=acc[g][:, 64:65], in1=sinkf[:, hh:hh + 1], op=ALU.add),
                      r=[B_acc[g], B0], w=[Bt])
                    V(lambda e, t_=t_: e.reciprocal(out=t_[:, 1:2], in_=t_[:, 0:1]), r=[Bt], w=[Bt])
                    V(lambda e, t_=t_, g=g, hh=hh: e.tensor_scalar(out=at[:, 256 + hh * 64:256 + (hh + 1) * 64], in0=acc[g][:, 0:64],
                                                                 scalar1=t_[:, 1:2], scalar2=None, op0=ALU.mult), r=[B_acc[g], Bt], w=[Bat])
            for kvh in range(2):
                P = slice(kvh * 64, kvh * 64 + 64)
                Q = hT[P, 8:11, qs]
                ncols = min(127, 8 * qb + 7)
                off = 121 - 8 * qb
                s_ = score_mm(KcT[P, 0:ncols], Q, 384, [(C["Zc"][0:8, off:off + ncols], C["Rc"][0:8, :])], mparts=ncols)
                for g in range(3):
                    hh = kvh * 3 + g
                    exp_pv(psS[s_][0:ncols, g * 128:(g + 1) * 128], B_psS[s_], C["cbias"][0:ncols, hh, qb:qb + 1], None,
                           VcA[0:ncols, kvh, :], True, True, kparts=ncols, acc_ap=psO[:, g, 0:97])
                V(lambda e: e.tensor_scalar(out=rdc[:, 0:3], in0=psO[:, :, 64], scalar1=1e-30, scalar2=None, op0=ALU.max),
                  r=[B_psO], w=[B_rdc])
                V(lambda e: e.reciprocal(out=rdc[:, 0:3], in_=rdc[:, 0:3]), r=[B_rdc], w=[B_rdc])
                V(lambda e: e.tensor_scalar(out=pslc[:], in0=psO[:, 0, 65:97], scalar1=rdc[:, 0:1], scalar2=None, op0=ALU.mult),
                  r=[B_psO, B_rdc], w=[B_sel])
                for g in (1, 2):
                    V(lambda e, g=g: e.scalar_tensor_tensor(out=pslc[:], in0=psO[:, g, 65:97], scalar=rdc[:, g:g + 1], in1=pslc[:],
                                                            op0=ALU.mult, op1=ALU.add), r=[B_psO, B_rdc, B_sel], w=[B_sel])
                V(lambda e: e.tensor_tensor(out=score[:], in0=pslc[:], in1=C["sel_nf"][:, qb, :], op=ALU.mult), r=[B_sel], w=[B_sel])
                V(lambda e: e.tensor_tensor(out=score[:], in0=score[:], in1=C["sel_ad"][:, qb, :], op=ALU.add), r=[B_sel], w=[B_sel])
                V(lambda e: e.max(out=m8[:], in_=score[:]), r=[B_sel], w=[B_sel])
                V(lambda e: e.tensor_scalar(out=nsel[:], in0=score[:], scalar1=m8[:, 7:8], scalar2=1.0, op0=ALU.is_ge, op1=ALU.subtract),
                  r=[B_sel], w=[B_sel])
                M(lambda e: e.transpose(out=psX[0:32, 0:128], in_=nsel[:], identity=C["ident_f"][:]), r=[B_sel], w=[B_psX])
                for g in range(3):
                    A(lambda e, g=g: e.activation(out=nselT[:, g, :], in_=psX[0:32, 0:128], func=AF.Copy, scale=-NEG),
                      r=[B_psX], w=[B_nselT])
                for kb in range(qb + 1):
                    ks = slice(kb * 128, (kb + 1) * 128)
                    extra = [(C["expand"][:, kb, :], nselT[:].rearrange("j g t -> j (g t)"))]
                    if kb == qb:
                        extra.append((C["ident_bf"][:], C["maskC"][:]))
                    s_ = score_mm(hT[P, 13, ks], Q, 384, extra)
                    for g in range(3):
                        hh = kvh * 3 + g
                        exp_pv(psS[s_][:, g * 128:(g + 1) * 128], B_psS[s_], C["al_sel"][:, hh, qb - kb:qb - kb + 1], g,
                               vaug[:, kb, 6 + kvh, :], kb == 0, kb == qb)
                for g in range(3):
                    hh = kvh * 3 + g
                    t_, Bt = new_sm()
                    V(lambda e, t_=t_, g=g: e.reciprocal(out=t_[:, 0:1], in_=acc[g][:, 64:65]), r=[B_acc[g]], w=[Bt])
                    V(lambda e, t_=t_, hh=hh: e.tensor_tensor(out=t_[:, 1:2], in0=t_[:, 0:1], in1=sgate[:, qb, hh * 3 + 1:hh * 3 + 2], op=ALU.mult),
                      r=[Bt, B0], w=[Bt])
                    V(lambda e, t_=t_, g=g: e.tensor_scalar(out=onsa[g][:], in0=acc[g][:, 0:64], scalar1=t_[:, 1:2], scalar2=None, op0=ALU.mult),
                      r=[B_acc[g], Bt], w=[B_on[g]])
                kbs = ([(qb - 4, "W")] if qb >= 4 else []) + [(kb, None) for kb in range(max(0, qb - 3), qb)] + [(qb, "C")]
                for n_, (kb, mk) in enumerate(kbs):
                    ks = slice(kb * 128, (kb + 1) * 128)
                    extra = [(C["ident_bf"][:], C["mask" + mk][:])] if mk else []
                    s_ = score_mm(hT[P, 14, ks], Q, 384, extra)
                    for g in range(3):
                        hh = kvh * 3 + g
                        exp_pv(psS[s_][:, g * 128:(g + 1) * 128], B_psS[s_], C["al_win"][:, hh, qb - kb:qb - kb + 1], g,
                               vaug[:, kb, 8 + kvh, :], n_ == 0, n_ == len(kbs) - 1)
                for g in range(3):
                    hh = kvh * 3 + g
                    t_, Bt = new_sm()
                    V(lambda e, t_=t_, g=g: e.reciprocal(out=t_[:, 0:1], in_=acc[g][:, 64:65]), r=[B_acc[g]], w=[Bt])
                    V(lambda e, t_=t_, hh=hh: e.tensor_tensor(out=t_[:, 1:2], in0=t_[:, 0:1], in1=sgate[:, qb, hh * 3 + 2:hh * 3 + 3], op=ALU.mult),
                      r=[Bt, B0], w=[Bt])
                    V(lambda e, t_=t_, g=g: e.scalar_tensor_tensor(out=onsa[g][:], in0=acc[g][:, 0:64], scalar=t_[:, 1:2], in1=onsa[g][:],
                                                                  op0=ALU.mult, op1=ALU.add), r=[B_acc[g], Bt, B_on[g]], w=[B_on[g]])
                    V(lambda e, t_=t_, g=g, hh=hh: e.tensor_tensor(out=t_[:, 2:3], in0=rdc[:, g:g + 1], in1=sgate[:, qb, hh * 3:hh * 3 + 1], op=ALU.mult),
                      r=[B_rdc, B0, Bt], w=[Bt])
                    V(lambda e, t_=t_, g=g, hh=hh: e.scalar_tensor_tensor(out=at[:, 640 + hh * 64:640 + (hh + 1) * 64], in0=psO[:, g, 0:64],
                                                                         scalar=t_[:, 2:3], in1=onsa[g][:], op0=ALU.mult, op1=ALU.add),
                      r=[B_psO, Bt, B_on[g]], w=[Bat])
            r0 = b * S + qb * 128
            LD(attn_d[r0:r0 + 128, :], at[:], r=[Bat])


def layer_norm(V, A, G, xin, Bx, s1, g_bc, b_bc, xo, Bxo, tmp, Bt, sm, Bsm, Bc):
    V(lambda e: e.tensor_tensor(out=sm[:, 2:3], in0=s1[:, 0:1], in1=s1[:, 1:2], op=ALU.add), r=[Bsm], w=[Bsm])
    V(lambda e: e.tensor_scalar(out=sm[:, 3:4], in0=sm[:, 2:3], scalar1=1.0 / D, scalar2=None, op0=ALU.mult), r=[Bsm], w=[Bsm])
    V(lambda e: e.tensor_scalar(out=xin[:], in0=xin[:], scalar1=sm[:, 3:4], scalar2=None, op0=ALU.subtract), r=[Bx, Bsm], w=[Bx])
    V(lambda e: e.memset(sm[:, 4:5], 0.0), r=[Bsm], w=[Bsm])
    A(lambda e: e.activation(out=tmp[:], in_=xin[:], func=AF.Square, accum_out=sm[:, 4:5]), r=[Bx, Bsm], w=[Bt, Bsm])
    A(lambda e: e.activation(out=sm[:, 5:6], in_=sm[:, 4:5], func=AF.Sqrt, bias=LN_EPS, scale=1.0 / D), r=[Bsm], w=[Bsm])
    V(lambda e: e.reciprocal(out=sm[:, 6:7], in_=sm[:, 5:6]), r=[Bsm], w=[Bsm])
    V(lambda e: e.scalar_tensor_tensor(out=tmp[:], in0=xin[:], scalar=sm[:, 6:7], in1=g_bc[:], op0=ALU.mult, op1=ALU.mult),
      r=[Bx, Bsm, Bc], w=[Bt])
    G(lambda e: e.tensor_tensor(out=xo[:], in0=tmp[:], in1=b_bc[:], op=ALU.add), r=[Bt, Bc], w=[Bxo])


def phase_C(nc, T, C, l, b, x_src, attn_d, wout_d, ln1g_d, ln1b_d, rw_d, rb_d, x1_d, xg_d, meta_dl,
            acum, B_acum, sbt, pst, V, A, G, M, LD):
    with contextlib.ExitStack() as st:
        wobf = sbt(st, "wobf", [128, 8, D], BF16)
        wost = [sbt(st, "wost%d" % i, [128, D]) for i in range(2)]
        g_bc = sbt(st, "g_bc", [128, D]); b_bc = sbt(st, "b_bc", [128, D])
        rw = sbt(st, "rw", [128, 8, NEXP]); rb = sbt(st, "rb", [128, NEXP])
        att = [sbt(st, "att%d" % i, [128, D], BF16) for i in range(2)]
        attT = [sbt(st, "attT%d" % i, [128, 8, 128], BF16) for i in range(2)]
        xt = [sbt(st, "xt%d" % i, [128, D]) for i in range(2)]
        x1p = [sbt(st, "x1p%d" % i, [128, D]) for i in range(2)]
        x1 = [sbt(st, "x1_%d" % i, [128, D]) for i in range(2)]
        x1b = [sbt(st, "x1b%d" % i, [128, D], BF16) for i in range(2)]
        x1T = [sbt(st, "x1T%d" % i, [128, 8, 128]) for i in range(2)]
        tmp = sbt(st, "tmpC", [128, D])
        sm = [sbt(st, "smC%d" % i, [128, 8]) for i in range(2)]
        rt = [sbt(st, "rt%d" % i, [128, 8, NEXP]) for i in range(2)]
        rtb = [sbt(st, "rtb%d" % i, [128, NEXP], BF16) for i in range(2)]
        m8 = [sbt(st, "m8C%d" % i, [128, 16]) for i in range(2)]
        wk = [sbt(st, "wk%d" % i, [128, 8]) for i in range(2)]
        dsti = [sbt(st, "dsti%d" % i, [128, 4], I32) for i in range(2)]
        meta = [sbt(st, "metaC%d" % i, [128, 4, 2]) for i in range(2)]
        psT = pst(st, "psTc", [128, D], BF16)
        psM = [pst(st, "psM%d" % i, [128, 512]) for i in range(2)]
        psF = [pst(st, "psF%d" % i, [128, 512]) for i in range(2)]
        psR = pst(st, "psR", [128, 512])
        Bw = Buf(); Bwst = [Buf(), Buf()]; Bc = Buf()
        Batt = [Buf(), Buf()]; BattT = [Buf(), Buf()]; Bxt = [Buf(), Buf()]; Bx1p = [Buf(), Buf()]
        Bx1 = [Buf(), Buf()]; Bx1b = [Buf(), Buf()]; Bx1T = [Buf(), Buf()]; Bt = Buf(); Bsm = [Buf(), Buf()]
        Brt = [Buf(), Buf()]; BpsT = Buf(); BpsM = Buf(); BpsF = Buf(); BpsR = Buf(); Bmeta = [Buf(), Buf()]
        wv = wout_d[l].rearrange("(k p) n -> p k n", p=128)
        for k in range(8):
            s_ = k % 2
            LD(wost[s_][:], wv[:, k, :], w=[Bwst[s_]])
            (V if k % 2 else G)(lambda e, s_=s_, k=k: e.tensor_copy(out=wobf[:, k, :], in_=wost[s_][:]), r=[Bwst[s_]], w=[Bw])
        LD(g_bc[:], ln1g_d[l].partition_broadcast(128), w=[Bc])
        LD(b_bc[:], ln1b_d[l].partition_broadcast(128), w=[Bc])
        LD(rw[:], rw_d[l].rearrange("(k p) n -> p k n", p=128), w=[Bc])
        LD(rb[:], rb_d[l].partition_broadcast(128), w=[Bc])
        for tt in range(NT):
            s_ = tt % 2
            gt = b * NT + tt
            r0 = gt * 128
            LD(att[s_][:], attn_d[r0:r0 + 128, :], w=[Batt[s_]])
            LD(xt[s_][:], x_src[r0:r0 + 128, :], w=[Bxt[s_]])
            for k in range(8):
                M(lambda e, s_=s_, k=k: e.transpose(out=psT[:, k * 128:(k + 1) * 128], in_=att[s_][:, k * 128:(k + 1) * 128],
                                                     identity=C["ident_bf"][:]), r=[Batt[s_]], w=[BpsT])
            A(lambda e, s_=s_: e.copy(out=attT[s_][:], in_=psT[:].rearrange("p (k t) -> p k t", k=8)), r=[BpsT], w=[BattT[s_]])
            for nh in range(2):
                for k in range(8):
                    M(lambda e, s_=s_, k=k, nh=nh: e.matmul(psM[nh][:], lhsT=attT[s_][:, k, :], rhs=wobf[:, k, nh * 512:(nh + 1) * 512],
                                                           start=(k == 0), stop=(k == 7)), r=[BattT[s_], Bw], w=[BpsM])
            V(lambda e, s_=s_: e.memset(sm[s_][:, 0:2], 0.0), w=[Bsm[s_]])
            for nh in range(2):
                V(lambda e, s_=s_, nh=nh: e.scalar_tensor_tensor(out=x1p[s_][:, nh * 512:(nh + 1) * 512], in0=xt[s_][:, nh * 512:(nh + 1) * 512],
                                                                scalar=ALPHA, in1=psM[nh][:], op0=ALU.mult, op1=ALU.add,
                                                                accum_out=sm[s_][:, nh:nh + 1]),
                  r=[Bxt[s_], BpsM, Bsm[s_]], w=[Bx1p[s_], Bsm[s_]])
            layer_norm(V, A, G, x1p[s_], Bx1p[s_], sm[s_], g_bc, b_bc, x1[s_], Bx1[s_], tmp, Bt, sm[s_], Bsm[s_], Bc)
            LD(x1_d[r0:r0 + 128, :], x1[s_][:], r=[Bx1[s_]])
            A(lambda e, s_=s_: e.copy(out=x1b[s_][:], in_=x1[s_][:]), r=[Bx1[s_]], w=[Bx1b[s_]])
            for k in range(8):
                M(lambda e, s_=s_, k=k: e.transpose(out=psF[k // 4][:, (k % 4) * 128:(k % 4 + 1) * 128], in_=x1[s_][:, k * 128:(k + 1) * 128],
                                                     identity=C["ident_f"][:]), r=[Bx1[s_]], w=[BpsF])
            V(lambda e, s_=s_: e.tensor_copy(out=x1T[s_][:, 0:4, :], in_=psF[0][:].rearrange("p (k t) -> p k t", k=4)), r=[BpsF], w=[Bx1T[s_]])
            V(lambda e, s_=s_: e.tensor_copy(out=x1T[s_][:, 4:8, :], in_=psF[1][:].rearrange("p (k t) -> p k t", k=4)), r=[BpsF], w=[Bx1T[s_]])
            for k in range(8):
                M(lambda e, s_=s_, k=k: e.matmul(psR[:, 0:32], lhsT=x1T[s_][:, k, :], rhs=rw[:, k, :], start=(k == 0), stop=(k == 7)),
                  r=[Bx1T[s_], Bc], w=[BpsR])
            R_ = rt[s_]; Br = Brt[s_]
            lg, Am, ex, gg, dv, junk = (R_[:, i, :] for i in range(6))
            m_ = m8[s_]
            V(lambda e: e.tensor_tensor(out=lg, in0=psR[:, 0:32], in1=rb[:], op=ALU.add), r=[BpsR, Bc], w=[Br])
            V(lambda e: e.max(out=m_[:, 0:8], in_=lg), r=[Br], w=[Br])
            V(lambda e: e.tensor_scalar(out=Am, in0=lg, scalar1=m_[:, 3:4], scalar2=None, op0=ALU.is_ge), r=[Br], w=[Br])
            V(lambda e: e.tensor_scalar(out=m_[:, 8:9], in0=m_[:, 0:1], scalar1=-1.0, scalar2=None, op0=ALU.mult), r=[Br], w=[Br])
            A(lambda e: e.activation(out=ex, in_=lg, func=AF.Exp, bias=m_[:, 8:9], scale=1.0), r=[Br], w=[Br])
            V(lambda e: e.tensor_tensor(out=ex, in0=ex, in1=Am, op=ALU.mult), r=[Br], w=[Br])
            V(lambda e: e.tensor_reduce(out=m_[:, 9:10], in_=ex, axis=AX.X, op=ALU.add), r=[Br], w=[Br])
            V(lambda e: e.reciprocal(out=m_[:, 10:11], in_=m_[:, 9:10]), r=[Br], w=[Br])
            V(lambda e: e.tensor_scalar(out=gg, in0=ex, scalar1=m_[:, 10:11], scalar2=None, op0=ALU.mult), r=[Br], w=[Br])
            V(lambda e, s_=s_: e.tensor_copy(out=rtb[s_][:], in_=Am), r=[Br], w=[Br])
            M(lambda e, s_=s_: e.matmul(psR[:, 64:96], lhsT=C["ltri"][:], rhs=rtb[s_][:], start=True, stop=False), r=[Br, B_acum], w=[BpsR])
            M(lambda e: e.matmul(psR[:, 64:96], lhsT=C["ones_bf"][:], rhs=acum[:], start=False, stop=True), r=[Br, B_acum], w=[BpsR])
            G(lambda e, s_=s_: e.tensor_tensor(out=acum[:], in0=acum[:], in1=rtb[s_][:], op=ALU.add), r=[Br, B_acum], w=[B_acum])
            V(lambda e: e.tensor_scalar(out=dv, in0=psR[:, 64:96], scalar1=float(CAP), scalar2=None, op0=ALU.is_lt), r=[BpsR, Br], w=[Br])
            V(lambda e: e.tensor_tensor(out=dv, in0=dv, in1=Am, op=ALU.mult), r=[Br], w=[Br])
            V(lambda e: e.tensor_tensor(out=junk, in0=psR[:, 64:96], in1=C["iota_e"][:], op=ALU.add), r=[BpsR, Br], w=[Br])
            V(lambda e: e.tensor_tensor(out=dv, in0=dv, in1=junk, op=ALU.mult), r=[Br], w=[Br])
            V(lambda e: e.max(out=m_[:, 0:8], in_=dv), r=[Br], w=[Br])
            V(lambda e, s_=s_: e.tensor_copy(out=dsti[s_][:], in_=m_[:, 0:4]), r=[Br, Bmeta[s_]], w=[Bmeta[s_]])
            V(lambda e, s_=s_: e.memset(wk[s_][:], 0.0), r=[Bmeta[s_]], w=[Bmeta[s_]])
            for k in range(4):
                V(lambda e, s_=s_, k=k: e.scalar_tensor_tensor(out=junk, in0=dv, scalar=m_[:, k:k + 1], in1=gg, op0=ALU.is_equal, op1=ALU.mult,
                                                              accum_out=wk[s_][:, k:k + 1]), r=[Br, Bmeta[s_]], w=[Br, Bmeta[s_]])
            mi = meta[s_][:].bitcast(I32)
            for k in range(4):
                V(lambda e, s_=s_, k=k, gt=gt: e.tensor_copy(out=mi[:, k, 0:1], in_=C["tokid"][:, gt:gt + 1]), r=[Bmeta[s_]], w=[Bmeta[s_]])
                V(lambda e, s_=s_, k=k: e.tensor_copy(out=meta[s_][:, k, 1:2], in_=wk[s_][:, k:k + 1]), r=[Bmeta[s_]], w=[Bmeta[s_]])
            for k in range(4):
                T.dma("pool", lambda e, s_=s_, k=k: e.indirect_dma_start(
                    out=xg_d, out_offset=bass.IndirectOffsetOnAxis(ap=dsti[s_][:, k:k + 1], axis=0), in_=x1b[s_][:], in_offset=None),
                    reads=[Bx1b[s_], Bmeta[s_]])
                T.dma("pool", lambda e, s_=s_, k=k: e.indirect_dma_start(
                    out=meta_dl, out_offset=bass.IndirectOffsetOnAxis(ap=dsti[s_][:, k:k + 1], axis=0), in_=meta[s_][:, k, :], in_offset=None),
                    reads=[Bmeta[s_]])


def phase_D(nc, T, C, l, wgu_d, bgu_d, wdn_d, bdn_d, xg_d, meta_dl, yacc_dl, sbt, pst, V, A, G, M, LD):
    NST = CAP // 128
    HN = CAP // 2
    with contextlib.ExitStack() as st:
        wgu = [sbt(st, "wgu%d" % i, [128, 8, 2 * D], BF16) for i in range(2)]
        wdn = [sbt(st, "wdn%d" % i, [128, 8, D], BF16) for i in range(2)]
        stg = [sbt(st, "stg%d" % i, [128, 2 * D]) for i in range(2)]
        bgu = [sbt(st, "bgu%d" % i, [128, 16]) for i in range(2)]
        bdn = [sbt(st, "bdn%d" % i, [128, D]) for i in range(2)]
        meta = [sbt(st, "metaD%d" % i, [128, NST, 2]) for i in range(2)]
        xgr = [sbt(st, "xgr%d" % i, [128, D], BF16) for i in range(2)]
        xgT = sbt(st, "xgT", [128, 8, CAP], BF16)
        actT = sbt(st, "actT", [128, 8, CAP], BF16)
        ew = [sbt(st, "ew%d" % i, [128, HN]) for i in range(5)]
        yt = [sbt(st, "yt%d" % i, [128, D]) for i in range(2)]
        psT = pst(st, "psTd", [128, D], BF16)
        psG = pst(st, "psG", [128, 512]); psU = pst(st, "psU", [128, 512])
        psY = [pst(st, "psY%d" % i, [128, 512]) for i in range(2)]
        Bwgu = [Buf(), Buf()]; Bwdn = [Buf(), Buf()]; Bstg = [Buf(), Buf()]; Bsm = [Buf(), Buf()]
        Bxgr = [Buf(), Buf()]; BxgT = Buf(); BactT = Buf(); Bew = [Buf() for _ in range(5)]; Byt = [Buf(), Buf()]
        BpsT = Buf(); BpsG = Buf(); BpsU = Buf(); BpsY = Buf(); Byacc = Buf()
        sidx = [0]

        def load_weights(e_):
            p = e_ % 2
            wv = wgu_d[l, e_].rearrange("(k p) n -> p k n", p=128)
            for k in range(8):
                s_ = sidx[0] % 2
                sidx[0] += 1
                LD(stg[s_][:], wv[:, k, :], w=[Bstg[s_]])
                G(lambda e, s_=s_, k=k, p=p: e.tensor_copy(out=wgu[p][:, k, :].rearrange("p (two f) -> p two f", two=2),
                                                          in_=stg[s_][:].rearrange("p (f two) -> p two f", two=2)),
                  r=[Bstg[s_]], w=[Bwgu[p]])
            wv2 = wdn_d[l, e_].rearrange("(k p) n -> p k n", p=128)
            for k2 in range(4):
                s_ = sidx[0] % 2
                sidx[0] += 1
                LD(stg[s_][:].rearrange("p (k n) -> p k n", k=2), wv2[:, 2 * k2:2 * k2 + 2, :], w=[Bstg[s_]])
                G(lambda e, s_=s_, k2=k2, p=p: e.tensor_copy(out=wdn[p][:, 2 * k2:2 * k2 + 2, :], in_=stg[s_][:].rearrange("p (k n) -> p k n", k=2)),
                  r=[Bstg[s_]], w=[Bwdn[p]])
            LD(bgu[p][:], bgu_d[l, e_], w=[Bsm[p]])
            LD(bdn[p][:], bdn_d[l, e_:e_ + 1, :].partition_broadcast(128), w=[Bsm[p]])
            s0 = 1 + e_ * CAP
            LD(meta[p][:], meta_dl[s0:s0 + CAP, :].rearrange("(s p) c -> p s c", p=128), w=[Bsm[p]])

        load_weights(0)
        for e_ in range(NEXP):
            p = e_ % 2
            if e_ + 1 < NEXP:
                load_weights(e_ + 1)
            s0 = 1 + e_ * CAP
            for stl in range(NST):
                s_ = stl % 2
                LD(xgr[s_][:], xg_d[s0 + stl * 128:s0 + (stl + 1) * 128, :], w=[Bxgr[s_]])
                for k in range(8):
                    M(lambda e, s_=s_, k=k: e.transpose(out=psT[:, k * 128:(k + 1) * 128], in_=xgr[s_][:, k * 128:(k + 1) * 128],
                                                         identity=C["ident_bf"][:]), r=[Bxgr[s_]], w=[BpsT])
                A(lambda e, stl=stl: e.copy(out=xgT[:, :, stl * 128:(stl + 1) * 128], in_=psT[:].rearrange("p (k t) -> p k t", k=8)),
                  r=[BpsT], w=[BxgT])
            for j in range(8):
                for hn in range(2):
                    cs = slice(hn * HN, (hn + 1) * HN)
                    for k in range(8):
                        M(lambda e, k=k, j=j, cs=cs, p=p: e.matmul(psG[:, 0:HN], lhsT=wgu[p][:, k, j * 128:(j + 1) * 128], rhs=xgT[:, k, cs],
                                                                  start=(k == 0), stop=(k == 7)), r=[Bwgu[p], BxgT], w=[BpsG])
                    for k in range(8):
                        M(lambda e, k=k, j=j, cs=cs, p=p: e.matmul(psU[:, 0:HN], lhsT=wgu[p][:, k, D + j * 128:D + (j + 1) * 128], rhs=xgT[:, k, cs],
                                                                  start=(k == 0), stop=(k == 7)), r=[Bwgu[p], BxgT], w=[BpsU])
                    g1, sg, u1, u2, glu = ew
                    V(lambda e, j=j, p=p: e.tensor_scalar(out=g1[:], in0=psG[:, 0:HN], scalar1=bgu[p][:, j:j + 1], scalar2=7.0, op0=ALU.add, op1=ALU.min),
                      r=[BpsG, Bsm[p]], w=[Bew[0]])
                    A(lambda e: e.activation(out=sg[:], in_=g1[:], func=AF.Sigmoid, scale=1.702), r=[Bew[0]], w=[Bew[1]])
                    V(lambda e, j=j, p=p: e.tensor_scalar(out=u1[:], in0=psU[:, 0:HN], scalar1=bgu[p][:, 8 + j:9 + j], scalar2=7.0, op0=ALU.add, op1=ALU.min),
                      r=[BpsU, Bsm[p]], w=[Bew[2]])
                    G(lambda e: e.tensor_scalar(out=u2[:], in0=u1[:], scalar1=-7.0, scalar2=1.0, op0=ALU.max, op1=ALU.add), r=[Bew[2]], w=[Bew[3]])
                    G(lambda e: e.tensor_tensor(out=glu[:], in0=g1[:], in1=sg[:], op=ALU.mult), r=[Bew[0], Bew[1]], w=[Bew[4]])
                    V(lambda e, j=j, cs=cs: e.tensor_tensor(out=actT[:, j, cs], in0=u2[:], in1=glu[:], op=ALU.mult), r=[Bew[3], Bew[4]], w=[BactT])
            mi = meta[p][:].bitcast(I32)
            for stl in range(NST):
                s_ = stl % 2
                for nh in range(2):
                    for k in range(8):
                        M(lambda e, k=k, nh=nh, stl=stl, p=p: e.matmul(psY[nh][:], lhsT=actT[:, k, stl * 128:(stl + 1) * 128],
                                                                      rhs=wdn[p][:, k, nh * 512:(nh + 1) * 512], start=(k == 0), stop=(k == 7)),
                          r=[BactT, Bwdn[p]], w=[BpsY])
                for nh in range(2):
                    V(lambda e, nh=nh, s_=s_, p=p: e.tensor_tensor(out=yt[s_][:, nh * 512:(nh + 1) * 512], in0=psY[nh][:],
                                                                  in1=bdn[p][:, nh * 512:(nh + 1) * 512], op=ALU.add), r=[BpsY, Bsm[p]], w=[Byt[s_]])
                A(lambda e, s_=s_, stl=stl, p=p: e.activation(out=yt[s_][:], in_=yt[s_][:], func=AF.Copy, scale=meta[p][:, stl, 1:2]),
                  r=[Byt[s_], Bsm[p]], w=[Byt[s_]])
                T.dma("pool", lambda e, s_=s_, stl=stl: e.indirect_dma_start(
                    out=yacc_dl, out_offset=bass.IndirectOffsetOnAxis(ap=mi[:, stl, 0:1], axis=0), in_=yt[s_][:], in_offset=None,
                    compute_op=ALU.add), reads=[Byt[s_], Bsm[p]], writes=[Byacc])


def phase_E(nc, T, C, l, NGT, x1_d, yacc_dl, ln2g_d, ln2b_d, x_dst, sbt, pst, V, A, G, M, LD):
    with contextlib.ExitStack() as st:
        g_bc = sbt(st, "g2_bc", [128, D]); b_bc = sbt(st, "b2_bc", [128, D])
        xt = [sbt(st, "xe%d" % i, [128, D]) for i in range(2)]
        yt = [sbt(st, "ye%d" % i, [128, D]) for i in range(2)]
        xp = [sbt(st, "xpe%d" % i, [128, D]) for i in range(2)]
        xo = [sbt(st, "xoe%d" % i, [128, D]) for i in range(2)]
        tmp = sbt(st, "tmpE", [128, D])
        sm = [sbt(st, "smE%d" % i, [128, 8]) for i in range(2)]
        Bc = Buf(); Bxt = [Buf(), Buf()]; Byt = [Buf(), Buf()]; Bxp = [Buf(), Buf()]; Bxo = [Buf(), Buf()]; Bt = Buf(); Bsm = [Buf(), Buf()]
        LD(g_bc[:], ln2g_d[l].partition_broadcast(128), w=[Bc])
        LD(b_bc[:], ln2b_d[l].partition_broadcast(128), w=[Bc])
        for gt in range(NGT):
            s_ = gt % 2
            r0 = gt * 128
            LD(xt[s_][:], x1_d[r0:r0 + 128, :], w=[Bxt[s_]])
            LD(yt[s_][:], yacc_dl[r0:r0 + 128, :], w=[Byt[s_]])
            V(lambda e, s_=s_: e.memset(sm[s_][:, 0:2], 0.0), w=[Bsm[s_]])
            V(lambda e, s_=s_: e.scalar_tensor_tensor(out=xp[s_][:], in0=xt[s_][:], scalar=ALPHA, in1=yt[s_][:], op0=ALU.mult, op1=ALU.add,
                                                     accum_out=sm[s_][:, 0:1]), r=[Bxt[s_], Byt[s_], Bsm[s_]], w=[Bxp[s_], Bsm[s_]])
            layer_norm(V, A, G, xp[s_], Bxp[s_], sm[s_], g_bc, b_bc, xo[s_], Bxo[s_], tmp, Bt, sm[s_], Bsm[s_], Bc)
            LD(x_dst[r0:r0 + 128, :], xo[s_][:], r=[Bxo[s_]])


def prep_inputs(inputs, core, nseq=2):
    perm = _col_perm()
    f = lambda a: np.ascontiguousarray(np.asarray(a, dtype=np.float32))
    m = {}
    x = np.asarray(inputs["x"])
    m["x"] = f(x[core * 2:core * 2 + nseq].reshape(nseq * S, D))
    w_in = np.asarray(inputs["w_in"])[:, :, perm]
    m["w_in"] = f(w_in)
    b_in = np.asarray(inputs["b_in"])[:, perm]
    m["bT"] = f(b_in[:, :NTC * 128].reshape(2, NTC, 128).transpose(0, 2, 1))
    m["bV"] = f(b_in[:, NTC * 128:].reshape(2, 1, NTOKC))
    m["sinks"] = f(np.asarray(inputs["sinks"]).reshape(2, 1, 6))
    m["peT"] = f(np.asarray(inputs["cmp_pe"]).transpose(0, 3, 1, 2))
    m["cmp_w1"] = f(inputs["cmp_w1"])
    m["cmp_w2"] = f(inputs["cmp_w2"])
    m["w_out"] = f(inputs["w_out"])
    for k in ("ln1_g", "ln1_b", "ln2_g", "ln2_b"):
        m[k] = f(np.asarray(inputs[k]).reshape(2, 1, D))
    m["router_w"] = f(inputs["router_w"])
    m["router_b"] = f(np.asarray(inputs["router_b"]).reshape(2, 1, NEXP))
    m["w_gate_up"] = f(inputs["w_gate_up"])
    bgu = np.asarray(inputs["b_gate_up"]).reshape(2, NEXP, 8, 128, 2)
    m["b_gu"] = f(bgu.transpose(0, 1, 3, 4, 2).reshape(2, NEXP, 128, 16))
    m["w_down"] = f(inputs["w_down"])
    m["b_down"] = f(inputs["b_down"])
    return m


_CACHE = {}


def kernel(**inputs):
    if "nc" not in _CACHE:
        _CACHE["nc"] = build()
    nc, consts = _CACHE["nc"]
    shared = None
    in_maps = []
    for core in range(8):
        m = prep_inputs(inputs, core) if shared is None else dict(shared)
        if shared is None:
            shared = {k: v for k, v in m.items() if k != "x"}
            for k, v in consts.items():
                shared["c_" + k] = v
            m = dict(shared, x=m["x"])
        else:
            x = np.asarray(inputs["x"])
            m["x"] = np.ascontiguousarray(x[core * 2:core * 2 + 2].reshape(2 * S, D), dtype=np.float32)
        in_maps.append(m)
    res = run_bass_kernel_spmd(nc, in_maps, core_ids=list(range(8)))
    out = np.concatenate([np.asarray(r["out"]).reshape(2, S, D) for r in res.results], axis=0)
    return out.astype(np.float32)
```

```python
import contextlib
import numpy as np
import ml_dtypes
import concourse.bass as bass
import concourse.mybir as mybir
from concourse.bass_utils import run_bass_kernel_spmd

F32 = mybir.dt.float32
BF16 = mybir.dt.bfloat16
I32 = mybir.dt.int32
ALU = mybir.AluOpType
AF = mybir.ActivationFunctionType
AX = mybir.AxisListType

S = 2048
D = 1024
NT = 16
NEXP = 32
CAP = 768
NSLOT = 1 + NEXP * CAP
NCOLS = 2582
NTC = 15
NTOKC = 662
ALPHA = 4.0 ** 0.25
LN_EPS = 1e-5
NEG = -30000.0
SCALE = 0.125


class Buf:
    __slots__ = ("name", "w", "r")

    def __init__(self, name=""):
        self.name = name
        self.w = None
        self.r = {}


class _Eng:
    def __init__(self, name, eng, sem):
        self.name = name
        self.eng = eng
        self.sem = sem
        self.count = 0
        self.seen = {}


class Tracker:
    def __init__(self, nc, stack, n_dma_sems=24):
        self.nc = nc
        self.e = {}
        for name, eng in (("pe", nc.tensor), ("act", nc.scalar), ("dve", nc.vector),
                          ("pool", nc.gpsimd), ("sp", nc.sync)):
            sem = stack.enter_context(nc.semaphore("prog_" + name))
            self.e[name] = _Eng(name, eng, sem)
        self.dsems = []
        for i in range(n_dma_sems):
            sem = stack.enter_context(nc.semaphore("dma_%d" % i))
            self.dsems.append([sem, 0])
        self.dnext = 0

    def _wait(self, E, tok):
        sem, val = tok
        k = id(sem)
        if E.seen.get(k, 0) >= val:
            return
        E.eng.wait_ge(sem, val)
        E.seen[k] = val

    def _deps(self, E, reads, writes, skip_self):
        toks = []
        for b in reads:
            if b.w is not None:
                toks.append(b.w)
        for b in writes:
            if b.w is not None:
                toks.append(b.w)
            toks.extend(b.r.values())
        for t in toks:
            if skip_self and t[0] is E.sem:
                continue
            self._wait(E, t)

    def _commit(self, tok, reads, writes):
        for b in writes:
            b.w = tok
            b.r = {}
        for b in reads:
            k = id(tok[0])
            if k not in b.r or b.r[k][1] < tok[1]:
                b.r[k] = tok

    def op(self, ename, fn, reads=(), writes=()):
        E = self.e[ename]
        self._deps(E, reads, writes, skip_self=(ename == "pe"))
        ins = fn(E.eng)
        E.count += 1
        ins.then_inc(E.sem, 1)
        tok = (E.sem, E.count)
        self._commit(tok, reads, writes)
        return tok

    def dma(self, qname, fn, reads=(), writes=()):
        E = self.e[qname]
        self._deps(E, reads, writes, skip_self=False)
        slot = self.dsems[self.dnext]
        self.dnext = (self.dnext + 1) % len(self.dsems)
        if slot[1] > 0:
            self._wait(E, (slot[0], slot[1]))
        ins = fn(E.eng)
        slot[1] += 16
        ins.then_inc(slot[0], 16)
        tok = (slot[0], slot[1])
        self._commit(tok, reads, writes)
        return tok

    def barrier(self):
        sp = self.e["sp"]
        for name, E in self.e.items():
            if E is not sp and E.count > 0:
                self._wait(sp, (E.sem, E.count))
        for slot in self.dsems:
            if slot[1] > 0:
                self._wait(sp, (slot[0], slot[1]))
        ins = sp.eng.nop()
        sp.count += 1
        ins.then_inc(sp.sem, 1)
        tok = (sp.sem, sp.count)
        for name, E in self.e.items():
            if E is not sp:
                self._wait(E, tok)
        for name, E in self.e.items():
            for name2, E2 in self.e.items():
                E.seen[id(E2.sem)] = E2.count
            for slot in self.dsems:
                E.seen[id(slot[0])] = slot[1]


def _slopes():
    n = 12
    s = 2.0 ** (-8.0 * np.arange(1, n + 1) / n)
    return s[0::2].astype(np.float64), s[1::2].astype(np.float64)


def _col_perm():
    FOXW, SWAQ, KVW = 256, 384, 128
    off = {}
    names = ["fq", "fk", "fv", "ff", "sq", "sk", "sv", "nq", "nkc", "nvc", "nks", "nvs", "nkw", "nvw", "ng"]
    sizes = [256, 256, 256, 4, 384, 128, 128, 384, 128, 128, 128, 128, 128, 128, 18]
    o = 0
    for n_, s_ in zip(names, sizes):
        off[n_] = o
        o += s_
    cols = []
    r = lambda name, a, b: list(range(off[name] + a, off[name] + b))
    cols += r("fq", 0, 256)
    cols += r("fk", 0, 256)
    for g in range(3):
        cols += r("sq", (0 * 3 + g) * 64, (0 * 3 + g) * 64 + 64) + r("sq", (3 + g) * 64, (3 + g) * 64 + 64)
    cols += r("sk", 0, 128)
    for g in range(3):
        cols += r("nq", (0 * 3 + g) * 64, (0 * 3 + g) * 64 + 64) + r("nq", (3 + g) * 64, (3 + g) * 64 + 64)
    cols += r("nkc", 0, 128)
    cols += r("nvc", 0, 128)
    cols += r("nks", 0, 128)
    cols += r("nkw", 0, 128)
    assert len(cols) == NTC * 128
    cols += r("fv", 0, 256) + r("sv", 0, 128) + r("nvs", 0, 128)
    cols += r("nvw", 0, 128) + r("ff", 0, 4) + r("ng", 0, 18)
    assert len(cols) == NCOLS and len(set(cols)) == NCOLS
    return np.array(cols)


def make_consts():
    c = {}
    bf = ml_dtypes.bfloat16
    swa, nsa = _slopes()
    j = np.arange(128)
    c["ident_bf"] = np.eye(128, dtype=np.float32).astype(bf)
    c["ident_f"] = np.eye(128, dtype=np.float32)
    mC = np.where(j[:, None] > j[None, :], NEG, 0.0).astype(np.float32)
    mW = np.where(j[:, None] <= j[None, :], NEG, 0.0).astype(np.float32)
    c["maskC"] = np.tile(mC, (1, 3)).astype(bf)
    c["maskW"] = np.tile(mW, (1, 3)).astype(bf)
    def alibi(sl, nd):
        t = np.zeros((128, 6, nd), np.float32)
        for h in range(6):
            for d in range(nd):
                t[:, h, d] = sl[h] * (j - 128.0 * d)
        return t
    c["al_swa"] = alibi(swa, 2)
    c["al_win"] = alibi(nsa, 5)
    c["al_sel"] = alibi(nsa, 16)
    c["al_i"] = (swa[None, :] * j[:, None]).astype(np.float32)
    cb = np.zeros((128, 6, 16), np.float32)
    cc = np.arange(128)
    for h in range(6):
        for qb in range(16):
            cb[:, h, qb] = nsa[h] * (16.0 * cc + 31 - 128.0 * qb)
    c["cbias"] = cb
    Z = np.zeros((8, 256), np.float32)
    for r in range(8):
        Z[r, 120 + r] = 1.0
    c["Zc"] = Z.astype(bf)
    R = np.zeros((8, 128), np.float32)
    for r in range(8):
        R[r, :] = np.where(16 * r + 15 > j, NEG, 0.0)
    c["Rc"] = np.tile(R, (1, 3)).astype(bf)
    nf = np.zeros((128, 16, 32), np.float32)
    ad = np.zeros((128, 16, 32), np.float32)
    jb = np.arange(32)
    for qb in range(16):
        t = qb * 128 + j
        cur = t // 64
        forced = (jb[None, :] == 0) | (jb[None, :] == cur[:, None]) | (jb[None, :] == cur[:, None] - 1)
        future = jb[None, :] > cur[:, None]
        nf[:, qb, :] = np.where(future | forced, 0.0, 1.0)
        ad[:, qb, :] = np.where(future, -1.0, np.where(forced, 1.0e4, 0.0))
    c["sel_nf"] = nf
    c["sel_ad"] = ad
    ex = np.zeros((32, 16, 128), np.float32)
    for kb in range(16):
        ex[2 * kb, kb, 0:64] = 1.0
        ex[2 * kb + 1, kb, 64:128] = 1.0
    c["expand"] = ex.astype(bf)
    ncmp = 127
    cs = np.arange(ncmp) * 16
    ss = np.arange(32) * 64
    ov = np.clip(np.minimum(cs[:, None] + 32, ss[None, :] + 64) - np.maximum(cs[:, None], ss[None, :]), 0, None) / 32.0
    ovp = np.zeros((128, 32), np.float32)
    ovp[:127] = ov
    c["ov"] = ovp.astype(bf)
    c["ltri"] = (j[:, None] < j[None, :]).astype(np.float32).astype(bf)
    c["ones_bf"] = np.ones((128, 128), np.float32).astype(bf)
    c["utri_f"] = (j[:, None] <= j[None, :]).astype(np.float32)
    c["ones_f"] = np.ones((128, 128), np.float32)
    c["iota_e"] = np.tile((1.0 + np.arange(32) * CAP)[None, :], (128, 1)).astype(np.float32)
    c["tokid"] = (np.arange(32)[None, :] * 128 + j[:, None]).astype(np.int32)
    meta = np.zeros((NSLOT, 2), np.float32)
    meta[:, 0] = np.array([4096], np.int32).view(np.float32)[0]
    c["meta_init"] = meta
    return c


CONST_DT = {"ident_bf": BF16, "maskC": BF16, "maskW": BF16, "Zc": BF16, "Rc": BF16, "expand": BF16,
            "ov": BF16, "ltri": BF16, "ones_bf": BF16, "tokid": I32}


def build(nseq=2, nlayers=2, dbg=()):
    nc = bass.Bass("TRN2", target_bir_lowering=False)
    NTOK = nseq * S
    NGT = nseq * NT
    L = 2

    def din(name, shape, dt=F32):
        return nc.dram_tensor(name, list(shape), dt, kind="ExternalInput").ap()

    def dscr(name, shape, dt=F32):
        kind = "ExternalOutput" if name in dbg else "Internal"
        return nc.dram_tensor(name, list(shape), dt, kind=kind).ap()

    x_d = din("x", [NTOK, D])
    w_in_d = din("w_in", [L, D, NCOLS])
    bT_d = din("bT", [L, 128, NTC])
    bV_d = din("bV", [L, 1, NTOKC])
    sinks_d = din("sinks", [L, 1, 6])
    peT_d = din("peT", [L, 64, 2, 32])
    w1_d = din("cmp_w1", [L, 2, 2048, 64])
    w2_d = din("cmp_w2", [L, 2, 64, 64])
    wout_d = din("w_out", [L, D, D])
    ln1g_d = din("ln1_g", [L, 1, D]); ln1b_d = din("ln1_b", [L, 1, D])
    ln2g_d = din("ln2_g", [L, 1, D]); ln2b_d = din("ln2_b", [L, 1, D])
    rw_d = din("router_w", [L, D, NEXP]); rb_d = din("router_b", [L, 1, NEXP])
    wgu_d = din("w_gate_up", [L, NEXP, D, 2 * D])
    bgu_d = din("b_gu", [L, NEXP, 128, 16])
    wdn_d = din("w_down", [L, NEXP, D, D])
    bdn_d = din("b_down", [L, NEXP, D])
    consts_np = make_consts()
    cd = {k: din("c_" + k, v.shape, CONST_DT.get(k, F32)) for k, v in consts_np.items()}
    out_d = nc.dram_tensor("out", [NTOK, D], F32, kind="ExternalOutput").ap()

    attn_d = dscr("attn_s", [NTOK, D], BF16)
    x1_d = dscr("x1_s", [NTOK, D])
    xcur_d = dscr("xcur_s", [NTOK, D])
    yacc_d = [dscr("yacc_s%d" % l, [NTOK + 1, D]) for l in range(L)]
    xg_d = dscr("xg_s", [NSLOT, D], BF16)
    meta_d = [dscr("meta_s%d" % l, [NSLOT, 2]) for l in range(L)]

    with contextlib.ExitStack() as gst:
        T = Tracker(nc, gst)

        uid = [0]

        def sbt(st, name, shape, dt=F32):
            uid[0] += 1
            return st.enter_context(nc.sbuf_tensor("s%d_%s" % (uid[0], name), list(shape), dt))

        def pst(st, name, shape, dt=F32):
            uid[0] += 1
            return st.enter_context(nc.psum_tensor("p%d_%s" % (uid[0], name), list(shape), dt))

        def V(fn, r=(), w=()):
            return T.op("dve", fn, r, w)

        def A(fn, r=(), w=()):
            return T.op("act", fn, r, w)

        def G(fn, r=(), w=()):
            return T.op("pool", fn, r, w)

        def M(fn, r=(), w=()):
            return T.op("pe", fn, r, w)

        def LD(out, in_, r=(), w=(), q="sp"):
            return T.dma(q, lambda e: e.dma_start(out=out, in_=in_), r, w)

        C = {}
        for k, v in consts_np.items():
            if k == "meta_init":
                continue
            C[k] = sbt(gst, "k_" + k, v.shape, CONST_DT.get(k, F32))
            LD(C[k][:], cd[k])
        acum = sbt(gst, "acum", [128, NEXP], BF16)
        B_acum = Buf()
        zero_t = sbt(gst, "zero_t", [128, D])
        V(lambda e: e.memset(zero_t[:], 0.0))
        T.barrier()

        for l in range(nlayers):
            x_src = x_d if l == 0 else xcur_d
            x_dst = out_d if l == nlayers - 1 else xcur_d
            G(lambda e: e.memset(acum[:], 0.0), w=[B_acum])
            for r0 in range(0, NTOK + 1, 128):
                rows = min(128, NTOK + 1 - r0)
                LD(yacc_d[l][r0:r0 + rows, :], zero_t[0:rows, :])
            T.dma("pool", lambda e: e.dma_start(out=meta_d[l], in_=cd["meta_init"]))
            T.barrier()

            for b in range(nseq):
                with contextlib.ExitStack() as sst:
                    hT = sbt(sst, "hT", [128, NTC, S], BF16)
                    vaug = sbt(sst, "vaug", [128, NT, 10, 65], BF16)
                    fg = sbt(sst, "fg", [128, NT, 22])
                    phase_A(nc, T, C, l, b, x_src, w_in_d, bT_d, bV_d, hT, vaug, fg, sbt, pst, V, A, G, M, LD)
                    T.barrier()
                    phase_B(nc, T, C, l, b, hT, vaug, fg, sinks_d, peT_d, w1_d, w2_d, attn_d,
                            sbt, pst, V, A, G, M, LD)
                    T.barrier()
                phase_C(nc, T, C, l, b, x_src, attn_d, wout_d, ln1g_d, ln1b_d, rw_d, rb_d, x1_d, xg_d, meta_d[l],
                        acum, B_acum, sbt, pst, V, A, G, M, LD)
                T.barrier()
            phase_D(nc, T, C, l, wgu_d, bgu_d, wdn_d, bdn_d, xg_d, meta_d[l], yacc_d[l], sbt, pst, V, A, G, M, LD)
            T.barrier()
            phase_E(nc, T, C, l, NGT, x1_d, yacc_d[l], ln2g_d, ln2b_d, x_dst, sbt, pst, V, A, G, M, LD)
            T.barrier()
    return nc, consts_np


def phase_A(nc, T, C, l, b, x_src, w_in_d, bT_d, bV_d, hT, vaug, fg, sbt, pst, V, A, G, M, LD):
    with contextlib.ExitStack() as st:
        wbf = sbt(st, "wbf", [128, 8, NCOLS], BF16)
        xT = sbt(st, "xT", [128, 8, S], BF16)
        bT = sbt(st, "bT", [128, NTC])
        bV = sbt(st, "bV", [128, NTOKC])
        xbf = [sbt(st, "xbf%d" % i, [128, D], BF16) for i in range(4)]
        psT = [pst(st, "psTa%d" % i, [128, D], BF16) for i in range(2)]
        psA = [pst(st, "psA%d" % i, [128, 512]) for i in range(2)]
        psV0 = pst(st, "psV0", [128, 512])
        psV1 = pst(st, "psV1", [128, 512])
        B_wbf = Buf(); B_xbf = [Buf() for _ in range(4)]
        B_psT = [Buf(), Buf()]; B_xT = Buf(); B_psA = [Buf(), Buf()]; B_bias = Buf()
        B_pV = Buf(); B_h = Buf()
        LD(bT[:], bT_d[l], w=[B_bias])
        LD(bV[:], bV_d[l].partition_broadcast(128), w=[B_bias])
        G(lambda e: e.memset(vaug[:, :, :, 64:65], 1.0), w=[B_h])
        wv = w_in_d[l].rearrange("(k p) n -> p k n", p=128)
        for k in range(8):
            LD(wbf[:, k, :], wv[:, k, :], w=[B_wbf], q="pool")
        for tt in range(NT):
            s_ = tt % 2
            x_ = tt % 4
            r0 = b * S + tt * 128
            LD(xbf[x_][:], x_src[r0:r0 + 128, :], w=[B_xbf[x_]], q="pool")
            for k in range(8):
                M(lambda e: e.transpose(out=psT[s_][:, k * 128:(k + 1) * 128], in_=xbf[x_][:, k * 128:(k + 1) * 128],
                                        identity=C["ident_bf"][:]), r=[B_xbf[x_]], w=[B_psT[s_]])
            if tt % 2 == 0:
                V(lambda e: e.tensor_copy(out=xT[:, :, tt * 128:(tt + 1) * 128], in_=psT[s_][:].rearrange("p (k t) -> p k t", k=8)),
                  r=[B_psT[s_]], w=[B_xT])
            else:
                A(lambda e: e.copy(out=xT[:, :, tt * 128:(tt + 1) * 128], in_=psT[s_][:].rearrange("p (k t) -> p k t", k=8)),
                  r=[B_psT[s_]], w=[B_xT])
        i = 0
        for c in range(NTC):
            for tq in range(4):
                s_ = i % 2
                for k in range(8):
                    M(lambda e, s_=s_, k=k, c=c, tq=tq: e.matmul(psA[s_][:], lhsT=wbf[:, k, c * 128:(c + 1) * 128],
                                                                rhs=xT[:, k, tq * 512:(tq + 1) * 512], start=(k == 0), stop=(k == 7)),
                      r=[B_wbf, B_xT], w=[B_psA[s_]])
                if i % 2 == 0:
                    A(lambda e, s_=s_, c=c, tq=tq: e.activation(out=hT[:, c, tq * 512:(tq + 1) * 512], in_=psA[s_][:], func=AF.Identity,
                                                               bias=bT[:, c:c + 1], scale=1.0), r=[B_psA[s_], B_bias], w=[B_h])
                else:
                    V(lambda e, s_=s_, c=c, tq=tq: e.tensor_scalar(out=hT[:, c, tq * 512:(tq + 1) * 512], in0=psA[s_][:],
                                                                  scalar1=bT[:, c:c + 1], scalar2=None, op0=ALU.add),
                      r=[B_psA[s_], B_bias], w=[B_h])
                i += 1
        for tt in range(NT):
            for k in range(8):
                M(lambda e, k=k, tt=tt: e.matmul(psV0[:], lhsT=xT[:, k, tt * 128:(tt + 1) * 128], rhs=wbf[:, k, 1920:2432],
                                                 start=(k == 0), stop=(k == 7)), r=[B_wbf, B_xT], w=[B_pV])
            for k in range(8):
                M(lambda e, k=k, tt=tt: e.matmul(psV1[:, 0:150], lhsT=xT[:, k, tt * 128:(tt + 1) * 128], rhs=wbf[:, k, 2432:2582],
                                                 start=(k == 0), stop=(k == 7)), r=[B_wbf, B_xT], w=[B_pV])
            V(lambda e, tt=tt: e.tensor_tensor(out=vaug[:, tt, 0:8, 0:64], in0=psV0[:].rearrange("p (h d) -> p h d", h=8),
                                               in1=bV[:, 0:512].rearrange("p (h d) -> p h d", h=8), op=ALU.add),
              r=[B_pV, B_bias], w=[B_h])
            V(lambda e, tt=tt: e.tensor_tensor(out=vaug[:, tt, 8:10, 0:64], in0=psV1[:, 0:128].rearrange("p (h d) -> p h d", h=2),
                                               in1=bV[:, 512:640].rearrange("p (h d) -> p h d", h=2), op=ALU.add),
              r=[B_pV, B_bias], w=[B_h])
            V(lambda e, tt=tt: e.tensor_tensor(out=fg[:, tt, :], in0=psV1[:, 128:150], in1=bV[:, 640:662], op=ALU.add),
              r=[B_pV, B_bias], w=[B_h])


def phase_B(nc, T, C, l, b, hT, vaug, fg, sinks_d, peT_d, w1_d, w2_d, attn_d, sbt, pst, V, A, G, M, LD):
    with contextlib.ExitStack() as st:
        w1st = sbt(st, "w1st", [128, 2, 32, 64])
        w1bf = sbt(st, "w1bf", [128, 2, 32, 64], BF16)
        w2st = sbt(st, "w2st", [64, 2, 64])
        w2bf = sbt(st, "w2bf", [64, 2, 64], BF16)
        peT = sbt(st, "peT", [64, 2, 32])
        peTb = sbt(st, "peTb", [64, 2, 32], BF16)
        cbc = sbt(st, "cbc", [64, 2])
        sinkb = sbt(st, "sinkb", [128, 6])
        sinkf = sbt(st, "sinkf", [128, 6])
        sgate = sbt(st, "sgate", [128, NT, 18])
        lpos = sbt(st, "lpos", [128, NT, 4])
        tot = sbt(st, "tot", [128, NT, 4])
        Lpre = sbt(st, "Lpre", [128, NT, 4])
        Lc = sbt(st, "Lc", [128, NT, 4])
        KcT = sbt(st, "KcT", [128, 128], BF16)
        VcA = sbt(st, "VcA", [128, 2, 97], BF16)
        gel = [sbt(st, "gel%d" % i, [64, 128]) for i in range(4)]
        Gt = sbt(st, "Gt", [64, 128], BF16)
        psS = [pst(st, "psS%d" % i, [128, 512]) for i in range(3)]
        acc = [pst(st, "acc%d" % i, [128, 512]) for i in range(3)]
        psO = pst(st, "psO", [128, 3, 128])
        psX = pst(st, "psX", [128, 512])
        B_psS = [Buf(), Buf(), Buf()]; B_acc = [Buf(), Buf(), Buf()]; B_psO = Buf(); B_psX = Buf()
        B0 = Buf()
        NE = 6
        Et = [sbt(st, "Et%d" % i, [128, 128], BF16) for i in range(NE)]
        B_Et = [Buf() for _ in range(NE)]
        ei = [0]
        fb = [sbt(st, "fb%d" % i, [128, NT]) for i in range(2)]
        B_fb = [Buf(), Buf()]
        attn_t = [sbt(st, "attn_t%d" % i, [128, D], BF16) for i in range(2)]
        B_at = [Buf(), Buf()]
        sm = [sbt(st, "sm%d" % i, [128, 16]) for i in range(4)]
        B_sm = [Buf() for _ in range(4)]
        smi = [0]
        onsa = [sbt(st, "onsa%d" % i, [128, 64]) for i in range(3)]
        B_on = [Buf(), Buf(), Buf()]
        pslc = sbt(st, "pslc", [128, 32]); score = sbt(st, "score", [128, 32]); m8 = sbt(st, "m8", [128, 8])
        nsel = sbt(st, "nsel", [128, 32]); nselT = sbt(st, "nselT", [32, 3, 128], BF16)
        rdc = sbt(st, "rdc", [128, 4]); gco = sbt(st, "gco", [128, 4])
        B_sel = Buf(); B_nselT = Buf(); B_rdc = Buf()

        w1v = w1_d[l].rearrange("w (l d) j -> d w l j", d=64)
        LD(w1st[0:64], w1v, w=[B0])
        LD(w1st[64:128], w1v, w=[B0])
        LD(w2st[:], w2_d[l].rearrange("w j k -> j w k"), w=[B0])
        LD(peT[:], peT_d[l], w=[B0])
        LD(sinkb[:], sinks_d[l].partition_broadcast(128), w=[B0])
        V(lambda e: e.tensor_copy(out=w1bf[:], in_=w1st[:]), r=[B0], w=[B0])
        V(lambda e: e.tensor_copy(out=w2bf[:], in_=w2st[:]), r=[B0], w=[B0])
        V(lambda e: e.tensor_copy(out=peTb[:], in_=peT[:]), r=[B0], w=[B0])
        V(lambda e: e.tensor_tensor(out=sinkb[:], in0=sinkb[:], in1=C["al_i"][:], op=ALU.add), r=[B0], w=[B0])
        A(lambda e: e.activation(out=sinkf[:], in_=sinkb[:], func=AF.Exp), r=[B0], w=[B0])
        A(lambda e: e.activation(out=sgate[:], in_=fg[:, :, 4:22], func=AF.Sigmoid), r=[B0], w=[B0])
        A(lambda e: e.activation(out=lpos[:], in_=fg[:, :, 0:4], func=AF.Exp, scale=-1.0), r=[B0], w=[B0])
        A(lambda e: e.activation(out=lpos[:], in_=lpos[:], func=AF.Ln, bias=1.0, scale=1.0), r=[B0], w=[B0])
        lp2 = lpos[:].rearrange("p a h -> p (a h)")
        M(lambda e: e.matmul(psX[:, 0:64], lhsT=C["utri_f"][:], rhs=lp2, start=True, stop=True), r=[B0], w=[B_psX])
        M(lambda e: e.matmul(psX[:, 64:128], lhsT=C["ones_f"][:], rhs=lp2, start=True, stop=True), r=[B0], w=[B_psX])
        V(lambda e: e.tensor_copy(out=tot[:].rearrange("p a h -> p (a h)"), in_=psX[:, 64:128]), r=[B_psX], w=[B0])
        V(lambda e: e.memset(Lpre[:, 0, :], 0.0), r=[B0], w=[B0])
        for tt in range(1, NT):
            V(lambda e, tt=tt: e.tensor_tensor(out=Lpre[:, tt, :], in0=Lpre[:, tt - 1, :], in1=tot[:, tt - 1, :], op=ALU.add), r=[B0], w=[B0])
        V(lambda e: e.tensor_tensor(out=Lc[:].rearrange("p a h -> p (a h)"), in0=psX[:, 0:64],
                                    in1=Lpre[:].rearrange("p a h -> p (a h)"), op=ALU.add), r=[B_psX, B0], w=[B0])
        for wh in range(2):
            for ll in range(32):
                M(lambda e, wh=wh, ll=ll: e.matmul(psX[0:64, 200 + wh:201 + wh], lhsT=w1bf[0:64, wh, ll, :], rhs=peTb[0:64, wh, ll:ll + 1],
                                                   start=(ll == 0), stop=(ll == 31)), r=[B0], w=[B_psX])
        V(lambda e: e.tensor_copy(out=cbc[:], in_=psX[0:64, 200:202]), r=[B_psX], w=[B0])
        V(lambda e: e.tensor_copy(out=VcA[:, 0, 65:97], in_=C["ov"][:]), r=[B0], w=[B0])
        V(lambda e: e.tensor_copy(out=VcA[:, 1, 65:97], in_=C["ov"][:]), r=[B0], w=[B0])
        V(lambda e: e.memset(VcA[:, :, 64:65], 1.0), r=[B0], w=[B0])
        for kvh in range(2):
            P = slice(kvh * 64, kvh * 64 + 64)
            for wh in range(2):
                for ll in range(32):
                    M(lambda e, wh=wh, ll=ll, P=P: e.matmul(psX[0:64, 0:127], lhsT=w1bf[P, wh, ll, :],
                                                          rhs=hT[P, 11 + wh, ll:ll + 2017:16], start=(ll == 0), stop=(ll == 31)),
                      r=[B0], w=[B_psX])
                u, x2, inner, sg = gel
                A(lambda e, wh=wh: e.activation(out=u[:, 0:127], in_=psX[0:64, 0:127], func=AF.Identity, bias=cbc[:, wh:wh + 1], scale=1.0),
                  r=[B_psX, B0], w=[B0])
                V(lambda e: e.tensor_tensor(out=x2[:, 0:127], in0=u[:, 0:127], in1=u[:, 0:127], op=ALU.mult), r=[B0], w=[B0])
                V(lambda e: e.tensor_scalar(out=x2[:, 0:127], in0=x2[:, 0:127], scalar1=0.044715, scalar2=1.0, op0=ALU.mult, op1=ALU.add), r=[B0], w=[B0])
                V(lambda e: e.tensor_tensor(out=inner[:, 0:127], in0=x2[:, 0:127], in1=u[:, 0:127], op=ALU.mult), r=[B0], w=[B0])
                A(lambda e: e.activation(out=sg[:, 0:127], in_=inner[:, 0:127], func=AF.Sigmoid, scale=1.5957691216057308), r=[B0], w=[B0])
                V(lambda e: e.tensor_tensor(out=Gt[:, 0:127], in0=u[:, 0:127], in1=sg[:, 0:127], op=ALU.mult), r=[B0], w=[B0])
                if wh == 0:
                    M(lambda e, P=P: e.matmul(psX[P, 256:383], lhsT=w2bf[0:64, 0, :], rhs=Gt[0:64, 0:127], start=True, stop=True),
                      r=[B0], w=[B_psX])
                    V(lambda e, P=P: e.tensor_copy(out=KcT[P, 0:127], in_=psX[P, 256:383]), r=[B_psX], w=[B0])
                else:
                    M(lambda e: e.matmul(psX[0:127, 384:448], lhsT=Gt[0:64, 0:127], rhs=w2bf[0:64, 1, :], start=True, stop=True),
                      r=[B0], w=[B_psX])
                    V(lambda e, kvh=kvh: e.tensor_copy(out=VcA[0:127, kvh, 0:64], in_=psX[0:127, 384:448]), r=[B_psX], w=[B0])

        def new_sm():
            i_ = smi[0] % 4
            smi[0] += 1
            return sm[i_], B_sm[i_]

        LOOK = 2
        queue = []

        def drain(limit):
            while sum(1 for k_, _ in queue if k_ == "ep") > limit:
                queue.pop(0)[1]()

        def flush():
            while queue:
                queue.pop(0)[1]()

        def Q(fn):
            queue.append(("o", fn))

        si = [0]

        def submit(lhsT_ap, rhs_ap, ncol, extra, ep_fn, mparts=128):
            s_ = si[0] % 3
            si[0] += 1
            nmm = 1 + len(extra)
            M(lambda e: e.matmul(psS[s_][0:mparts, 0:ncol], lhsT=lhsT_ap, rhs=rhs_ap, start=True, stop=(nmm == 1)),
              r=[B0, B_nselT], w=[B_psS[s_]])
            for j_, (la, ra) in enumerate(extra):
                M(lambda e: e.matmul(psS[s_][0:mparts, 0:ncol], lhsT=la, rhs=ra, start=False, stop=(j_ == nmm - 2)),
                  r=[B0, B_nselT], w=[B_psS[s_]])
            queue.append(("ep", lambda: ep_fn(s_)))
            drain(LOOK)

        def exp_pv(ps_ap, Bps, bias_ap, acc_i, v_ap, first, last, kparts=128, acc_ap=None, rb=()):
            i_ = ei[0] % NE
            ei[0] += 1
            A(lambda e: e.activation(out=Et[i_][0:kparts, :], in_=ps_ap, func=AF.Exp, bias=bias_ap, scale=SCALE),
              r=[Bps, B0] + list(rb), w=[B_Et[i_]])
            oap = acc[acc_i][:, 0:65] if acc_ap is None else acc_ap
            M(lambda e: e.matmul(oap, lhsT=Et[i_][0:kparts, :], rhs=v_ap, start=first, stop=last),
              r=[B_Et[i_], B0], w=[B_acc[acc_i]] if acc_ap is None else [B_psO])

        def fox_head(qb, h, at, Bat):
            P = slice((h % 2) * 64, (h % 2) * 64 + 64)
            qs = slice(qb * 128, (qb + 1) * 128)
            qc, kc = h // 2, 2 + h // 2
            f_ = fb[h % 2]; Bf = B_fb[h % 2]
            ai = h % 3
            Q(lambda: V(lambda e: e.tensor_scalar(out=f_[:, 0:qb + 1], in0=Lc[:, 0:qb + 1, h], scalar1=Lpre[:, qb, h:h + 1],
                                                  scalar2=None, op0=ALU.subtract), r=[B0], w=[Bf]))
            for kb in range(qb + 1):
                ks = slice(kb * 128, (kb + 1) * 128)
                extra = [(C["ident_bf"][:], C["maskC"][:, 0:128])] if kb == qb else []

                def ep(s_, kb=kb):
                    exp_pv(psS[s_][:, 0:128], B_psS[s_], f_[:, kb:kb + 1], ai, vaug[:, kb, h, :], kb == 0, kb == qb, rb=[Bf])
                submit(hT[P, kc, ks], hT[P, qc, qs], 128, extra, ep)
            t_, Bt = new_sm()

            def norm():
                V(lambda e: e.reciprocal(out=t_[:, 0:1], in_=acc[ai][:, 64:65]), r=[B_acc[ai]], w=[Bt])
                V(lambda e: e.tensor_scalar(out=at[:, h * 64:(h + 1) * 64], in0=acc[ai][:, 0:64], scalar1=t_[:, 0:1],
                                            scalar2=None, op0=ALU.mult), r=[B_acc[ai], Bt], w=[Bat])
            Q(norm)

        def gqa_branch(qb, kvh, qc0, kc, vh, kbs, altab, extra_fn=None):
            P = slice(kvh * 64, kvh * 64 + 64)
            qs = slice(qb * 128, (qb + 1) * 128)
            Qap = hT[P, qc0:qc0 + 3, qs]
            for n_, (kb, mk) in enumerate(kbs):
                ks = slice(kb * 128, (kb + 1) * 128)
                extra = list(extra_fn(kb)) if extra_fn else []
                if mk:
                    extra.append((C["ident_bf"][:], C["mask" + mk][:]))

                def ep(s_, kb=kb, n_=n_):
                    for g in range(3):
                        hh = kvh * 3 + g
                        exp_pv(psS[s_][:, g * 128:(g + 1) * 128], B_psS[s_], altab[:, hh, qb - kb:qb - kb + 1], g,
                               vaug[:, kb, vh, :], n_ == 0, n_ == len(kbs) - 1)
                submit(hT[P, kc, ks], Qap, 384, extra, ep)

        def swa_norm(qb, kvh, at, Bat):
            for g in range(3):
                hh = kvh * 3 + g
                t_, Bt = new_sm()

                def norm(g=g, hh=hh, t_=t_, Bt=Bt):
                    V(lambda e: e.tensor_tensor(out=t_[:, 0:1], in0=acc[g][:, 64:65], in1=sinkf[:, hh:hh + 1], op=ALU.add),
                      r=[B_acc[g], B0], w=[Bt])
                    V(lambda e: e.reciprocal(out=t_[:, 1:2], in_=t_[:, 0:1]), r=[Bt], w=[Bt])
                    V(lambda e: e.tensor_scalar(out=at[:, 256 + hh * 64:256 + (hh + 1) * 64], in0=acc[g][:, 0:64],
                                                scalar1=t_[:, 1:2], scalar2=None, op0=ALU.mult), r=[B_acc[g], Bt], w=[Bat])
                Q(norm)

        def nsa_cmp(qb, kvh):
            P = slice(kvh * 64, kvh * 64 + 64)
            qs = slice(qb * 128, (qb + 1) * 128)
            Qap = hT[P, 8:11, qs]
            ncols = min(127, 8 * qb + 7)
            off = 121 - 8 * qb

            def ep(s_):
                for g in range(3):
                    hh = kvh * 3 + g
                    exp_pv(psS[s_][0:ncols, g * 128:(g + 1) * 128], B_psS[s_], C["cbias"][0:ncols, hh, qb:qb + 1], None,
                           VcA[0:ncols, kvh, :], True, True, kparts=ncols, acc_ap=psO[:, g, 0:97])
            submit(KcT[P, 0:ncols], Qap, 384, [(C["Zc"][0:8, off:off + ncols], C["Rc"][0:8, :])], ep, mparts=ncols)

            def selmask():
                V(lambda e: e.tensor_scalar(out=rdc[:, 0:3], in0=psO[:, :, 64], scalar1=1e-30, scalar2=None, op0=ALU.max),
                  r=[B_psO], w=[B_rdc])
                V(lambda e: e.reciprocal(out=rdc[:, 0:3], in_=rdc[:, 0:3]), r=[B_rdc], w=[B_rdc])
                V(lambda e: e.tensor_scalar(out=pslc[:], in0=psO[:, 0, 65:97], scalar1=rdc[:, 0:1], scalar2=None, op0=ALU.mult),
                  r=[B_psO, B_rdc], w=[B_sel])
                for g in (1, 2):
                    V(lambda e: e.scalar_tensor_tensor(out=pslc[:], in0=psO[:, g, 65:97], scalar=rdc[:, g:g + 1], in1=pslc[:],
                                                       op0=ALU.mult, op1=ALU.add), r=[B_psO, B_rdc, B_sel], w=[B_sel])
                V(lambda e: e.tensor_tensor(out=score[:], in0=pslc[:], in1=C["sel_nf"][:, qb, :], op=ALU.mult), r=[B_sel], w=[B_sel])
                V(lambda e: e.tensor_tensor(out=score[:], in0=score[:], in1=C["sel_ad"][:, qb, :], op=ALU.add), r=[B_sel], w=[B_sel])
                V(lambda e: e.max(out=m8[:], in_=score[:]), r=[B_sel], w=[B_sel])
                V(lambda e: e.tensor_scalar(out=nsel[:], in0=score[:], scalar1=m8[:, 7:8], scalar2=1.0, op0=ALU.is_ge, op1=ALU.subtract),
                  r=[B_sel], w=[B_sel])
                M(lambda e: e.transpose(out=psX[0:32, 0:128], in_=nsel[:], identity=C["ident_f"][:]), r=[B_sel], w=[B_psX])
                for g in range(3):
                    A(lambda e: e.activation(out=nselT[:, g, :], in_=psX[0:32, 0:128], func=AF.Copy, scale=-NEG),
                      r=[B_psX], w=[B_nselT])
            Q(selmask)
            flush()

        def nsa_sel_norm(qb, kvh):
            for g in range(3):
                hh = kvh * 3 + g
                t_, Bt = new_sm()

                def norm(g=g, hh=hh, t_=t_, Bt=Bt):
                    V(lambda e: e.reciprocal(out=t_[:, 0:1], in_=acc[g][:, 64:65]), r=[B_acc[g]], w=[Bt])
                    V(lambda e: e.tensor_tensor(out=t_[:, 1:2], in0=t_[:, 0:1], in1=sgate[:, qb, hh * 3 + 1:hh * 3 + 2], op=ALU.mult),
                      r=[Bt, B0], w=[Bt])
                    V(lambda e: e.tensor_scalar(out=onsa[g][:], in0=acc[g][:, 0:64], scalar1=t_[:, 1:2], scalar2=None, op0=ALU.mult),
                      r=[B_acc[g], Bt], w=[B_on[g]])
                Q(norm)

        def nsa_win_norm(qb, kvh, at, Bat):
            for g in range(3):
                hh = kvh * 3 + g
                t_, Bt = new_sm()

                def norm(g=g, hh=hh, t_=t_, Bt=Bt):
                    V(lambda e: e.reciprocal(out=t_[:, 0:1], in_=acc[g][:, 64:65]), r=[B_acc[g]], w=[Bt])
                    V(lambda e: e.tensor_tensor(out=t_[:, 1:2], in0=t_[:, 0:1], in1=sgate[:, qb, hh * 3 + 2:hh * 3 + 3], op=ALU.mult),
                      r=[Bt, B0], w=[Bt])
                    V(lambda e: e.scalar_tensor_tensor(out=onsa[g][:], in0=acc[g][:, 0:64], scalar=t_[:, 1:2], in1=onsa[g][:],
                                                       op0=ALU.mult, op1=ALU.add), r=[B_acc[g], Bt, B_on[g]], w=[B_on[g]])
                    V(lambda e: e.tensor_tensor(out=t_[:, 2:3], in0=rdc[:, g:g + 1], in1=sgate[:, qb, hh * 3:hh * 3 + 1], op=ALU.mult),
                      r=[B_rdc, B0, Bt], w=[Bt])
                    V(lambda e: e.scalar_tensor_tensor(out=at[:, 640 + hh * 64:640 + (hh + 1) * 64], in0=psO[:, g, 0:64],
                                                       scalar=t_[:, 2:3], in1=onsa[g][:], op0=ALU.mult, op1=ALU.add),
                      r=[B_psO, Bt, B_on[g]], w=[Bat])
                Q(norm)

        for qb in range(NT):
            at = attn_t[qb % 2]
            Bat = B_at[qb % 2]
            for h in range(4):
                fox_head(qb, h, at, Bat)
            for kvh in range(2):
                kbs = ([(qb - 1, "W")] if qb >= 1 else []) + [(qb, "C")]
                gqa_branch(qb, kvh, 4, 7, 4 + kvh, kbs, C["al_swa"])
                swa_norm(qb, kvh, at, Bat)
            for kvh in range(2):
                nsa_cmp(qb, kvh)
                nT = nselT[:].rearrange("j g t -> j (g t)")
                gqa_branch(qb, kvh, 8, 13, 6 + kvh, [(kb, "C" if kb == qb else None) for kb in range(qb + 1)], C["al_sel"],
                           extra_fn=lambda kb: [(C["expand"][:, kb, :], nT)])
                nsa_sel_norm(qb, kvh)
                kbs = ([(qb - 4, "W")] if qb >= 4 else []) + [(kb, None) for kb in range(max(0, qb - 3), qb)] + [(qb, "C")]
                gqa_branch(qb, kvh, 8, 14, 8 + kvh, kbs, C["al_win"])
                nsa_win_norm(qb, kvh, at, Bat)

            def store(qb=qb, at=at, Bat=Bat):
                r0 = b * S + qb * 128
                LD(attn_d[r0:r0 + 128, :], at[:], r=[Bat])
            Q(store)
        flush()


def layer_norm(V, A, G, xin, Bx, s1, g_bc, b_bc, xo, Bxo, tmp, Bt, sm, Bsm, Bc):
    V(lambda e: e.tensor_tensor(out=sm[:, 2:3], in0=s1[:, 0:1], in1=s1[:, 1:2], op=ALU.add), r=[Bsm], w=[Bsm])
    V(lambda e: e.tensor_scalar(out=sm[:, 3:4], in0=sm[:, 2:3], scalar1=1.0 / D, scalar2=None, op0=ALU.mult), r=[Bsm], w=[Bsm])
    V(lambda e: e.tensor_scalar(out=xin[:], in0=xin[:], scalar1=sm[:, 3:4], scalar2=None, op0=ALU.subtract), r=[Bx, Bsm], w=[Bx])
    V(lambda e: e.memset(sm[:, 4:5], 0.0), r=[Bsm], w=[Bsm])
    A(lambda e: e.activation(out=tmp[:], in_=xin[:], func=AF.Square, accum_out=sm[:, 4:5]), r=[Bx, Bsm], w=[Bt, Bsm])
    A(lambda e: e.activation(out=sm[:, 5:6], in_=sm[:, 4:5], func=AF.Sqrt, bias=LN_EPS, scale=1.0 / D), r=[Bsm], w=[Bsm])
    V(lambda e: e.reciprocal(out=sm[:, 6:7], in_=sm[:, 5:6]), r=[Bsm], w=[Bsm])
    V(lambda e: e.scalar_tensor_tensor(out=tmp[:], in0=xin[:], scalar=sm[:, 6:7], in1=g_bc[:], op0=ALU.mult, op1=ALU.mult),
      r=[Bx, Bsm, Bc], w=[Bt])
    G(lambda e: e.tensor_tensor(out=xo[:], in0=tmp[:], in1=b_bc[:], op=ALU.add), r=[Bt, Bc], w=[Bxo])


def phase_C(nc, T, C, l, b, x_src, attn_d, wout_d, ln1g_d, ln1b_d, rw_d, rb_d, x1_d, xg_d, meta_dl,
            acum, B_acum, sbt, pst, V, A, G, M, LD):
    with contextlib.ExitStack() as st:
        wobf = sbt(st, "wobf", [128, 8, D], BF16)
        g_bc = sbt(st, "g_bc", [128, D]); b_bc = sbt(st, "b_bc", [128, D])
        rw = sbt(st, "rw", [128, 8, NEXP]); rb = sbt(st, "rb", [128, NEXP])
        att = [sbt(st, "att%d" % i, [128, D], BF16) for i in range(2)]
        attT = [sbt(st, "attT%d" % i, [128, 8, 128], BF16) for i in range(2)]
        xt = [sbt(st, "xt%d" % i, [128, D]) for i in range(2)]
        x1p = [sbt(st, "x1p%d" % i, [128, D]) for i in range(2)]
        x1 = [sbt(st, "x1_%d" % i, [128, D]) for i in range(2)]
        x1b = [sbt(st, "x1b%d" % i, [128, D], BF16) for i in range(2)]
        x1T = [sbt(st, "x1T%d" % i, [128, 8, 128]) for i in range(2)]
        tmp = sbt(st, "tmpC", [128, D])
        sm = [sbt(st, "smC%d" % i, [128, 8]) for i in range(2)]
        rt = [sbt(st, "rt%d" % i, [128, 8, NEXP]) for i in range(2)]
        rtb = [sbt(st, "rtb%d" % i, [128, NEXP], BF16) for i in range(2)]
        m8 = [sbt(st, "m8C%d" % i, [128, 16]) for i in range(2)]
        wk = [sbt(st, "wk%d" % i, [128, 8]) for i in range(2)]
        dsti = [sbt(st, "dsti%d" % i, [128, 4], I32) for i in range(2)]
        meta = [sbt(st, "metaC%d" % i, [128, 4, 2]) for i in range(2)]
        psT = pst(st, "psTc", [128, D], BF16)
        psM = [pst(st, "psM%d" % i, [128, 512]) for i in range(2)]
        psF = [pst(st, "psF%d" % i, [128, 512]) for i in range(2)]
        psR = pst(st, "psR", [128, 512])
        Bw = Buf(); Bwst = [Buf(), Buf()]; Bc = Buf()
        Batt = [Buf(), Buf()]; BattT = [Buf(), Buf()]; Bxt = [Buf(), Buf()]; Bx1p = [Buf(), Buf()]
        Bx1 = [Buf(), Buf()]; Bx1b = [Buf(), Buf()]; Bx1T = [Buf(), Buf()]; Bt = Buf(); Bsm = [Buf(), Buf()]
        Brt = [Buf(), Buf()]; BpsT = Buf(); BpsM = Buf(); BpsF = Buf(); BpsR = Buf(); Bmeta = [Buf(), Buf()]
        wv = wout_d[l].rearrange("(k p) n -> p k n", p=128)
        for k in range(8):
            LD(wobf[:, k, :], wv[:, k, :], w=[Bw], q="pool")
        LD(g_bc[:], ln1g_d[l].partition_broadcast(128), w=[Bc])
        LD(b_bc[:], ln1b_d[l].partition_broadcast(128), w=[Bc])
        LD(rw[:], rw_d[l].rearrange("(k p) n -> p k n", p=128), w=[Bc])
        LD(rb[:], rb_d[l].partition_broadcast(128), w=[Bc])
        for tt in range(NT):
            s_ = tt % 2
            gt = b * NT + tt
            r0 = gt * 128
            LD(att[s_][:], attn_d[r0:r0 + 128, :], w=[Batt[s_]])
            LD(xt[s_][:], x_src[r0:r0 + 128, :], w=[Bxt[s_]])
            for k in range(8):
                M(lambda e, s_=s_, k=k: e.transpose(out=psT[:, k * 128:(k + 1) * 128], in_=att[s_][:, k * 128:(k + 1) * 128],
                                                     identity=C["ident_bf"][:]), r=[Batt[s_]], w=[BpsT])
            A(lambda e, s_=s_: e.copy(out=attT[s_][:], in_=psT[:].rearrange("p (k t) -> p k t", k=8)), r=[BpsT], w=[BattT[s_]])
            for nh in range(2):
                for k in range(8):
                    M(lambda e, s_=s_, k=k, nh=nh: e.matmul(psM[nh][:], lhsT=attT[s_][:, k, :], rhs=wobf[:, k, nh * 512:(nh + 1) * 512],
                                                           start=(k == 0), stop=(k == 7)), r=[BattT[s_], Bw], w=[BpsM])
            V(lambda e, s_=s_: e.memset(sm[s_][:, 0:2], 0.0), w=[Bsm[s_]])
            for nh in range(2):
                V(lambda e, s_=s_, nh=nh: e.scalar_tensor_tensor(out=x1p[s_][:, nh * 512:(nh + 1) * 512], in0=xt[s_][:, nh * 512:(nh + 1) * 512],
                                                                scalar=ALPHA, in1=psM[nh][:], op0=ALU.mult, op1=ALU.add,
                                                                accum_out=sm[s_][:, nh:nh + 1]),
                  r=[Bxt[s_], BpsM, Bsm[s_]], w=[Bx1p[s_], Bsm[s_]])
            layer_norm(V, A, G, x1p[s_], Bx1p[s_], sm[s_], g_bc, b_bc, x1[s_], Bx1[s_], tmp, Bt, sm[s_], Bsm[s_], Bc)
            LD(x1_d[r0:r0 + 128, :], x1[s_][:], r=[Bx1[s_]])
            A(lambda e, s_=s_: e.copy(out=x1b[s_][:], in_=x1[s_][:]), r=[Bx1[s_]], w=[Bx1b[s_]])
            for k in range(8):
                M(lambda e, s_=s_, k=k: e.transpose(out=psF[k // 4][:, (k % 4) * 128:(k % 4 + 1) * 128], in_=x1[s_][:, k * 128:(k + 1) * 128],
                                                     identity=C["ident_f"][:]), r=[Bx1[s_]], w=[BpsF])
            V(lambda e, s_=s_: e.tensor_copy(out=x1T[s_][:, 0:4, :], in_=psF[0][:].rearrange("p (k t) -> p k t", k=4)), r=[BpsF], w=[Bx1T[s_]])
            V(lambda e, s_=s_: e.tensor_copy(out=x1T[s_][:, 4:8, :], in_=psF[1][:].rearrange("p (k t) -> p k t", k=4)), r=[BpsF], w=[Bx1T[s_]])
            for k in range(8):
                M(lambda e, s_=s_, k=k: e.matmul(psR[:, 0:32], lhsT=x1T[s_][:, k, :], rhs=rw[:, k, :], start=(k == 0), stop=(k == 7)),
                  r=[Bx1T[s_], Bc], w=[BpsR])
            R_ = rt[s_]; Br = Brt[s_]
            lg, Am, ex, gg, dv, junk = (R_[:, i, :] for i in range(6))
            m_ = m8[s_]
            V(lambda e: e.tensor_tensor(out=lg, in0=psR[:, 0:32], in1=rb[:], op=ALU.add), r=[BpsR, Bc], w=[Br])
            V(lambda e: e.max(out=m_[:, 0:8], in_=lg), r=[Br], w=[Br])
            V(lambda e: e.tensor_scalar(out=Am, in0=lg, scalar1=m_[:, 3:4], scalar2=None, op0=ALU.is_ge), r=[Br], w=[Br])
            V(lambda e: e.tensor_scalar(out=m_[:, 8:9], in0=m_[:, 0:1], scalar1=-1.0, scalar2=None, op0=ALU.mult), r=[Br], w=[Br])
            A(lambda e: e.activation(out=ex, in_=lg, func=AF.Exp, bias=m_[:, 8:9], scale=1.0), r=[Br], w=[Br])
            V(lambda e: e.tensor_tensor(out=ex, in0=ex, in1=Am, op=ALU.mult), r=[Br], w=[Br])
            V(lambda e: e.tensor_reduce(out=m_[:, 9:10], in_=ex, axis=AX.X, op=ALU.add), r=[Br], w=[Br])
            V(lambda e: e.reciprocal(out=m_[:, 10:11], in_=m_[:, 9:10]), r=[Br], w=[Br])
            V(lambda e: e.tensor_scalar(out=gg, in0=ex, scalar1=m_[:, 10:11], scalar2=None, op0=ALU.mult), r=[Br], w=[Br])
            V(lambda e, s_=s_: e.tensor_copy(out=rtb[s_][:], in_=Am), r=[Br], w=[Br])
            M(lambda e, s_=s_: e.matmul(psR[:, 64:96], lhsT=C["ltri"][:], rhs=rtb[s_][:], start=True, stop=False), r=[Br, B_acum], w=[BpsR])
            M(lambda e: e.matmul(psR[:, 64:96], lhsT=C["ones_bf"][:], rhs=acum[:], start=False, stop=True), r=[Br, B_acum], w=[BpsR])
            G(lambda e, s_=s_: e.tensor_tensor(out=acum[:], in0=acum[:], in1=rtb[s_][:], op=ALU.add), r=[Br, B_acum], w=[B_acum])
            V(lambda e: e.tensor_scalar(out=dv, in0=psR[:, 64:96], scalar1=float(CAP), scalar2=None, op0=ALU.is_lt), r=[BpsR, Br], w=[Br])
            V(lambda e: e.tensor_tensor(out=dv, in0=dv, in1=Am, op=ALU.mult), r=[Br], w=[Br])
            V(lambda e: e.tensor_tensor(out=junk, in0=psR[:, 64:96], in1=C["iota_e"][:], op=ALU.add), r=[BpsR, Br], w=[Br])
            V(lambda e: e.tensor_tensor(out=dv, in0=dv, in1=junk, op=ALU.mult), r=[Br], w=[Br])
            V(lambda e: e.max(out=m_[:, 0:8], in_=dv), r=[Br], w=[Br])
            V(lambda e, s_=s_: e.tensor_copy(out=dsti[s_][:], in_=m_[:, 0:4]), r=[Br, Bmeta[s_]], w=[Bmeta[s_]])
            V(lambda e, s_=s_: e.memset(wk[s_][:], 0.0), r=[Bmeta[s_]], w=[Bmeta[s_]])
            for k in range(4):
                V(lambda e, s_=s_, k=k: e.scalar_tensor_tensor(out=junk, in0=dv, scalar=m_[:, k:k + 1], in1=gg, op0=ALU.is_equal, op1=ALU.mult,
                                                              accum_out=wk[s_][:, k:k + 1]), r=[Br, Bmeta[s_]], w=[Br, Bmeta[s_]])
            mi = meta[s_][:].bitcast(I32)
            for k in range(4):
                V(lambda e, s_=s_, k=k, gt=gt: e.tensor_copy(out=mi[:, k, 0:1], in_=C["tokid"][:, gt:gt + 1]), r=[Bmeta[s_]], w=[Bmeta[s_]])
                V(lambda e, s_=s_, k=k: e.tensor_copy(out=meta[s_][:, k, 1:2], in_=wk[s_][:, k:k + 1]), r=[Bmeta[s_]], w=[Bmeta[s_]])
            for k in range(4):
                T.dma("pool", lambda e, s_=s_, k=k: e.indirect_dma_start(
                    out=xg_d, out_offset=bass.IndirectOffsetOnAxis(ap=dsti[s_][:, k:k + 1], axis=0), in_=x1b[s_][:], in_offset=None),
                    reads=[Bx1b[s_], Bmeta[s_]])
                T.dma("pool", lambda e, s_=s_, k=k: e.indirect_dma_start(
                    out=meta_dl, out_offset=bass.IndirectOffsetOnAxis(ap=dsti[s_][:, k:k + 1], axis=0), in_=meta[s_][:, k, :], in_offset=None),
                    reads=[Bmeta[s_]])


def phase_D(nc, T, C, l, wgu_d, bgu_d, wdn_d, bdn_d, xg_d, meta_dl, yacc_dl, sbt, pst, V, A, G, M, LD):
    NST = CAP // 128
    HN = CAP // 2
    with contextlib.ExitStack() as st:
        wgu = [sbt(st, "wgu%d" % i, [128, 8, 2 * D], BF16) for i in range(2)]
        wdn = [sbt(st, "wdn%d" % i, [128, 8, D], BF16) for i in range(2)]
        bgu = [sbt(st, "bgu%d" % i, [128, 16]) for i in range(2)]
        bdn = [sbt(st, "bdn%d" % i, [128, D]) for i in range(2)]
        meta = [sbt(st, "metaD%d" % i, [128, NST, 2]) for i in range(2)]
        xgr = [sbt(st, "xgr%d" % i, [128, D], BF16) for i in range(3)]
        xgT = sbt(st, "xgT", [128, 8, CAP], BF16)
        actT = sbt(st, "actT", [128, 8, CAP], BF16)
        NW = 2
        ew = [[sbt(st, "ew%d_%d" % (i, w_), [128, HN]) for i in range(4)] for w_ in range(NW)]
        yt = [sbt(st, "yt%d" % i, [128, D]) for i in range(2)]
        psT = pst(st, "psTd", [128, D], BF16)
        psG = [pst(st, "psG%d" % i, [128, 512]) for i in range(2)]
        psU = [pst(st, "psU%d" % i, [128, 512]) for i in range(2)]
        psY = [pst(st, "psY%d" % i, [128, 512]) for i in range(2)]
        Bwgu = [Buf(), Buf()]; Bwdn = [Buf(), Buf()]; Bsm = [Buf(), Buf()]
        Bxgr = [Buf(), Buf(), Buf()]; BxgT = Buf(); BactT = Buf(); Byt = [Buf(), Buf()]
        Bew = [[Buf() for _ in range(4)] for _ in range(NW)]
        BpsT = Buf(); BpsG = [Buf(), Buf()]; BpsU = [Buf(), Buf()]; BpsY = Buf(); Byacc = Buf()

        def load_weights(e_):
            p = e_ % 2
            wv = wgu_d[l, e_].rearrange("(k p) n -> p k n", p=128)
            for k in range(8):
                LD(wgu[p][:, k, :], wv[:, k, :], w=[Bwgu[p]], q="pool")
            wv2 = wdn_d[l, e_].rearrange("(k p) n -> p k n", p=128)
            for k2 in range(4):
                LD(wdn[p][:, 2 * k2:2 * k2 + 2, :], wv2[:, 2 * k2:2 * k2 + 2, :], w=[Bwdn[p]], q="pool")
            LD(bgu[p][:], bgu_d[l, e_], w=[Bsm[p]])
            LD(bdn[p][:], bdn_d[l, e_:e_ + 1, :].partition_broadcast(128), w=[Bsm[p]])
            s0 = 1 + e_ * CAP
            LD(meta[p][:], meta_dl[s0:s0 + CAP, :].rearrange("(s p) c -> p s c", p=128), w=[Bsm[p]])
            V(lambda e: e.tensor_scalar(out=bgu[p][:, 8:16], in0=bgu[p][:, 8:16], scalar1=1.0, scalar2=None, op0=ALU.add),
              r=[Bsm[p]], w=[Bsm[p]])

        load_weights(0)
        it = 0
        for e_ in range(NEXP):
            p = e_ % 2
            if e_ + 1 < NEXP:
                load_weights(e_ + 1)
            s0 = 1 + e_ * CAP
            for stl in range(NST):
                s_ = stl % 3
                LD(xgr[s_][:], xg_d[s0 + stl * 128:s0 + (stl + 1) * 128, :], w=[Bxgr[s_]])
                for k in range(8):
                    M(lambda e: e.transpose(out=psT[:, k * 128:(k + 1) * 128], in_=xgr[s_][:, k * 128:(k + 1) * 128],
                                            identity=C["ident_bf"][:]), r=[Bxgr[s_]], w=[BpsT])
                A(lambda e: e.copy(out=xgT[:, :, stl * 128:(stl + 1) * 128], in_=psT[:].rearrange("p (k t) -> p k t", k=8)),
                  r=[BpsT], w=[BxgT])
            for j in range(8):
                for hn in range(2):
                    cs = slice(hn * HN, (hn + 1) * HN)
                    b_ = it % 2
                    w_ = it % NW
                    it += 1
                    for k in range(8):
                        M(lambda e: e.matmul(psG[b_][:, 0:HN], lhsT=wgu[p][:, k, j * 128:(j + 1) * 128], rhs=xgT[:, k, cs],
                                             start=(k == 0), stop=(k == 7)), r=[Bwgu[p], BxgT], w=[BpsG[b_]])
                    for k in range(8):
                        M(lambda e: e.matmul(psU[b_][:, 0:HN], lhsT=wgu[p][:, k, D + j * 128:D + (j + 1) * 128], rhs=xgT[:, k, cs],
                                             start=(k == 0), stop=(k == 7)), r=[Bwgu[p], BxgT], w=[BpsU[b_]])
                    g1, sg, u1, glu = ew[w_]
                    Bg1, Bsg, Bu1, Bglu = Bew[w_]
                    V(lambda e: e.tensor_scalar(out=g1[:], in0=psG[b_][:, 0:HN], scalar1=bgu[p][:, j:j + 1], scalar2=7.0,
                                                op0=ALU.add, op1=ALU.min), r=[BpsG[b_], Bsm[p]], w=[Bg1])
                    A(lambda e: e.activation(out=sg[:], in_=g1[:], func=AF.Sigmoid, scale=1.702), r=[Bg1], w=[Bsg])
                    V(lambda e: e.tensor_scalar(out=u1[:], in0=psU[b_][:, 0:HN], scalar1=bgu[p][:, 8 + j:9 + j], scalar2=8.0,
                                                op0=ALU.add, op1=ALU.min), r=[BpsU[b_], Bsm[p]], w=[Bu1])
                    G(lambda e: e.tensor_tensor(out=glu[:], in0=g1[:], in1=sg[:], op=ALU.mult), r=[Bg1, Bsg], w=[Bglu])
                    V(lambda e: e.scalar_tensor_tensor(out=actT[:, j, cs], in0=u1[:], scalar=-6.0, in1=glu[:], op0=ALU.max, op1=ALU.mult),
                      r=[Bu1, Bglu], w=[BactT])
            mi = meta[p][:].bitcast(I32)
            for stl in range(NST):
                s_ = stl % 2
                for nh in range(2):
                    for k in range(8):
                        M(lambda e: e.matmul(psY[nh][:], lhsT=actT[:, k, stl * 128:(stl + 1) * 128],
                                             rhs=wdn[p][:, k, nh * 512:(nh + 1) * 512], start=(k == 0), stop=(k == 7)),
                          r=[BactT, Bwdn[p]], w=[BpsY])
                for nh in range(2):
                    V(lambda e: e.tensor_tensor(out=yt[s_][:, nh * 512:(nh + 1) * 512], in0=psY[nh][:],
                                                in1=bdn[p][:, nh * 512:(nh + 1) * 512], op=ALU.add), r=[BpsY, Bsm[p]], w=[Byt[s_]])
                A(lambda e: e.activation(out=yt[s_][:], in_=yt[s_][:], func=AF.Copy, scale=meta[p][:, stl, 1:2]),
                  r=[Byt[s_], Bsm[p]], w=[Byt[s_]])
                T.dma("pool", lambda e: e.indirect_dma_start(
                    out=yacc_dl, out_offset=bass.IndirectOffsetOnAxis(ap=mi[:, stl, 0:1], axis=0), in_=yt[s_][:], in_offset=None,
                    compute_op=ALU.add), reads=[Byt[s_], Bsm[p]], writes=[Byacc])


def phase_E(nc, T, C, l, NGT, x1_d, yacc_dl, ln2g_d, ln2b_d, x_dst, sbt, pst, V, A, G, M, LD):
    with contextlib.ExitStack() as st:
        g_bc = sbt(st, "g2_bc", [128, D]); b_bc = sbt(st, "b2_bc", [128, D])
        xt = [sbt(st, "xe%d" % i, [128, D]) for i in range(2)]
        yt = [sbt(st, "ye%d" % i, [128, D]) for i in range(2)]
        xp = [sbt(st, "xpe%d" % i, [128, D]) for i in range(2)]
        xo = [sbt(st, "xoe%d" % i, [128, D]) for i in range(2)]
        tmp = sbt(st, "tmpE", [128, D])
        sm = [sbt(st, "smE%d" % i, [128, 8]) for i in range(2)]
        Bc = Buf(); Bxt = [Buf(), Buf()]; Byt = [Buf(), Buf()]; Bxp = [Buf(), Buf()]; Bxo = [Buf(), Buf()]; Bt = Buf(); Bsm = [Buf(), Buf()]
        LD(g_bc[:], ln2g_d[l].partition_broadcast(128), w=[Bc])
        LD(b_bc[:], ln2b_d[l].partition_broadcast(128), w=[Bc])
        for gt in range(NGT):
            s_ = gt % 2
            r0 = gt * 128
            LD(xt[s_][:], x1_d[r0:r0 + 128, :], w=[Bxt[s_]])
            LD(yt[s_][:], yacc_dl[r0:r0 + 128, :], w=[Byt[s_]])
            V(lambda e, s_=s_: e.memset(sm[s_][:, 0:2], 0.0), w=[Bsm[s_]])
            V(lambda e, s_=s_: e.scalar_tensor_tensor(out=xp[s_][:], in0=xt[s_][:], scalar=ALPHA, in1=yt[s_][:], op0=ALU.mult, op1=ALU.add,
                                                     accum_out=sm[s_][:, 0:1]), r=[Bxt[s_], Byt[s_], Bsm[s_]], w=[Bxp[s_], Bsm[s_]])
            layer_norm(V, A, G, xp[s_], Bxp[s_], sm[s_], g_bc, b_bc, xo[s_], Bxo[s_], tmp, Bt, sm[s_], Bsm[s_], Bc)
            LD(x_dst[r0:r0 + 128, :], xo[s_][:], r=[Bxo[s_]])


def prep_inputs(inputs, core, nseq=2):
    perm = _col_perm()
    f = lambda a: np.ascontiguousarray(np.asarray(a, dtype=np.float32))
    m = {}
    x = np.asarray(inputs["x"])
    m["x"] = f(x[core * 2:core * 2 + nseq].reshape(nseq * S, D))
    w_in = np.asarray(inputs["w_in"])[:, :, perm]
    m["w_in"] = f(w_in)
    b_in = np.asarray(inputs["b_in"])[:, perm]
    m["bT"] = f(b_in[:, :NTC * 128].reshape(2, NTC, 128).transpose(0, 2, 1))
    m["bV"] = f(b_in[:, NTC * 128:].reshape(2, 1, NTOKC))
    m["sinks"] = f(np.asarray(inputs["sinks"]).reshape(2, 1, 6))
    m["peT"] = f(np.asarray(inputs["cmp_pe"]).transpose(0, 3, 1, 2))
    m["cmp_w1"] = f(inputs["cmp_w1"])
    m["cmp_w2"] = f(inputs["cmp_w2"])
    m["w_out"] = f(inputs["w_out"])
    for k in ("ln1_g", "ln1_b", "ln2_g", "ln2_b"):
        m[k] = f(np.asarray(inputs[k]).reshape(2, 1, D))
    m["router_w"] = f(inputs["router_w"])
    m["router_b"] = f(np.asarray(inputs["router_b"]).reshape(2, 1, NEXP))
    wgu = np.asarray(inputs["w_gate_up"], dtype=np.float32).reshape(2, NEXP, D, D, 2)
    m["w_gate_up"] = np.ascontiguousarray(wgu.transpose(0, 1, 2, 4, 3)).reshape(2, NEXP, D, 2 * D)
    bgu = np.asarray(inputs["b_gate_up"]).reshape(2, NEXP, 8, 128, 2)
    m["b_gu"] = f(bgu.transpose(0, 1, 3, 4, 2).reshape(2, NEXP, 128, 16))
    m["w_down"] = f(inputs["w_down"])
    m["b_down"] = f(inputs["b_down"])
    return m


_CACHE = {}


def kernel(**inputs):
    if "nc" not in _CACHE:
        _CACHE["nc"] = build()
    nc, consts = _CACHE["nc"]
    shared = None
    in_maps = []
    for core in range(8):
        m = prep_inputs(inputs, core) if shared is None else dict(shared)
        if shared is None:
            shared = {k: v for k, v in m.items() if k != "x"}
            for k, v in consts.items():
                shared["c_" + k] = v
            m = dict(shared, x=m["x"])
        else:
            x = np.asarray(inputs["x"])
            m["x"] = np.ascontiguousarray(x[core * 2:core * 2 + 2].reshape(2 * S, D), dtype=np.float32)
        in_maps.append(m)
    res = run_bass_kernel_spmd(nc, in_maps, core_ids=list(range(8)))
    out = np.concatenate([np.asarray(r["out"]).reshape(2, S, D) for r in res.results], axis=0)
    return out.astype(np.float32)
```

```python
import contextlib
import numpy as np
import ml_dtypes
import concourse.bass as bass
import concourse.mybir as mybir
from concourse.bass_utils import run_bass_kernel_spmd

F32 = mybir.dt.float32
BF16 = mybir.dt.bfloat16
I32 = mybir.dt.int32
ALU = mybir.AluOpType
AF = mybir.ActivationFunctionType
AX = mybir.AxisListType

S = 2048
D = 1024
NT = 16
NEXP = 32
CAP = 768
NSLOT = 1 + NEXP * CAP
NCOLS = 2582
NTC = 15
NTOKC = 662
ALPHA = 4.0 ** 0.25
LN_EPS = 1e-5
NEG = -30000.0
SCALE = 0.125


class Buf:
    __slots__ = ("name", "w", "r")

    def __init__(self, name=""):
        self.name = name
        self.w = None
        self.r = {}


class _Eng:
    def __init__(self, name, eng, sem):
        self.name = name
        self.eng = eng
        self.sem = sem
        self.count = 0
        self.seen = {}


class Tracker:
    def __init__(self, nc, stack, n_dma_sems=24):
        self.nc = nc
        self.e = {}
        for name, eng in (("pe", nc.tensor), ("act", nc.scalar), ("dve", nc.vector),
                          ("pool", nc.gpsimd), ("sp", nc.sync)):
            sem = stack.enter_context(nc.semaphore("prog_" + name))
            self.e[name] = _Eng(name, eng, sem)
        self.dsems = []
        for i in range(n_dma_sems):
            sem = stack.enter_context(nc.semaphore("dma_%d" % i))
            self.dsems.append([sem, 0])
        self.dnext = 0

    def _wait(self, E, tok):
        sem, val = tok
        k = id(sem)
        if E.seen.get(k, 0) >= val:
            return
        E.eng.wait_ge(sem, val)
        E.seen[k] = val

    def _deps(self, E, reads, writes, skip_self):
        toks = []
        for b in reads:
            if b.w is not None:
                toks.append(b.w)
        for b in writes:
            if b.w is not None:
                toks.append(b.w)
            toks.extend(b.r.values())
        for t in toks:
            if skip_self and t[0] is E.sem:
                continue
            self._wait(E, t)

    def _commit(self, tok, reads, writes):
        for b in writes:
            b.w = tok
            b.r = {}
        for b in reads:
            k = id(tok[0])
            if k not in b.r or b.r[k][1] < tok[1]:
                b.r[k] = tok

    def op(self, ename, fn, reads=(), writes=()):
        E = self.e[ename]
        self._deps(E, reads, writes, skip_self=(ename == "pe"))
        ins = fn(E.eng)
        E.count += 1
        ins.then_inc(E.sem, 1)
        tok = (E.sem, E.count)
        self._commit(tok, reads, writes)
        return tok

    def dma(self, qname, fn, reads=(), writes=()):
        E = self.e[qname]
        self._deps(E, reads, writes, skip_self=False)
        slot = self.dsems[self.dnext]
        self.dnext = (self.dnext + 1) % len(self.dsems)
        if slot[1] > 0:
            self._wait(E, (slot[0], slot[1]))
        ins = fn(E.eng)
        slot[1] += 16
        ins.then_inc(slot[0], 16)
        tok = (slot[0], slot[1])
        self._commit(tok, reads, writes)
        return tok

    def barrier(self):
        sp = self.e["sp"]
        for name, E in self.e.items():
            if E is not sp and E.count > 0:
                self._wait(sp, (E.sem, E.count))
        for slot in self.dsems:
            if slot[1] > 0:
                self._wait(sp, (slot[0], slot[1]))
        ins = sp.eng.nop()
        sp.count += 1
        ins.then_inc(sp.sem, 1)
        tok = (sp.sem, sp.count)
        for name, E in self.e.items():
            if E is not sp:
                self._wait(E, tok)
        for name, E in self.e.items():
            for name2, E2 in self.e.items():
                E.seen[id(E2.sem)] = E2.count
            for slot in self.dsems:
                E.seen[id(slot[0])] = slot[1]


def _slopes():
    n = 12
    s = 2.0 ** (-8.0 * np.arange(1, n + 1) / n)
    return s[0::2].astype(np.float64), s[1::2].astype(np.float64)


def _col_perm():
    FOXW, SWAQ, KVW = 256, 384, 128
    off = {}
    names = ["fq", "fk", "fv", "ff", "sq", "sk", "sv", "nq", "nkc", "nvc", "nks", "nvs", "nkw", "nvw", "ng"]
    sizes = [256, 256, 256, 4, 384, 128, 128, 384, 128, 128, 128, 128, 128, 128, 18]
    o = 0
    for n_, s_ in zip(names, sizes):
        off[n_] = o
        o += s_
    cols = []
    r = lambda name, a, b: list(range(off[name] + a, off[name] + b))
    cols += r("fq", 0, 256)
    cols += r("fk", 0, 256)
    for g in range(3):
        cols += r("sq", (0 * 3 + g) * 64, (0 * 3 + g) * 64 + 64) + r("sq", (3 + g) * 64, (3 + g) * 64 + 64)
    cols += r("sk", 0, 128)
    for g in range(3):
        cols += r("nq", (0 * 3 + g) * 64, (0 * 3 + g) * 64 + 64) + r("nq", (3 + g) * 64, (3 + g) * 64 + 64)
    cols += r("nkc", 0, 128)
    cols += r("nvc", 0, 128)
    cols += r("nks", 0, 128)
    cols += r("nkw", 0, 128)
    assert len(cols) == NTC * 128
    cols += r("fv", 0, 256) + r("sv", 0, 128) + r("nvs", 0, 128)
    cols += r("nvw", 0, 128) + r("ff", 0, 4) + r("ng", 0, 18)
    assert len(cols) == NCOLS and len(set(cols)) == NCOLS
    return np.array(cols)


def make_consts():
    c = {}
    bf = ml_dtypes.bfloat16
    swa, nsa = _slopes()
    j = np.arange(128)
    c["ident_bf"] = np.eye(128, dtype=np.float32).astype(bf)
    c["ident_f"] = np.eye(128, dtype=np.float32)
    mC = np.where(j[:, None] > j[None, :], NEG, 0.0).astype(np.float32)
    mW = np.where(j[:, None] <= j[None, :], NEG, 0.0).astype(np.float32)
    c["maskC"] = np.tile(mC, (1, 3)).astype(bf)
    c["maskW"] = np.tile(mW, (1, 3)).astype(bf)
    def alibi(sl, nd):
        t = np.zeros((128, 6, nd), np.float32)
        for h in range(6):
            for d in range(nd):
                t[:, h, d] = sl[h] * (j - 128.0 * d)
        return t
    c["al_swa"] = alibi(swa, 2)
    c["al_win"] = alibi(nsa, 5)
    c["al_sel"] = alibi(nsa, 16)
    c["al_i"] = (swa[None, :] * j[:, None]).astype(np.float32)
    cb = np.zeros((128, 6, 16), np.float32)
    cc = np.arange(128)
    for h in range(6):
        for qb in range(16):
            cb[:, h, qb] = nsa[h] * (16.0 * cc + 31 - 128.0 * qb)
    c["cbias"] = cb
    Z = np.zeros((8, 256), np.float32)
    for r in range(8):
        Z[r, 120 + r] = 1.0
    c["Zc"] = Z.astype(bf)
    R = np.zeros((8, 128), np.float32)
    for r in range(8):
        R[r, :] = np.where(16 * r + 15 > j, NEG, 0.0)
    c["Rc"] = np.tile(R, (1, 3)).astype(bf)
    nf = np.zeros((128, 16, 32), np.float32)
    ad = np.zeros((128, 16, 32), np.float32)
    jb = np.arange(32)
    for qb in range(16):
        t = qb * 128 + j
        cur = t // 64
        forced = (jb[None, :] == 0) | (jb[None, :] == cur[:, None]) | (jb[None, :] == cur[:, None] - 1)
        future = jb[None, :] > cur[:, None]
        nf[:, qb, :] = np.where(future | forced, 0.0, 1.0)
        ad[:, qb, :] = np.where(future, -1.0, np.where(forced, 1.0e4, 0.0))
    c["sel_nf"] = nf
    c["sel_ad"] = ad
    ex = np.zeros((32, 16, 128), np.float32)
    for kb in range(16):
        ex[2 * kb, kb, 0:64] = 1.0
        ex[2 * kb + 1, kb, 64:128] = 1.0
    c["expand"] = ex.astype(bf)
    ncmp = 127
    cs = np.arange(ncmp) * 16
    ss = np.arange(32) * 64
    ov = np.clip(np.minimum(cs[:, None] + 32, ss[None, :] + 64) - np.maximum(cs[:, None], ss[None, :]), 0, None) / 32.0
    ovp = np.zeros((128, 32), np.float32)
    ovp[:127] = ov
    c["ov"] = ovp.astype(bf)
    c["ltri"] = (j[:, None] < j[None, :]).astype(np.float32).astype(bf)
    c["ones_bf"] = np.ones((128, 128), np.float32).astype(bf)
    c["utri_f"] = (j[:, None] <= j[None, :]).astype(np.float32)
    c["ones_f"] = np.ones((128, 128), np.float32)
    c["iota_e"] = np.tile((1.0 + np.arange(32) * CAP)[None, :], (128, 1)).astype(np.float32)
    c["tokid"] = (np.arange(32)[None, :] * 128 + j[:, None]).astype(np.int32)
    meta = np.zeros((NSLOT, 2), np.float32)
    meta[:, 0] = np.array([4096], np.int32).view(np.float32)[0]
    c["meta_init"] = meta
    return c


CONST_DT = {"ident_bf": BF16, "maskC": BF16, "maskW": BF16, "Zc": BF16, "Rc": BF16, "expand": BF16,
            "ov": BF16, "ltri": BF16, "ones_bf": BF16, "tokid": I32}


def build(nseq=2, nlayers=2, dbg=()):
    nc = bass.Bass("TRN2", target_bir_lowering=False)
    NTOK = nseq * S
    NGT = nseq * NT
    L = 2

    def din(name, shape, dt=F32):
        return nc.dram_tensor(name, list(shape), dt, kind="ExternalInput").ap()

    def dscr(name, shape, dt=F32):
        kind = "ExternalOutput" if name in dbg else "Internal"
        return nc.dram_tensor(name, list(shape), dt, kind=kind).ap()

    x_d = din("x", [NTOK, D])
    w_in_d = din("w_in", [L, D, NCOLS])
    bT_d = din("bT", [L, 128, NTC])
    bV_d = din("bV", [L, 1, NTOKC])
    sinks_d = din("sinks", [L, 1, 6])
    peT_d = din("peT", [L, 64, 2, 32])
    w1_d = din("cmp_w1", [L, 2, 2048, 64])
    w2_d = din("cmp_w2", [L, 2, 64, 64])
    wout_d = din("w_out", [L, D, D])
    ln1g_d = din("ln1_g", [L, 1, D]); ln1b_d = din("ln1_b", [L, 1, D])
    ln2g_d = din("ln2_g", [L, 1, D]); ln2b_d = din("ln2_b", [L, 1, D])
    rw_d = din("router_w", [L, D, NEXP]); rb_d = din("router_b", [L, 1, NEXP])
    wgu_d = din("w_gate_up", [L, NEXP, D, 2 * D])
    bgu_d = din("b_gu", [L, NEXP, 128, 16])
    wdn_d = din("w_down", [L, NEXP, D, D])
    bdn_d = din("b_down", [L, NEXP, D])
    consts_np = make_consts()
    cd = {k: din("c_" + k, v.shape, CONST_DT.get(k, F32)) for k, v in consts_np.items()}
    out_d = nc.dram_tensor("out", [NTOK, D], F32, kind="ExternalOutput").ap()

    attn_d = dscr("attn_s", [NTOK, D], BF16)
    x1_d = dscr("x1_s", [NTOK, D])
    xcur_d = dscr("xcur_s", [NTOK, D])
    yacc_d = [dscr("yacc_s%d" % l, [NTOK + 1, D]) for l in range(L)]
    xg_d = dscr("xg_s", [NSLOT, D], BF16)
    meta_d = [dscr("meta_s%d" % l, [NSLOT, 2]) for l in range(L)]

    with contextlib.ExitStack() as gst:
        T = Tracker(nc, gst)

        uid = [0]

        def sbt(st, name, shape, dt=F32):
            uid[0] += 1
            return st.enter_context(nc.sbuf_tensor("s%d_%s" % (uid[0], name), list(shape), dt))

        def pst(st, name, shape, dt=F32):
            uid[0] += 1
            return st.enter_context(nc.psum_tensor("p%d_%s" % (uid[0], name), list(shape), dt))

        def V(fn, r=(), w=()):
            return T.op("dve", fn, r, w)

        def A(fn, r=(), w=()):
            return T.op("act", fn, r, w)

        def G(fn, r=(), w=()):
            return T.op("pool", fn, r, w)

        def M(fn, r=(), w=()):
            return T.op("pe", fn, r, w)

        def LD(out, in_, r=(), w=(), q="sp"):
            return T.dma(q, lambda e: e.dma_start(out=out, in_=in_), r, w)

        C = {}
        for k, v in consts_np.items():
            if k == "meta_init":
                continue
            C[k] = sbt(gst, "k_" + k, v.shape, CONST_DT.get(k, F32))
            LD(C[k][:], cd[k])
        acum = sbt(gst, "acum", [128, NEXP], BF16)
        B_acum = Buf()
        zero_t = sbt(gst, "zero_t", [128, D])
        V(lambda e: e.memset(zero_t[:], 0.0))
        T.barrier()

        for l in range(nlayers):
            x_src = x_d if l == 0 else xcur_d
            x_dst = out_d if l == nlayers - 1 else xcur_d
            G(lambda e: e.memset(acum[:], 0.0), w=[B_acum])
            for r0 in range(0, NTOK + 1, 128):
                rows = min(128, NTOK + 1 - r0)
                LD(yacc_d[l][r0:r0 + rows, :], zero_t[0:rows, :])
            T.dma("pool", lambda e: e.dma_start(out=meta_d[l], in_=cd["meta_init"]))
            T.barrier()

            for b in range(nseq):
                with contextlib.ExitStack() as sst:
                    hT = sbt(sst, "hT", [128, NTC, S], BF16)
                    vaug = sbt(sst, "vaug", [128, NT, 10, 65], BF16)
                    fg = sbt(sst, "fg", [128, NT, 22])
                    phase_A(nc, T, C, l, b, x_src, w_in_d, bT_d, bV_d, hT, vaug, fg, sbt, pst, V, A, G, M, LD)
                    T.barrier()
                    phase_B(nc, T, C, l, b, hT, vaug, fg, sinks_d, peT_d, w1_d, w2_d, attn_d,
                            sbt, pst, V, A, G, M, LD)
                    T.barrier()
                phase_C(nc, T, C, l, b, x_src, attn_d, wout_d, ln1g_d, ln1b_d, rw_d, rb_d, x1_d, xg_d, meta_d[l],
                        acum, B_acum, sbt, pst, V, A, G, M, LD)
                T.barrier()
            phase_D(nc, T, C, l, wgu_d, bgu_d, wdn_d, bdn_d, xg_d, meta_d[l], yacc_d[l], sbt, pst, V, A, G, M, LD)
            T.barrier()
            phase_E(nc, T, C, l, NGT, x1_d, yacc_d[l], ln2g_d, ln2b_d, x_dst, sbt, pst, V, A, G, M, LD)
            T.barrier()
    return nc, consts_np


def phase_A(nc, T, C, l, b, x_src, w_in_d, bT_d, bV_d, hT, vaug, fg, sbt, pst, V, A, G, M, LD):
    with contextlib.ExitStack() as st:
        wbf = sbt(st, "wbf", [128, 8, NCOLS], BF16)
        xT = sbt(st, "xT", [128, 8, S], BF16)
        bT = sbt(st, "bT", [128, NTC])
        bV = sbt(st, "bV", [128, NTOKC])
        xbf = [sbt(st, "xbf%d" % i, [128, D], BF16) for i in range(4)]
        psT = [pst(st, "psTa%d" % i, [128, D], BF16) for i in range(2)]
        psA = [pst(st, "psA%d" % i, [128, 512]) for i in range(2)]
        psV0 = pst(st, "psV0", [128, 512])
        psV1 = pst(st, "psV1", [128, 512])
        B_wbf = Buf(); B_xbf = [Buf() for _ in range(4)]
        B_psT = [Buf(), Buf()]; B_xT = Buf(); B_psA = [Buf(), Buf()]; B_bias = Buf()
        B_pV = Buf(); B_h = Buf()
        LD(bT[:], bT_d[l], w=[B_bias])
        LD(bV[:], bV_d[l].partition_broadcast(128), w=[B_bias])
        G(lambda e: e.memset(vaug[:, :, :, 64:65], 1.0), w=[B_h])
        wv = w_in_d[l].rearrange("(k p) n -> p k n", p=128)
        for k in range(8):
            LD(wbf[:, k, :], wv[:, k, :], w=[B_wbf], q="pool")
        for tt in range(NT):
            s_ = tt % 2
            x_ = tt % 4
            r0 = b * S + tt * 128
            LD(xbf[x_][:], x_src[r0:r0 + 128, :], w=[B_xbf[x_]], q="pool")
            for k in range(8):
                M(lambda e: e.transpose(out=psT[s_][:, k * 128:(k + 1) * 128], in_=xbf[x_][:, k * 128:(k + 1) * 128],
                                        identity=C["ident_bf"][:]), r=[B_xbf[x_]], w=[B_psT[s_]])
            if tt % 2 == 0:
                V(lambda e: e.tensor_copy(out=xT[:, :, tt * 128:(tt + 1) * 128], in_=psT[s_][:].rearrange("p (k t) -> p k t", k=8)),
                  r=[B_psT[s_]], w=[B_xT])
            else:
                A(lambda e: e.copy(out=xT[:, :, tt * 128:(tt + 1) * 128], in_=psT[s_][:].rearrange("p (k t) -> p k t", k=8)),
                  r=[B_psT[s_]], w=[B_xT])
        i = 0
        for c in range(NTC):
            for tq in range(4):
                s_ = i % 2
                for k in range(8):
                    M(lambda e, s_=s_, k=k, c=c, tq=tq: e.matmul(psA[s_][:], lhsT=wbf[:, k, c * 128:(c + 1) * 128],
                                                                rhs=xT[:, k, tq * 512:(tq + 1) * 512], start=(k == 0), stop=(k == 7)),
                      r=[B_wbf, B_xT], w=[B_psA[s_]])
                if i % 2 == 0:
                    A(lambda e, s_=s_, c=c, tq=tq: e.activation(out=hT[:, c, tq * 512:(tq + 1) * 512], in_=psA[s_][:], func=AF.Identity,
                                                               bias=bT[:, c:c + 1], scale=1.0), r=[B_psA[s_], B_bias], w=[B_h])
                else:
                    V(lambda e, s_=s_, c=c, tq=tq: e.tensor_scalar(out=hT[:, c, tq * 512:(tq + 1) * 512], in0=psA[s_][:],
                                                                  scalar1=bT[:, c:c + 1], scalar2=None, op0=ALU.add),
                      r=[B_psA[s_], B_bias], w=[B_h])
                i += 1
        for tt in range(NT):
            for k in range(8):
                M(lambda e, k=k, tt=tt: e.matmul(psV0[:], lhsT=xT[:, k, tt * 128:(tt + 1) * 128], rhs=wbf[:, k, 1920:2432],
                                                 start=(k == 0), stop=(k == 7)), r=[B_wbf, B_xT], w=[B_pV])
            for k in range(8):
                M(lambda e, k=k, tt=tt: e.matmul(psV1[:, 0:150], lhsT=xT[:, k, tt * 128:(tt + 1) * 128], rhs=wbf[:, k, 2432:2582],
                                                 start=(k == 0), stop=(k == 7)), r=[B_wbf, B_xT], w=[B_pV])
            V(lambda e, tt=tt: e.tensor_tensor(out=vaug[:, tt, 0:8, 0:64], in0=psV0[:].rearrange("p (h d) -> p h d", h=8),
                                               in1=bV[:, 0:512].rearrange("p (h d) -> p h d", h=8), op=ALU.add),
              r=[B_pV, B_bias], w=[B_h])
            V(lambda e, tt=tt: e.tensor_tensor(out=vaug[:, tt, 8:10, 0:64], in0=psV1[:, 0:128].rearrange("p (h d) -> p h d", h=2),
                                               in1=bV[:, 512:640].rearrange("p (h d) -> p h d", h=2), op=ALU.add),
              r=[B_pV, B_bias], w=[B_h])
            V(lambda e, tt=tt: e.tensor_tensor(out=fg[:, tt, :], in0=psV1[:, 128:150], in1=bV[:, 640:662], op=ALU.add),
              r=[B_pV, B_bias], w=[B_h])


def phase_B(nc, T, C, l, b, hT, vaug, fg, sinks_d, peT_d, w1_d, w2_d, attn_d, sbt, pst, V, A, G, M, LD):
    with contextlib.ExitStack() as st:
        w1st = sbt(st, "w1st", [128, 2, 32, 64])
        w1bf = sbt(st, "w1bf", [128, 2, 32, 64], BF16)
        w2st = sbt(st, "w2st", [64, 2, 64])
        w2bf = sbt(st, "w2bf", [64, 2, 64], BF16)
        peT = sbt(st, "peT", [64, 2, 32])
        peTb = sbt(st, "peTb", [64, 2, 32], BF16)
        cbc = sbt(st, "cbc", [64, 2])
        sinkb = sbt(st, "sinkb", [128, 6])
        sinkf = sbt(st, "sinkf", [128, 6])
        sgate = sbt(st, "sgate", [128, NT, 18])
        lpos = sbt(st, "lpos", [128, NT, 4])
        tot = sbt(st, "tot", [128, NT, 4])
        Lpre = sbt(st, "Lpre", [128, NT, 4])
        Lc = sbt(st, "Lc", [128, NT, 4])
        KcT = sbt(st, "KcT", [128, 128], BF16)
        VcA = sbt(st, "VcA", [128, 2, 97], BF16)
        gel = [sbt(st, "gel%d" % i, [64, 128]) for i in range(4)]
        Gt = sbt(st, "Gt", [64, 128], BF16)
        psS = [pst(st, "psS%d" % i, [128, 512]) for i in range(3)]
        acc = [pst(st, "acc%d" % i, [128, 512]) for i in range(3)]
        psO = pst(st, "psO", [128, 3, 128])
        psX = pst(st, "psX", [128, 512])
        B_psS = [Buf(), Buf(), Buf()]; B_acc = [Buf(), Buf(), Buf()]; B_psO = Buf(); B_psX = Buf()
        B0 = Buf()
        NE = 6
        Et = [sbt(st, "Et%d" % i, [128, 128], BF16) for i in range(NE)]
        B_Et = [Buf() for _ in range(NE)]
        ei = [0]
        fb = [sbt(st, "fb%d" % i, [128, NT]) for i in range(2)]
        B_fb = [Buf(), Buf()]
        attn_t = [sbt(st, "attn_t%d" % i, [128, D], BF16) for i in range(2)]
        B_at = [Buf(), Buf()]
        sm = [sbt(st, "sm%d" % i, [128, 16]) for i in range(4)]
        B_sm = [Buf() for _ in range(4)]
        smi = [0]
        onsa = [sbt(st, "onsa%d" % i, [128, 64]) for i in range(3)]
        B_on = [Buf(), Buf(), Buf()]
        pslc = sbt(st, "pslc", [128, 32]); score = sbt(st, "score", [128, 32]); m8 = sbt(st, "m8", [128, 8])
        nsel = sbt(st, "nsel", [128, 32]); nselT = sbt(st, "nselT", [32, 3, 128], BF16)
        rdc = sbt(st, "rdc", [128, 4]); gco = sbt(st, "gco", [128, 4])
        B_sel = Buf(); B_nselT = Buf(); B_rdc = Buf()

        w1v = w1_d[l].rearrange("w (l d) j -> d w l j", d=64)
        LD(w1st[0:64], w1v, w=[B0])
        LD(w1st[64:128], w1v, w=[B0])
        LD(w2st[:], w2_d[l].rearrange("w j k -> j w k"), w=[B0])
        LD(peT[:], peT_d[l], w=[B0])
        LD(sinkb[:], sinks_d[l].partition_broadcast(128), w=[B0])
        V(lambda e: e.tensor_copy(out=w1bf[:], in_=w1st[:]), r=[B0], w=[B0])
        V(lambda e: e.tensor_copy(out=w2bf[:], in_=w2st[:]), r=[B0], w=[B0])
        V(lambda e: e.tensor_copy(out=peTb[:], in_=peT[:]), r=[B0], w=[B0])
        V(lambda e: e.tensor_tensor(out=sinkb[:], in0=sinkb[:], in1=C["al_i"][:], op=ALU.add), r=[B0], w=[B0])
        A(lambda e: e.activation(out=sinkf[:], in_=sinkb[:], func=AF.Exp), r=[B0], w=[B0])
        A(lambda e: e.activation(out=sgate[:], in_=fg[:, :, 4:22], func=AF.Sigmoid), r=[B0], w=[B0])
        A(lambda e: e.activation(out=lpos[:], in_=fg[:, :, 0:4], func=AF.Exp, scale=-1.0), r=[B0], w=[B0])
        A(lambda e: e.activation(out=lpos[:], in_=lpos[:], func=AF.Ln, bias=1.0, scale=1.0), r=[B0], w=[B0])
        lp2 = lpos[:].rearrange("p a h -> p (a h)")
        M(lambda e: e.matmul(psX[:, 0:64], lhsT=C["utri_f"][:], rhs=lp2, start=True, stop=True), r=[B0], w=[B_psX])
        M(lambda e: e.matmul(psX[:, 64:128], lhsT=C["ones_f"][:], rhs=lp2, start=True, stop=True), r=[B0], w=[B_psX])
        V(lambda e: e.tensor_copy(out=tot[:].rearrange("p a h -> p (a h)"), in_=psX[:, 64:128]), r=[B_psX], w=[B0])
        V(lambda e: e.memset(Lpre[:, 0, :], 0.0), r=[B0], w=[B0])
        for tt in range(1, NT):
            V(lambda e, tt=tt: e.tensor_tensor(out=Lpre[:, tt, :], in0=Lpre[:, tt - 1, :], in1=tot[:, tt - 1, :], op=ALU.add), r=[B0], w=[B0])
        V(lambda e: e.tensor_tensor(out=Lc[:].rearrange("p a h -> p (a h)"), in0=psX[:, 0:64],
                                    in1=Lpre[:].rearrange("p a h -> p (a h)"), op=ALU.add), r=[B_psX, B0], w=[B0])
        for wh in range(2):
            for ll in range(32):
                M(lambda e, wh=wh, ll=ll: e.matmul(psX[0:64, 200 + wh:201 + wh], lhsT=w1bf[0:64, wh, ll, :], rhs=peTb[0:64, wh, ll:ll + 1],
                                                   start=(ll == 0), stop=(ll == 31)), r=[B0], w=[B_psX])
        V(lambda e: e.tensor_copy(out=cbc[:], in_=psX[0:64, 200:202]), r=[B_psX], w=[B0])
        V(lambda e: e.tensor_copy(out=VcA[:, 0, 65:97], in_=C["ov"][:]), r=[B0], w=[B0])
        V(lambda e: e.tensor_copy(out=VcA[:, 1, 65:97], in_=C["ov"][:]), r=[B0], w=[B0])
        V(lambda e: e.memset(VcA[:, :, 64:65], 1.0), r=[B0], w=[B0])
        for kvh in range(2):
            P = slice(kvh * 64, kvh * 64 + 64)
            for wh in range(2):
                for ll in range(32):
                    M(lambda e, wh=wh, ll=ll, P=P: e.matmul(psX[0:64, 0:127], lhsT=w1bf[P, wh, ll, :],
                                                          rhs=hT[P, 11 + wh, ll:ll + 2017:16], start=(ll == 0), stop=(ll == 31)),
                      r=[B0], w=[B_psX])
                u, x2, inner, sg = gel
                A(lambda e, wh=wh: e.activation(out=u[:, 0:127], in_=psX[0:64, 0:127], func=AF.Identity, bias=cbc[:, wh:wh + 1], scale=1.0),
                  r=[B_psX, B0], w=[B0])
                V(lambda e: e.tensor_tensor(out=x2[:, 0:127], in0=u[:, 0:127], in1=u[:, 0:127], op=ALU.mult), r=[B0], w=[B0])
                V(lambda e: e.tensor_scalar(out=x2[:, 0:127], in0=x2[:, 0:127], scalar1=0.044715, scalar2=1.0, op0=ALU.mult, op1=ALU.add), r=[B0], w=[B0])
                V(lambda e: e.tensor_tensor(out=inner[:, 0:127], in0=x2[:, 0:127], in1=u[:, 0:127], op=ALU.mult), r=[B0], w=[B0])
                A(lambda e: e.activation(out=sg[:, 0:127], in_=inner[:, 0:127], func=AF.Sigmoid, scale=1.5957691216057308), r=[B0], w=[B0])
                V(lambda e: e.tensor_tensor(out=Gt[:, 0:127], in0=u[:, 0:127], in1=sg[:, 0:127], op=ALU.mult), r=[B0], w=[B0])
                if wh == 0:
                    M(lambda e, P=P: e.matmul(psX[P, 256:383], lhsT=w2bf[0:64, 0, :], rhs=Gt[0:64, 0:127], start=True, stop=True),
                      r=[B0], w=[B_psX])
                    V(lambda e, P=P: e.tensor_copy(out=KcT[P, 0:127], in_=psX[P, 256:383]), r=[B_psX], w=[B0])
                else:
                    M(lambda e: e.matmul(psX[0:127, 384:448], lhsT=Gt[0:64, 0:127], rhs=w2bf[0:64, 1, :], start=True, stop=True),
                      r=[B0], w=[B_psX])
                    V(lambda e, kvh=kvh: e.tensor_copy(out=VcA[0:127, kvh, 0:64], in_=psX[0:127, 384:448]), r=[B_psX], w=[B0])

        def new_sm():
            i_ = smi[0] % 4
            smi[0] += 1
            return sm[i_], B_sm[i_]

        LOOK = 2
        queue = []

        def drain(limit):
            while sum(1 for k_, _ in queue if k_ == "ep") > limit:
                queue.pop(0)[1]()

        def flush():
            while queue:
                queue.pop(0)[1]()

        def Q(fn):
            queue.append(("o", fn))

        si = [0]

        def submit(lhsT_ap, rhs_ap, ncol, extra, ep_fn, mparts=128):
            s_ = si[0] % 3
            si[0] += 1
            nmm = 1 + len(extra)
            M(lambda e: e.matmul(psS[s_][0:mparts, 0:ncol], lhsT=lhsT_ap, rhs=rhs_ap, start=True, stop=(nmm == 1)),
              r=[B0, B_nselT], w=[B_psS[s_]])
            for j_, (la, ra) in enumerate(extra):
                M(lambda e: e.matmul(psS[s_][0:mparts, 0:ncol], lhsT=la, rhs=ra, start=False, stop=(j_ == nmm - 2)),
                  r=[B0, B_nselT], w=[B_psS[s_]])
            queue.append(("ep", lambda: ep_fn(s_)))
            drain(LOOK)

        def exp_pv(ps_ap, Bps, bias_ap, acc_i, v_ap, first, last, kparts=128, acc_ap=None, rb=()):
            i_ = ei[0] % NE
            ei[0] += 1
            A(lambda e: e.activation(out=Et[i_][0:kparts, :], in_=ps_ap, func=AF.Exp, bias=bias_ap, scale=SCALE),
              r=[Bps, B0] + list(rb), w=[B_Et[i_]])
            oap = acc[acc_i][:, 0:65] if acc_ap is None else acc_ap
            M(lambda e: e.matmul(oap, lhsT=Et[i_][0:kparts, :], rhs=v_ap, start=first, stop=last),
              r=[B_Et[i_], B0], w=[B_acc[acc_i]] if acc_ap is None else [B_psO])

        def fox_head(qb, h, at, Bat):
            P = slice((h % 2) * 64, (h % 2) * 64 + 64)
            qs = slice(qb * 128, (qb + 1) * 128)
            qc, kc = h // 2, 2 + h // 2
            f_ = fb[h % 2]; Bf = B_fb[h % 2]
            ai = h % 3
            Q(lambda: V(lambda e: e.tensor_scalar(out=f_[:, 0:qb + 1], in0=Lc[:, 0:qb + 1, h], scalar1=Lpre[:, qb, h:h + 1],
                                                  scalar2=None, op0=ALU.subtract), r=[B0], w=[Bf]))
            for kb in range(qb + 1):
                ks = slice(kb * 128, (kb + 1) * 128)
                extra = [(C["ident_bf"][:], C["maskC"][:, 0:128])] if kb == qb else []

                def ep(s_, kb=kb):
                    exp_pv(psS[s_][:, 0:128], B_psS[s_], f_[:, kb:kb + 1], ai, vaug[:, kb, h, :], kb == 0, kb == qb, rb=[Bf])
                submit(hT[P, kc, ks], hT[P, qc, qs], 128, extra, ep)
            t_, Bt = new_sm()

            def norm():
                V(lambda e: e.reciprocal(out=t_[:, 0:1], in_=acc[ai][:, 64:65]), r=[B_acc[ai]], w=[Bt])
                V(lambda e: e.tensor_scalar(out=at[:, h * 64:(h + 1) * 64], in0=acc[ai][:, 0:64], scalar1=t_[:, 0:1],
                                            scalar2=None, op0=ALU.mult), r=[B_acc[ai], Bt], w=[Bat])
            Q(norm)

        def gqa_branch(qb, kvh, qc0, kc, vh, kbs, altab, extra_fn=None):
            P = slice(kvh * 64, kvh * 64 + 64)
            qs = slice(qb * 128, (qb + 1) * 128)
            Qap = hT[P, qc0:qc0 + 3, qs]
            for n_, (kb, mk) in enumerate(kbs):
                ks = slice(kb * 128, (kb + 1) * 128)
                extra = list(extra_fn(kb)) if extra_fn else []
                if mk:
                    extra.append((C["ident_bf"][:], C["mask" + mk][:]))

                def ep(s_, kb=kb, n_=n_):
                    for g in range(3):
                        hh = kvh * 3 + g
                        exp_pv(psS[s_][:, g * 128:(g + 1) * 128], B_psS[s_], altab[:, hh, qb - kb:qb - kb + 1], g,
                               vaug[:, kb, vh, :], n_ == 0, n_ == len(kbs) - 1)
                submit(hT[P, kc, ks], Qap, 384, extra, ep)

        def swa_norm(qb, kvh, at, Bat):
            for g in range(3):
                hh = kvh * 3 + g
                t_, Bt = new_sm()

                def norm(g=g, hh=hh, t_=t_, Bt=Bt):
                    V(lambda e: e.tensor_tensor(out=t_[:, 0:1], in0=acc[g][:, 64:65], in1=sinkf[:, hh:hh + 1], op=ALU.add),
                      r=[B_acc[g], B0], w=[Bt])
                    V(lambda e: e.reciprocal(out=t_[:, 1:2], in_=t_[:, 0:1]), r=[Bt], w=[Bt])
                    V(lambda e: e.tensor_scalar(out=at[:, 256 + hh * 64:256 + (hh + 1) * 64], in0=acc[g][:, 0:64],
                                                scalar1=t_[:, 1:2], scalar2=None, op0=ALU.mult), r=[B_acc[g], Bt], w=[Bat])
                Q(norm)

        def nsa_cmp(qb, kvh):
            P = slice(kvh * 64, kvh * 64 + 64)
            qs = slice(qb * 128, (qb + 1) * 128)
            Qap = hT[P, 8:11, qs]
            ncols = min(127, 8 * qb + 7)
            off = 121 - 8 * qb

            def ep(s_):
                for g in range(3):
                    hh = kvh * 3 + g
                    exp_pv(psS[s_][0:ncols, g * 128:(g + 1) * 128], B_psS[s_], C["cbias"][0:ncols, hh, qb:qb + 1], None,
                           VcA[0:ncols, kvh, :], True, True, kparts=ncols, acc_ap=psO[:, g, 0:97])
            submit(KcT[P, 0:ncols], Qap, 384, [(C["Zc"][0:8, off:off + ncols], C["Rc"][0:8, :])], ep, mparts=ncols)

            def selmask():
                V(lambda e: e.tensor_scalar(out=rdc[:, 0:3], in0=psO[:, :, 64], scalar1=1e-30, scalar2=None, op0=ALU.max),
                  r=[B_psO], w=[B_rdc])
                V(lambda e: e.reciprocal(out=rdc[:, 0:3], in_=rdc[:, 0:3]), r=[B_rdc], w=[B_rdc])
                V(lambda e: e.tensor_scalar(out=pslc[:], in0=psO[:, 0, 65:97], scalar1=rdc[:, 0:1], scalar2=None, op0=ALU.mult),
                  r=[B_psO, B_rdc], w=[B_sel])
                for g in (1, 2):
                    V(lambda e: e.scalar_tensor_tensor(out=pslc[:], in0=psO[:, g, 65:97], scalar=rdc[:, g:g + 1], in1=pslc[:],
                                                       op0=ALU.mult, op1=ALU.add), r=[B_psO, B_rdc, B_sel], w=[B_sel])
                V(lambda e: e.tensor_tensor(out=score[:], in0=pslc[:], in1=C["sel_nf"][:, qb, :], op=ALU.mult), r=[B_sel], w=[B_sel])
                V(lambda e: e.tensor_tensor(out=score[:], in0=score[:], in1=C["sel_ad"][:, qb, :], op=ALU.add), r=[B_sel], w=[B_sel])
                V(lambda e: e.max(out=m8[:], in_=score[:]), r=[B_sel], w=[B_sel])
                V(lambda e: e.tensor_scalar(out=nsel[:], in0=score[:], scalar1=m8[:, 7:8], scalar2=1.0, op0=ALU.is_ge, op1=ALU.subtract),
                  r=[B_sel], w=[B_sel])
                M(lambda e: e.transpose(out=psX[0:32, 0:128], in_=nsel[:], identity=C["ident_f"][:]), r=[B_sel], w=[B_psX])
                for g in range(3):
                    A(lambda e: e.activation(out=nselT[:, g, :], in_=psX[0:32, 0:128], func=AF.Copy, scale=-NEG),
                      r=[B_psX], w=[B_nselT])
            Q(selmask)
            flush()

        def nsa_sel_norm(qb, kvh):
            for g in range(3):
                hh = kvh * 3 + g
                t_, Bt = new_sm()

                def norm(g=g, hh=hh, t_=t_, Bt=Bt):
                    V(lambda e: e.reciprocal(out=t_[:, 0:1], in_=acc[g][:, 64:65]), r=[B_acc[g]], w=[Bt])
                    V(lambda e: e.tensor_tensor(out=t_[:, 1:2], in0=t_[:, 0:1], in1=sgate[:, qb, hh * 3 + 1:hh * 3 + 2], op=ALU.mult),
                      r=[Bt, B0], w=[Bt])
                    V(lambda e: e.tensor_scalar(out=onsa[g][:], in0=acc[g][:, 0:64], scalar1=t_[:, 1:2], scalar2=None, op0=ALU.mult),
                      r=[B_acc[g], Bt], w=[B_on[g]])
                Q(norm)

        def nsa_win_norm(qb, kvh, at, Bat):
            for g in range(3):
                hh = kvh * 3 + g
                t_, Bt = new_sm()

                def norm(g=g, hh=hh, t_=t_, Bt=Bt):
                    V(lambda e: e.reciprocal(out=t_[:, 0:1], in_=acc[g][:, 64:65]), r=[B_acc[g]], w=[Bt])
                    V(lambda e: e.tensor_tensor(out=t_[:, 1:2], in0=t_[:, 0:1], in1=sgate[:, qb, hh * 3 + 2:hh * 3 + 3], op=ALU.mult),
                      r=[Bt, B0], w=[Bt])
                    V(lambda e: e.scalar_tensor_tensor(out=onsa[g][:], in0=acc[g][:, 0:64], scalar=t_[:, 1:2], in1=onsa[g][:],
                                                       op0=ALU.mult, op1=ALU.add), r=[B_acc[g], Bt, B_on[g]], w=[B_on[g]])
                    V(lambda e: e.tensor_tensor(out=t_[:, 2:3], in0=rdc[:, g:g + 1], in1=sgate[:, qb, hh * 3:hh * 3 + 1], op=ALU.mult),
                      r=[B_rdc, B0, Bt], w=[Bt])
                    V(lambda e: e.scalar_tensor_tensor(out=at[:, 640 + hh * 64:640 + (hh + 1) * 64], in0=psO[:, g, 0:64],
                                                       scalar=t_[:, 2:3], in1=onsa[g][:], op0=ALU.mult, op1=ALU.add),
                      r=[B_psO, Bt, B_on[g]], w=[Bat])
                Q(norm)

        for qb in range(NT):
            at = attn_t[qb % 2]
            Bat = B_at[qb % 2]
            for h in range(4):
                fox_head(qb, h, at, Bat)
            for kvh in range(2):
                kbs = ([(qb - 1, "W")] if qb >= 1 else []) + [(qb, "C")]
                gqa_branch(qb, kvh, 4, 7, 4 + kvh, kbs, C["al_swa"])
                swa_norm(qb, kvh, at, Bat)
            for kvh in range(2):
                nsa_cmp(qb, kvh)
                nT = nselT[:].rearrange("j g t -> j (g t)")
                gqa_branch(qb, kvh, 8, 13, 6 + kvh, [(kb, "C" if kb == qb else None) for kb in range(qb + 1)], C["al_sel"],
                           extra_fn=lambda kb: [(C["expand"][:, kb, :], nT)])
                nsa_sel_norm(qb, kvh)
                kbs = ([(qb - 4, "W")] if qb >= 4 else []) + [(kb, None) for kb in range(max(0, qb - 3), qb)] + [(qb, "C")]
                gqa_branch(qb, kvh, 8, 14, 8 + kvh, kbs, C["al_win"])
                nsa_win_norm(qb, kvh, at, Bat)

            def store(qb=qb, at=at, Bat=Bat):
                r0 = b * S + qb * 128
                LD(attn_d[r0:r0 + 128, :], at[:], r=[Bat])
            Q(store)
        flush()


def layer_norm(V, A, G, xin, Bx, s1, g_bc, b_bc, xo, Bxo, tmp, Bt, sm, Bsm, Bc):
    V(lambda e: e.tensor_tensor(out=sm[:, 2:3], in0=s1[:, 0:1], in1=s1[:, 1:2], op=ALU.add), r=[Bsm], w=[Bsm])
    V(lambda e: e.tensor_scalar(out=sm[:, 3:4], in0=sm[:, 2:3], scalar1=1.0 / D, scalar2=None, op0=ALU.mult), r=[Bsm], w=[Bsm])
    V(lambda e: e.tensor_scalar(out=xin[:], in0=xin[:], scalar1=sm[:, 3:4], scalar2=None, op0=ALU.subtract), r=[Bx, Bsm], w=[Bx])
    V(lambda e: e.memset(sm[:, 4:5], 0.0), r=[Bsm], w=[Bsm])
    A(lambda e: e.activation(out=tmp[:], in_=xin[:], func=AF.Square, accum_out=sm[:, 4:5]), r=[Bx, Bsm], w=[Bt, Bsm])
    A(lambda e: e.activation(out=sm[:, 5:6], in_=sm[:, 4:5], func=AF.Sqrt, bias=LN_EPS, scale=1.0 / D), r=[Bsm], w=[Bsm])
    V(lambda e: e.reciprocal(out=sm[:, 6:7], in_=sm[:, 5:6]), r=[Bsm], w=[Bsm])
    V(lambda e: e.scalar_tensor_tensor(out=tmp[:], in0=xin[:], scalar=sm[:, 6:7], in1=g_bc[:], op0=ALU.mult, op1=ALU.mult),
      r=[Bx, Bsm, Bc], w=[Bt])
    G(lambda e: e.tensor_tensor(out=xo[:], in0=tmp[:], in1=b_bc[:], op=ALU.add), r=[Bt, Bc], w=[Bxo])


def phase_C(nc, T, C, l, b, x_src, attn_d, wout_d, ln1g_d, ln1b_d, rw_d, rb_d, x1_d, xg_d, meta_dl,
            acum, B_acum, sbt, pst, V, A, G, M, LD):
    with contextlib.ExitStack() as st:
        wobf = sbt(st, "wobf", [128, 8, D], BF16)
        g_bc = sbt(st, "g_bc", [128, D]); b_bc = sbt(st, "b_bc", [128, D])
        rw = sbt(st, "rw", [128, 8, NEXP]); rb = sbt(st, "rb", [128, NEXP])
        att = [sbt(st, "att%d" % i, [128, D], BF16) for i in range(2)]
        attT = [sbt(st, "attT%d" % i, [128, 8, 128], BF16) for i in range(2)]
        xt = [sbt(st, "xt%d" % i, [128, D]) for i in range(2)]
        x1p = [sbt(st, "x1p%d" % i, [128, D]) for i in range(2)]
        x1 = [sbt(st, "x1_%d" % i, [128, D]) for i in range(2)]
        x1b = [sbt(st, "x1b%d" % i, [128, D], BF16) for i in range(2)]
        x1T = [sbt(st, "x1T%d" % i, [128, 8, 128]) for i in range(2)]
        tmp = sbt(st, "tmpC", [128, D])
        sm = [sbt(st, "smC%d" % i, [128, 8]) for i in range(2)]
        rt = [sbt(st, "rt%d" % i, [128, 8, NEXP]) for i in range(2)]
        rtb = [sbt(st, "rtb%d" % i, [128, NEXP], BF16) for i in range(2)]
        m8 = [sbt(st, "m8C%d" % i, [128, 16]) for i in range(2)]
        wk = [sbt(st, "wk%d" % i, [128, 8]) for i in range(2)]
        dsti = [sbt(st, "dsti%d" % i, [128, 4], I32) for i in range(2)]
        meta = [sbt(st, "metaC%d" % i, [128, 4, 2]) for i in range(2)]
        psT = pst(st, "psTc", [128, D], BF16)
        psM = [pst(st, "psM%d" % i, [128, 512]) for i in range(2)]
        psF = [pst(st, "psF%d" % i, [128, 512]) for i in range(2)]
        psR = pst(st, "psR", [128, 512])
        Bw = Buf(); Bwst = [Buf(), Buf()]; Bc = Buf()
        Batt = [Buf(), Buf()]; BattT = [Buf(), Buf()]; Bxt = [Buf(), Buf()]; Bx1p = [Buf(), Buf()]
        Bx1 = [Buf(), Buf()]; Bx1b = [Buf(), Buf()]; Bx1T = [Buf(), Buf()]; Bt = Buf(); Bsm = [Buf(), Buf()]
        Brt = [Buf(), Buf()]; BpsT = Buf(); BpsM = Buf(); BpsF = Buf(); BpsR = Buf(); Bmeta = [Buf(), Buf()]
        wv = wout_d[l].rearrange("(k p) n -> p k n", p=128)
        for k in range(8):
            LD(wobf[:, k, :], wv[:, k, :], w=[Bw], q="pool")
        LD(g_bc[:], ln1g_d[l].partition_broadcast(128), w=[Bc])
        LD(b_bc[:], ln1b_d[l].partition_broadcast(128), w=[Bc])
        LD(rw[:], rw_d[l].rearrange("(k p) n -> p k n", p=128), w=[Bc])
        LD(rb[:], rb_d[l].partition_broadcast(128), w=[Bc])
        def stage1(tt):
            s_ = tt % 2
            gt = b * NT + tt
            r0 = gt * 128
            LD(att[s_][:], attn_d[r0:r0 + 128, :], w=[Batt[s_]])
            LD(xt[s_][:], x_src[r0:r0 + 128, :], w=[Bxt[s_]])
            for k in range(8):
                M(lambda e, s_=s_, k=k: e.transpose(out=psT[:, k * 128:(k + 1) * 128], in_=att[s_][:, k * 128:(k + 1) * 128],
                                                     identity=C["ident_bf"][:]), r=[Batt[s_]], w=[BpsT])
            A(lambda e, s_=s_: e.copy(out=attT[s_][:], in_=psT[:].rearrange("p (k t) -> p k t", k=8)), r=[BpsT], w=[BattT[s_]])
            for nh in range(2):
                for k in range(8):
                    M(lambda e, s_=s_, k=k, nh=nh: e.matmul(psM[nh][:], lhsT=attT[s_][:, k, :], rhs=wobf[:, k, nh * 512:(nh + 1) * 512],
                                                           start=(k == 0), stop=(k == 7)), r=[BattT[s_], Bw], w=[BpsM])
            V(lambda e, s_=s_: e.memset(sm[s_][:, 0:2], 0.0), w=[Bsm[s_]])
            for nh in range(2):
                V(lambda e, s_=s_, nh=nh: e.scalar_tensor_tensor(out=x1p[s_][:, nh * 512:(nh + 1) * 512], in0=xt[s_][:, nh * 512:(nh + 1) * 512],
                                                                scalar=ALPHA, in1=psM[nh][:], op0=ALU.mult, op1=ALU.add,
                                                                accum_out=sm[s_][:, nh:nh + 1]),
                  r=[Bxt[s_], BpsM, Bsm[s_]], w=[Bx1p[s_], Bsm[s_]])
            layer_norm(V, A, G, x1p[s_], Bx1p[s_], sm[s_], g_bc, b_bc, x1[s_], Bx1[s_], tmp, Bt, sm[s_], Bsm[s_], Bc)
            LD(x1_d[r0:r0 + 128, :], x1[s_][:], r=[Bx1[s_]])
            A(lambda e, s_=s_: e.copy(out=x1b[s_][:], in_=x1[s_][:]), r=[Bx1[s_]], w=[Bx1b[s_]])

        def stage2(tt):
            s_ = tt % 2
            gt = b * NT + tt
            r0 = gt * 128
            for k in range(8):
                M(lambda e, s_=s_, k=k: e.transpose(out=psF[k // 4][:, (k % 4) * 128:(k % 4 + 1) * 128], in_=x1[s_][:, k * 128:(k + 1) * 128],
                                                     identity=C["ident_f"][:]), r=[Bx1[s_]], w=[BpsF])
            V(lambda e, s_=s_: e.tensor_copy(out=x1T[s_][:, 0:4, :], in_=psF[0][:].rearrange("p (k t) -> p k t", k=4)), r=[BpsF], w=[Bx1T[s_]])
            V(lambda e, s_=s_: e.tensor_copy(out=x1T[s_][:, 4:8, :], in_=psF[1][:].rearrange("p (k t) -> p k t", k=4)), r=[BpsF], w=[Bx1T[s_]])
            for k in range(8):
                M(lambda e, s_=s_, k=k: e.matmul(psR[:, 0:32], lhsT=x1T[s_][:, k, :], rhs=rw[:, k, :], start=(k == 0), stop=(k == 7)),
                  r=[Bx1T[s_], Bc], w=[BpsR])
            R_ = rt[s_]; Br = Brt[s_]
            lg, Am, ex, gg, dv, junk = (R_[:, i, :] for i in range(6))
            m_ = m8[s_]
            V(lambda e: e.tensor_tensor(out=lg, in0=psR[:, 0:32], in1=rb[:], op=ALU.add), r=[BpsR, Bc], w=[Br])
            V(lambda e: e.max(out=m_[:, 0:8], in_=lg), r=[Br], w=[Br])
            V(lambda e: e.tensor_scalar(out=Am, in0=lg, scalar1=m_[:, 3:4], scalar2=None, op0=ALU.is_ge), r=[Br], w=[Br])
            V(lambda e: e.tensor_scalar(out=m_[:, 8:9], in0=m_[:, 0:1], scalar1=-1.0, scalar2=None, op0=ALU.mult), r=[Br], w=[Br])
            A(lambda e: e.activation(out=ex, in_=lg, func=AF.Exp, bias=m_[:, 8:9], scale=1.0), r=[Br], w=[Br])
            V(lambda e: e.tensor_tensor(out=ex, in0=ex, in1=Am, op=ALU.mult), r=[Br], w=[Br])
            V(lambda e: e.tensor_reduce(out=m_[:, 9:10], in_=ex, axis=AX.X, op=ALU.add), r=[Br], w=[Br])
            V(lambda e: e.reciprocal(out=m_[:, 10:11], in_=m_[:, 9:10]), r=[Br], w=[Br])
            V(lambda e: e.tensor_scalar(out=gg, in0=ex, scalar1=m_[:, 10:11], scalar2=None, op0=ALU.mult), r=[Br], w=[Br])
            V(lambda e, s_=s_: e.tensor_copy(out=rtb[s_][:], in_=Am), r=[Br], w=[Br])
            M(lambda e, s_=s_: e.matmul(psR[:, 64:96], lhsT=C["ltri"][:], rhs=rtb[s_][:], start=True, stop=False), r=[Br, B_acum], w=[BpsR])
            M(lambda e: e.matmul(psR[:, 64:96], lhsT=C["ones_bf"][:], rhs=acum[:], start=False, stop=True), r=[Br, B_acum], w=[BpsR])
            G(lambda e, s_=s_: e.tensor_tensor(out=acum[:], in0=acum[:], in1=rtb[s_][:], op=ALU.add), r=[Br, B_acum], w=[B_acum])
            V(lambda e: e.tensor_scalar(out=dv, in0=psR[:, 64:96], scalar1=float(CAP), scalar2=None, op0=ALU.is_lt), r=[BpsR, Br], w=[Br])
            V(lambda e: e.tensor_tensor(out=dv, in0=dv, in1=Am, op=ALU.mult), r=[Br], w=[Br])
            V(lambda e: e.tensor_tensor(out=junk, in0=psR[:, 64:96], in1=C["iota_e"][:], op=ALU.add), r=[BpsR, Br], w=[Br])
            V(lambda e: e.tensor_tensor(out=dv, in0=dv, in1=junk, op=ALU.mult), r=[Br], w=[Br])
            V(lambda e: e.max(out=m_[:, 0:8], in_=dv), r=[Br], w=[Br])
            V(lambda e, s_=s_: e.tensor_copy(out=dsti[s_][:], in_=m_[:, 0:4]), r=[Br, Bmeta[s_]], w=[Bmeta[s_]])
            V(lambda e, s_=s_: e.memset(wk[s_][:], 0.0), r=[Bmeta[s_]], w=[Bmeta[s_]])
            for k in range(4):
                V(lambda e, s_=s_, k=k: e.scalar_tensor_tensor(out=junk, in0=dv, scalar=m_[:, k:k + 1], in1=gg, op0=ALU.is_equal, op1=ALU.mult,
                                                              accum_out=wk[s_][:, k:k + 1]), r=[Br, Bmeta[s_]], w=[Br, Bmeta[s_]])
            mi = meta[s_][:].bitcast(I32)
            for k in range(4):
                V(lambda e, s_=s_, k=k, gt=gt: e.tensor_copy(out=mi[:, k, 0:1], in_=C["tokid"][:, gt:gt + 1]), r=[Bmeta[s_]], w=[Bmeta[s_]])
                V(lambda e, s_=s_, k=k: e.tensor_copy(out=meta[s_][:, k, 1:2], in_=wk[s_][:, k:k + 1]), r=[Bmeta[s_]], w=[Bmeta[s_]])
            for k in range(4):
                T.dma("pool", lambda e, s_=s_, k=k: e.indirect_dma_start(
                    out=xg_d, out_offset=bass.IndirectOffsetOnAxis(ap=dsti[s_][:, k:k + 1], axis=0), in_=x1b[s_][:], in_offset=None),
                    reads=[Bx1b[s_], Bmeta[s_]])
                T.dma("pool", lambda e, s_=s_, k=k: e.indirect_dma_start(
                    out=meta_dl, out_offset=bass.IndirectOffsetOnAxis(ap=dsti[s_][:, k:k + 1], axis=0), in_=meta[s_][:, k, :], in_offset=None),
                    reads=[Bmeta[s_]])

        for tt in range(NT):
            stage1(tt)
            if tt >= 1:
                stage2(tt - 1)
        stage2(NT - 1)


def phase_D(nc, T, C, l, wgu_d, bgu_d, wdn_d, bdn_d, xg_d, meta_dl, yacc_dl, sbt, pst, V, A, G, M, LD):
    NST = CAP // 128
    HN = CAP // 2
    with contextlib.ExitStack() as st:
        wgu = [sbt(st, "wgu%d" % i, [128, 8, 2 * D], BF16) for i in range(2)]
        wdn = [sbt(st, "wdn%d" % i, [128, 8, D], BF16) for i in range(2)]
        NSTG = 3
        stg = [sbt(st, "stg%d" % i, [128, 2 * D]) for i in range(NSTG)]
        bgu = [sbt(st, "bgu%d" % i, [128, 16]) for i in range(2)]
        bdn = [sbt(st, "bdn%d" % i, [128, D]) for i in range(2)]
        meta = [sbt(st, "metaD%d" % i, [128, NST, 2]) for i in range(2)]
        NXG = 4
        xgr = [sbt(st, "xgr%d" % i, [128, D], BF16) for i in range(NXG)]
        xgT = sbt(st, "xgT", [128, 8, CAP], BF16)
        actT = sbt(st, "actT", [128, 8, CAP], BF16)
        NW = 2
        ew = [[sbt(st, "ew%d_%d" % (i, w_), [128, HN]) for i in range(4)] for w_ in range(NW)]
        yt = [sbt(st, "yt%d" % i, [128, D]) for i in range(3)]
        psT = [pst(st, "psTd%d" % i, [128, D], BF16) for i in range(2)]
        psG = [pst(st, "psG%d" % i, [128, 512]) for i in range(2)]
        psU = [pst(st, "psU%d" % i, [128, 512]) for i in range(2)]
        psY = [pst(st, "psY%d" % i, [128, 512]) for i in range(2)]
        Bwgu = [Buf(), Buf()]; Bwdn = [Buf(), Buf()]; Bsm = [Buf(), Buf()]; Bstg = [Buf() for _ in range(NSTG)]
        Bxgr = [Buf() for _ in range(NXG)]; BxgT = Buf(); BactT = Buf(); Byt = [Buf(), Buf(), Buf()]
        Bew = [[Buf() for _ in range(4)] for _ in range(NW)]
        BpsT = [Buf(), Buf()]; BpsG = [Buf(), Buf()]; BpsU = [Buf(), Buf()]; BpsY = [Buf(), Buf()]; Byacc = Buf()
        gstep = [0]

        def weight_steps(e_):
            p = e_ % 2
            wv = wgu_d[l, e_].rearrange("(k p) n -> p k n", p=128)
            wv2 = wdn_d[l, e_].rearrange("(k p) n -> p k n", p=128)
            loads, casts = [], []
            for i in range(12):
                g_ = gstep[0]
                gstep[0] += 1
                s_ = g_ % NSTG
                if i < 8:
                    src = wv[:, i, :]
                    dst = wgu[p][:, i, :]
                    stv = stg[s_][:]
                    Bd = Bwgu[p]
                else:
                    k2 = i - 8
                    src = wv2[:, 2 * k2:2 * k2 + 2, :]
                    dst = wdn[p][:, 2 * k2:2 * k2 + 2, :]
                    stv = stg[s_][:].rearrange("p (k n) -> p k n", k=2)
                    Bd = Bwdn[p]
                loads.append(lambda src=src, stv=stv, s_=s_: LD(stv, src, w=[Bstg[s_]]))
                if g_ % 2 == 0:
                    casts.append(lambda dst=dst, stv=stv, s_=s_, Bd=Bd: V(lambda e: e.tensor_copy(out=dst, in_=stv), r=[Bstg[s_]], w=[Bd]))
                else:
                    casts.append(lambda dst=dst, stv=stv, s_=s_, Bd=Bd: A(lambda e: e.copy(out=dst, in_=stv), r=[Bstg[s_]], w=[Bd]))
            steps = []
            PF = NSTG - 1
            for i in range(12):
                def step(i=i):
                    if i == 0:
                        for j_ in range(PF):
                            loads[j_]()
                    if i + PF < 12:
                        loads[i + PF]()
                    casts[i]()
                steps.append(step)
            return steps

        def small_loads(e_):
            p = e_ % 2
            LD(bgu[p][:], bgu_d[l, e_], w=[Bsm[p]])
            LD(bdn[p][:], bdn_d[l, e_:e_ + 1, :].partition_broadcast(128), w=[Bsm[p]])
            s0 = 1 + e_ * CAP
            LD(meta[p][:], meta_dl[s0:s0 + CAP, :].rearrange("(s p) c -> p s c", p=128), w=[Bsm[p]])
            V(lambda e: e.tensor_scalar(out=bgu[p][:, 8:16], in0=bgu[p][:, 8:16], scalar1=1.0, scalar2=None, op0=ALU.add),
              r=[Bsm[p]], w=[Bsm[p]])

        xi = [0]

        def xg_load(e_, stl):
            x_ = xi[0] % NXG
            xi[0] += 1
            s0 = 1 + e_ * CAP
            LD(xgr[x_][:], xg_d[s0 + stl * 128:s0 + (stl + 1) * 128, :], w=[Bxgr[x_]], q="pool")
            return x_

        small_loads(0)
        for stp in weight_steps(0):
            stp()
        it = 0
        prev_toks = []
        pre_x = [xg_load(0, stl) for stl in range(min(NXG, NST))]
        for e_ in range(NEXP):
            p = e_ % 2
            pend = []
            if e_ + 1 < NEXP:
                small_loads(e_ + 1)
                pend = weight_steps(e_ + 1)
            xs = list(pre_x)
            for stl in range(NST):
                if stl >= len(xs):
                    xs.append(xg_load(e_, stl))
                x_ = xs[stl]
                t_ = stl % 2
                for k in range(8):
                    M(lambda e: e.transpose(out=psT[t_][:, k * 128:(k + 1) * 128], in_=xgr[x_][:, k * 128:(k + 1) * 128],
                                            identity=C["ident_bf"][:]), r=[Bxgr[x_]], w=[BpsT[t_]])
                A(lambda e: e.copy(out=xgT[:, :, stl * 128:(stl + 1) * 128], in_=psT[t_][:].rearrange("p (k t) -> p k t", k=8)),
                  r=[BpsT[t_]], w=[BxgT])
            for hn in range(2):
                for j in range(8):
                    if pend:
                        pend.pop(0)()
                    cs = slice(hn * HN, (hn + 1) * HN)
                    b_ = it % 2
                    w_ = it % NW
                    it += 1
                    for k in range(8):
                        M(lambda e: e.matmul(psG[b_][:, 0:HN], lhsT=wgu[p][:, k, j * 128:(j + 1) * 128], rhs=xgT[:, k, cs],
                                             start=(k == 0), stop=(k == 7)), r=[Bwgu[p], BxgT], w=[BpsG[b_]])
                    for k in range(8):
                        M(lambda e: e.matmul(psU[b_][:, 0:HN], lhsT=wgu[p][:, k, D + j * 128:D + (j + 1) * 128], rhs=xgT[:, k, cs],
                                             start=(k == 0), stop=(k == 7)), r=[Bwgu[p], BxgT], w=[BpsU[b_]])
                    g1, sg, u1, glu = ew[w_]
                    Bg1, Bsg, Bu1, Bglu = Bew[w_]
                    V(lambda e: e.tensor_scalar(out=g1[:], in0=psG[b_][:, 0:HN], scalar1=bgu[p][:, j:j + 1], scalar2=7.0,
                                                op0=ALU.add, op1=ALU.min), r=[BpsG[b_], Bsm[p]], w=[Bg1])
                    A(lambda e: e.activation(out=sg[:], in_=g1[:], func=AF.Sigmoid, scale=1.702), r=[Bg1], w=[Bsg])
                    V(lambda e: e.tensor_scalar(out=u1[:], in0=psU[b_][:, 0:HN], scalar1=bgu[p][:, 8 + j:9 + j], scalar2=8.0,
                                                op0=ALU.add, op1=ALU.min), r=[BpsU[b_], Bsm[p]], w=[Bu1])
                    G(lambda e: e.tensor_tensor(out=glu[:], in0=g1[:], in1=sg[:], op=ALU.mult), r=[Bg1, Bsg], w=[Bglu])
                    V(lambda e: e.scalar_tensor_tensor(out=actT[:, j, cs], in0=u1[:], scalar=-6.0, in1=glu[:], op0=ALU.max, op1=ALU.mult),
                      r=[Bu1, Bglu], w=[BactT])
            while pend:
                pend.pop(0)()
            pre_x = [xg_load(e_ + 1, stl) for stl in range(min(NXG, NST))] if e_ + 1 < NEXP else []
            mi = meta[p][:].bitcast(I32)
            cur_toks = []
            for stl in range(NST):
                s_ = (e_ * NST + stl) % 3
                for nh in range(2):
                    for k in range(8):
                        M(lambda e: e.matmul(psY[nh][:], lhsT=actT[:, k, stl * 128:(stl + 1) * 128],
                                             rhs=wdn[p][:, k, nh * 512:(nh + 1) * 512], start=(k == 0), stop=(k == 7)),
                          r=[BactT, Bwdn[p]], w=[BpsY[nh]])
                for nh in range(2):
                    V(lambda e: e.tensor_tensor(out=yt[s_][:, nh * 512:(nh + 1) * 512], in0=psY[nh][:],
                                                in1=bdn[p][:, nh * 512:(nh + 1) * 512], op=ALU.add), r=[BpsY[nh], Bsm[p]], w=[Byt[s_]])
                A(lambda e: e.activation(out=yt[s_][:], in_=yt[s_][:], func=AF.Copy, scale=meta[p][:, stl, 1:2]),
                  r=[Byt[s_], Bsm[p]], w=[Byt[s_]])
                for tk in prev_toks:
                    T._wait(T.e["pool"], tk)
                prev_toks = []
                cur_toks.append(T.dma("pool", lambda e: e.indirect_dma_start(
                    out=yacc_dl, out_offset=bass.IndirectOffsetOnAxis(ap=mi[:, stl, 0:1], axis=0), in_=yt[s_][:], in_offset=None,
                    compute_op=ALU.add), reads=[Byt[s_], Bsm[p]]))
            prev_toks = cur_toks


def phase_E(nc, T, C, l, NGT, x1_d, yacc_dl, ln2g_d, ln2b_d, x_dst, sbt, pst, V, A, G, M, LD):
    with contextlib.ExitStack() as st:
        g_bc = sbt(st, "g2_bc", [128, D]); b_bc = sbt(st, "b2_bc", [128, D])
        xt = [sbt(st, "xe%d" % i, [128, D]) for i in range(2)]
        yt = [sbt(st, "ye%d" % i, [128, D]) for i in range(2)]
        xp = [sbt(st, "xpe%d" % i, [128, D]) for i in range(2)]
        xo = [sbt(st, "xoe%d" % i, [128, D]) for i in range(2)]
        tmp = sbt(st, "tmpE", [128, D])
        sm = [sbt(st, "smE%d" % i, [128, 8]) for i in range(2)]
        Bc = Buf(); Bxt = [Buf(), Buf()]; Byt = [Buf(), Buf()]; Bxp = [Buf(), Buf()]; Bxo = [Buf(), Buf()]; Bt = Buf(); Bsm = [Buf(), Buf()]
        LD(g_bc[:], ln2g_d[l].partition_broadcast(128), w=[Bc])
        LD(b_bc[:], ln2b_d[l].partition_broadcast(128), w=[Bc])
        for gt in range(NGT):
            s_ = gt % 2
            r0 = gt * 128
            LD(xt[s_][:], x1_d[r0:r0 + 128, :], w=[Bxt[s_]])
            LD(yt[s_][:], yacc_dl[r0:r0 + 128, :], w=[Byt[s_]])
            V(lambda e, s_=s_: e.memset(sm[s_][:, 0:2], 0.0), w=[Bsm[s_]])
            V(lambda e, s_=s_: e.scalar_tensor_tensor(out=xp[s_][:], in0=xt[s_][:], scalar=ALPHA, in1=yt[s_][:], op0=ALU.mult, op1=ALU.add,
                                                     accum_out=sm[s_][:, 0:1]), r=[Bxt[s_], Byt[s_], Bsm[s_]], w=[Bxp[s_], Bsm[s_]])
            layer_norm(V, A, G, xp[s_], Bxp[s_], sm[s_], g_bc, b_bc, xo[s_], Bxo[s_], tmp, Bt, sm[s_], Bsm[s_], Bc)
            LD(x_dst[r0:r0 + 128, :], xo[s_][:], r=[Bxo[s_]])


def prep_inputs(inputs, core, nseq=2):
    perm = _col_perm()
    f = lambda a: np.ascontiguousarray(np.asarray(a, dtype=np.float32))
    m = {}
    x = np.asarray(inputs["x"])
    m["x"] = f(x[core * 2:core * 2 + nseq].reshape(nseq * S, D))
    w_in = np.asarray(inputs["w_in"])[:, :, perm]
    m["w_in"] = f(w_in)
    b_in = np.asarray(inputs["b_in"])[:, perm]
    m["bT"] = f(b_in[:, :NTC * 128].reshape(2, NTC, 128).transpose(0, 2, 1))
    m["bV"] = f(b_in[:, NTC * 128:].reshape(2, 1, NTOKC))
    m["sinks"] = f(np.asarray(inputs["sinks"]).reshape(2, 1, 6))
    m["peT"] = f(np.asarray(inputs["cmp_pe"]).transpose(0, 3, 1, 2))
    m["cmp_w1"] = f(inputs["cmp_w1"])
    m["cmp_w2"] = f(inputs["cmp_w2"])
    m["w_out"] = f(inputs["w_out"])
    for k in ("ln1_g", "ln1_b", "ln2_g", "ln2_b"):
        m[k] = f(np.asarray(inputs[k]).reshape(2, 1, D))
    m["router_w"] = f(inputs["router_w"])
    m["router_b"] = f(np.asarray(inputs["router_b"]).reshape(2, 1, NEXP))
    wgu = np.asarray(inputs["w_gate_up"], dtype=np.float32).reshape(2, NEXP, D, D, 2)
    m["w_gate_up"] = np.ascontiguousarray(wgu.transpose(0, 1, 2, 4, 3)).reshape(2, NEXP, D, 2 * D)
    bgu = np.asarray(inputs["b_gate_up"]).reshape(2, NEXP, 8, 128, 2)
    m["b_gu"] = f(bgu.transpose(0, 1, 3, 4, 2).reshape(2, NEXP, 128, 16))
    m["w_down"] = f(inputs["w_down"])
    m["b_down"] = f(inputs["b_down"])
    return m


_CACHE = {}


def kernel(**inputs):
    if "nc" not in _CACHE:
        _CACHE["nc"] = build()
    nc, consts = _CACHE["nc"]
    shared = None
    in_maps = []
    for core in range(8):
        m = prep_inputs(inputs, core) if shared is None else dict(shared)
        if shared is None:
            shared = {k: v for k, v in m.items() if k != "x"}
            for k, v in consts.items():
                shared["c_" + k] = v
            m = dict(shared, x=m["x"])
        else:
            x = np.asarray(inputs["x"])
            m["x"] = np.ascontiguousarray(x[core * 2:core * 2 + 2].reshape(2 * S, D), dtype=np.float32)
        in_maps.append(m)
    res = run_bass_kernel_spmd(nc, in_maps, core_ids=list(range(8)))
    out = np.concatenate([np.asarray(r["out"]).reshape(2, S, D) for r in res.results], axis=0)
    return out.astype(np.float32)
```

```python
import contextlib
import numpy as np
import ml_dtypes
import concourse.bass as bass
import concourse.mybir as mybir
from concourse.bass_utils import run_bass_kernel_spmd

F32 = mybir.dt.float32
BF16 = mybir.dt.bfloat16
I32 = mybir.dt.int32
ALU = mybir.AluOpType
AF = mybir.ActivationFunctionType
AX = mybir.AxisListType

S = 2048
D = 1024
NT = 16
NEXP = 32
CAP = 768
NSLOT = 1 + NEXP * CAP
NCOLS = 2582
NTC = 15
NTOKC = 662
ALPHA = 4.0 ** 0.25
LN_EPS = 1e-5
NEG = -30000.0
SCALE = 0.125


class Buf:
    __slots__ = ("name", "w", "r")

    def __init__(self, name=""):
        self.name = name
        self.w = None
        self.r = {}


class _Eng:
    def __init__(self, name, eng, sem):
        self.name = name
        self.eng = eng
        self.sem = sem
        self.count = 0
        self.seen = {}


class Tracker:
    def __init__(self, nc, stack, n_dma_sems=24):
        self.nc = nc
        self.e = {}
        for name, eng in (("pe", nc.tensor), ("act", nc.scalar), ("dve", nc.vector),
                          ("pool", nc.gpsimd), ("sp", nc.sync)):
            sem = stack.enter_context(nc.semaphore("prog_" + name))
            self.e[name] = _Eng(name, eng, sem)
        self.dsems = []
        for i in range(n_dma_sems):
            sem = stack.enter_context(nc.semaphore("dma_%d" % i))
            self.dsems.append([sem, 0])
        self.dnext = 0

    def _wait(self, E, tok):
        sem, val = tok
        k = id(sem)
        if E.seen.get(k, 0) >= val:
            return
        E.eng.wait_ge(sem, val)
        E.seen[k] = val

    def _deps(self, E, reads, writes, skip_self):
        toks = []
        for b in reads:
            if b.w is not None:
                toks.append(b.w)
        for b in writes:
            if b.w is not None:
                toks.append(b.w)
            toks.extend(b.r.values())
        for t in toks:
            if skip_self and t[0] is E.sem:
                continue
            self._wait(E, t)

    def _commit(self, tok, reads, writes):
        for b in writes:
            b.w = tok
            b.r = {}
        for b in reads:
            k = id(tok[0])
            if k not in b.r or b.r[k][1] < tok[1]:
                b.r[k] = tok

    def op(self, ename, fn, reads=(), writes=()):
        E = self.e[ename]
        self._deps(E, reads, writes, skip_self=(ename == "pe"))
        ins = fn(E.eng)
        E.count += 1
        ins.then_inc(E.sem, 1)
        tok = (E.sem, E.count)
        self._commit(tok, reads, writes)
        return tok

    def dma(self, qname, fn, reads=(), writes=()):
        E = self.e[qname]
        self._deps(E, reads, writes, skip_self=False)
        slot = self.dsems[self.dnext]
        self.dnext = (self.dnext + 1) % len(self.dsems)
        if slot[1] > 0:
            self._wait(E, (slot[0], slot[1]))
        ins = fn(E.eng)
        slot[1] += 16
        ins.then_inc(slot[0], 16)
        tok = (slot[0], slot[1])
        self._commit(tok, reads, writes)
        return tok

    def barrier(self):
        sp = self.e["sp"]
        for name, E in self.e.items():
            if E is not sp and E.count > 0:
                self._wait(sp, (E.sem, E.count))
        for slot in self.dsems:
            if slot[1] > 0:
                self._wait(sp, (slot[0], slot[1]))
        ins = sp.eng.nop()
        sp.count += 1
        ins.then_inc(sp.sem, 1)
        tok = (sp.sem, sp.count)
        for name, E in self.e.items():
            if E is not sp:
                self._wait(E, tok)
        for name, E in self.e.items():
            for name2, E2 in self.e.items():
                E.seen[id(E2.sem)] = E2.count
            for slot in self.dsems:
                E.seen[id(slot[0])] = slot[1]


def _slopes():
    n = 12
    s = 2.0 ** (-8.0 * np.arange(1, n + 1) / n)
    return s[0::2].astype(np.float64), s[1::2].astype(np.float64)


def _col_perm():
    FOXW, SWAQ, KVW = 256, 384, 128
    off = {}
    names = ["fq", "fk", "fv", "ff", "sq", "sk", "sv", "nq", "nkc", "nvc", "nks", "nvs", "nkw", "nvw", "ng"]
    sizes = [256, 256, 256, 4, 384, 128, 128, 384, 128, 128, 128, 128, 128, 128, 18]
    o = 0
    for n_, s_ in zip(names, sizes):
        off[n_] = o
        o += s_
    cols = []
    r = lambda name, a, b: list(range(off[name] + a, off[name] + b))
    cols += r("fq", 0, 256)
    cols += r("fk", 0, 256)
    for g in range(3):
        cols += r("sq", (0 * 3 + g) * 64, (0 * 3 + g) * 64 + 64) + r("sq", (3 + g) * 64, (3 + g) * 64 + 64)
    cols += r("sk", 0, 128)
    for g in range(3):
        cols += r("nq", (0 * 3 + g) * 64, (0 * 3 + g) * 64 + 64) + r("nq", (3 + g) * 64, (3 + g) * 64 + 64)
    cols += r("nkc", 0, 128)
    cols += r("nvc", 0, 128)
    cols += r("nks", 0, 128)
    cols += r("nkw", 0, 128)
    assert len(cols) == NTC * 128
    cols += r("fv", 0, 256) + r("sv", 0, 128) + r("nvs", 0, 128)
    cols += r("nvw", 0, 128) + r("ff", 0, 4) + r("ng", 0, 18)
    assert len(cols) == NCOLS and len(set(cols)) == NCOLS
    return np.array(cols)


def make_consts():
    c = {}
    bf = ml_dtypes.bfloat16
    swa, nsa = _slopes()
    j = np.arange(128)
    c["ident_bf"] = np.eye(128, dtype=np.float32).astype(bf)
    c["ident_f"] = np.eye(128, dtype=np.float32)
    mC = np.where(j[:, None] > j[None, :], NEG, 0.0).astype(np.float32)
    mW = np.where(j[:, None] <= j[None, :], NEG, 0.0).astype(np.float32)
    c["maskC"] = np.tile(mC, (1, 3)).astype(bf)
    c["maskW"] = np.tile(mW, (1, 3)).astype(bf)
    def alibi(sl, nd):
        t = np.zeros((128, 6, nd), np.float32)
        for h in range(6):
            for d in range(nd):
                t[:, h, d] = sl[h] * (j - 128.0 * d)
        return t
    c["al_swa"] = alibi(swa, 2)
    c["al_win"] = alibi(nsa, 5)
    c["al_sel"] = alibi(nsa, 16)
    c["al_i"] = (swa[None, :] * j[:, None]).astype(np.float32)
    cb = np.zeros((128, 6, 16), np.float32)
    cc = np.arange(128)
    for h in range(6):
        for qb in range(16):
            cb[:, h, qb] = nsa[h] * (16.0 * cc + 31 - 128.0 * qb)
    c["cbias"] = cb
    Z = np.zeros((8, 256), np.float32)
    for r in range(8):
        Z[r, 120 + r] = 1.0
    c["Zc"] = Z.astype(bf)
    R = np.zeros((8, 128), np.float32)
    for r in range(8):
        R[r, :] = np.where(16 * r + 15 > j, NEG, 0.0)
    c["Rc"] = np.tile(R, (1, 3)).astype(bf)
    nf = np.zeros((128, 16, 32), np.float32)
    ad = np.zeros((128, 16, 32), np.float32)
    jb = np.arange(32)
    for qb in range(16):
        t = qb * 128 + j
        cur = t // 64
        forced = (jb[None, :] == 0) | (jb[None, :] == cur[:, None]) | (jb[None, :] == cur[:, None] - 1)
        future = jb[None, :] > cur[:, None]
        nf[:, qb, :] = np.where(future | forced, 0.0, 1.0)
        ad[:, qb, :] = np.where(future, -1.0, np.where(forced, 1.0e4, 0.0))
    c["sel_nf"] = nf
    c["sel_ad"] = ad
    ex = np.zeros((32, 16, 128), np.float32)
    for kb in range(16):
        ex[2 * kb, kb, 0:64] = 1.0
        ex[2 * kb + 1, kb, 64:128] = 1.0
    c["expand"] = ex.astype(bf)
    ncmp = 127
    cs = np.arange(ncmp) * 16
    ss = np.arange(32) * 64
    ov = np.clip(np.minimum(cs[:, None] + 32, ss[None, :] + 64) - np.maximum(cs[:, None], ss[None, :]), 0, None) / 32.0
    ovp = np.zeros((128, 32), np.float32)
    ovp[:127] = ov
    c["ov"] = ovp.astype(bf)
    c["ltri"] = (j[:, None] < j[None, :]).astype(np.float32).astype(bf)
    c["ones_bf"] = np.ones((128, 128), np.float32).astype(bf)
    c["utri_f"] = (j[:, None] <= j[None, :]).astype(np.float32)
    c["ones_f"] = np.ones((128, 128), np.float32)
    c["iota_e"] = np.tile((1.0 + np.arange(32) * CAP)[None, :], (128, 1)).astype(np.float32)
    c["tokid"] = (np.arange(32)[None, :] * 128 + j[:, None]).astype(np.int32)
    meta = np.zeros((NSLOT, 2), np.float32)
    meta[:, 0] = np.array([4096], np.int32).view(np.float32)[0]
    c["meta_init"] = meta
    return c


CONST_DT = {"ident_bf": BF16, "maskC": BF16, "maskW": BF16, "Zc": BF16, "Rc": BF16, "expand": BF16,
            "ov": BF16, "ltri": BF16, "ones_bf": BF16, "tokid": I32}


def build(nseq=2, nlayers=2, dbg=()):
    nc = bass.Bass("TRN2", target_bir_lowering=False)
    NTOK = nseq * S
    NGT = nseq * NT
    L = 2

    def din(name, shape, dt=F32):
        return nc.dram_tensor(name, list(shape), dt, kind="ExternalInput").ap()

    def dscr(name, shape, dt=F32):
        kind = "ExternalOutput" if name in dbg else "Internal"
        return nc.dram_tensor(name, list(shape), dt, kind=kind).ap()

    x_d = din("x", [NTOK, D])
    w_in_d = din("w_in", [L, D, NCOLS])
    bT_d = din("bT", [L, 128, NTC])
    bV_d = din("bV", [L, 1, NTOKC])
    sinks_d = din("sinks", [L, 1, 6])
    peT_d = din("peT", [L, 64, 2, 32])
    w1_d = din("cmp_w1", [L, 2, 2048, 64])
    w2_d = din("cmp_w2", [L, 2, 64, 64])
    wout_d = din("w_out", [L, D, D])
    ln1g_d = din("ln1_g", [L, 1, D]); ln1b_d = din("ln1_b", [L, 1, D])
    ln2g_d = din("ln2_g", [L, 1, D]); ln2b_d = din("ln2_b", [L, 1, D])
    rw_d = din("router_w", [L, D, NEXP]); rb_d = din("router_b", [L, 1, NEXP])
    wgu_d = din("w_gate_up", [L, NEXP, D, 2 * D])
    bgu_d = din("b_gu", [L, NEXP, 128, 16])
    wdn_d = din("w_down", [L, NEXP, D, D])
    bdn_d = din("b_down", [L, NEXP, D])
    consts_np = make_consts()
    cd = {k: din("c_" + k, v.shape, CONST_DT.get(k, F32)) for k, v in consts_np.items()}
    out_d = nc.dram_tensor("out", [NTOK, D], F32, kind="ExternalOutput").ap()

    attn_d = dscr("attn_s", [NTOK, D], BF16)
    x1_d = dscr("x1_s", [NTOK, D])
    xcur_d = dscr("xcur_s", [NTOK, D])
    yacc_d = [dscr("yacc_s%d" % l, [NTOK + 1, D]) for l in range(L)]
    xg_d = dscr("xg_s", [NSLOT, D], BF16)
    meta_d = [dscr("meta_s%d" % l, [NSLOT, 2]) for l in range(L)]

    with contextlib.ExitStack() as gst:
        T = Tracker(nc, gst)

        uid = [0]

        def sbt(st, name, shape, dt=F32):
            uid[0] += 1
            return st.enter_context(nc.sbuf_tensor("s%d_%s" % (uid[0], name), list(shape), dt))

        def pst(st, name, shape, dt=F32):
            uid[0] += 1
            return st.enter_context(nc.psum_tensor("p%d_%s" % (uid[0], name), list(shape), dt))

        def V(fn, r=(), w=()):
            return T.op("dve", fn, r, w)

        def A(fn, r=(), w=()):
            return T.op("act", fn, r, w)

        def G(fn, r=(), w=()):
            return T.op("pool", fn, r, w)

        def M(fn, r=(), w=()):
            return T.op("pe", fn, r, w)

        def LD(out, in_, r=(), w=(), q="sp"):
            return T.dma(q, lambda e: e.dma_start(out=out, in_=in_), r, w)

        C = {}
        for k, v in consts_np.items():
            if k == "meta_init":
                continue
            C[k] = sbt(gst, "k_" + k, v.shape, CONST_DT.get(k, F32))
            LD(C[k][:], cd[k])
        acum = sbt(gst, "acum", [128, NEXP], BF16)
        B_acum = Buf()
        zero_t = sbt(gst, "zero_t", [128, D])
        V(lambda e: e.memset(zero_t[:], 0.0))
        T.barrier()

        for l in range(nlayers):
            x_src = x_d if l == 0 else xcur_d
            x_dst = out_d if l == nlayers - 1 else xcur_d
            G(lambda e: e.memset(acum[:], 0.0), w=[B_acum])
            for r0 in range(0, NTOK + 1, 128):
                rows = min(128, NTOK + 1 - r0)
                LD(yacc_d[l][r0:r0 + rows, :], zero_t[0:rows, :])
            T.dma("pool", lambda e: e.dma_start(out=meta_d[l], in_=cd["meta_init"]))
            T.barrier()

            for b in range(nseq):
                with contextlib.ExitStack() as sst:
                    hT = sbt(sst, "hT", [128, NTC, S], BF16)
                    vaug = sbt(sst, "vaug", [128, NT, 10, 65], BF16)
                    fg = sbt(sst, "fg", [128, NT, 22])
                    phase_A(nc, T, C, l, b, x_src, w_in_d, bT_d, bV_d, hT, vaug, fg, sbt, pst, V, A, G, M, LD)
                    T.barrier()
                    phase_B(nc, T, C, l, b, hT, vaug, fg, sinks_d, peT_d, w1_d, w2_d, attn_d,
                            sbt, pst, V, A, G, M, LD)
                    T.barrier()
                phase_C(nc, T, C, l, b, x_src, attn_d, wout_d, ln1g_d, ln1b_d, rw_d, rb_d, x1_d, xg_d, meta_d[l],
                        acum, B_acum, sbt, pst, V, A, G, M, LD)
                T.barrier()
            phase_D(nc, T, C, l, wgu_d, bgu_d, wdn_d, bdn_d, xg_d, meta_d[l], yacc_d[l], sbt, pst, V, A, G, M, LD)
            T.barrier()
            phase_E(nc, T, C, l, NGT, x1_d, yacc_d[l], ln2g_d, ln2b_d, x_dst, sbt, pst, V, A, G, M, LD)
            T.barrier()
    return nc, consts_np


def phase_A(nc, T, C, l, b, x_src, w_in_d, bT_d, bV_d, hT, vaug, fg, sbt, pst, V, A, G, M, LD):
    with contextlib.ExitStack() as st:
        wbf = sbt(st, "wbf", [128, 8, NCOLS], BF16)
        xT = sbt(st, "xT", [128, 8, S], BF16)
        bT = sbt(st, "bT", [128, NTC])
        bV = sbt(st, "bV", [128, NTOKC])
        xbf = [sbt(st, "xbf%d" % i, [128, D], BF16) for i in range(4)]
        psT = [pst(st, "psTa%d" % i, [128, D], BF16) for i in range(2)]
        psA = [pst(st, "psA%d" % i, [128, 512]) for i in range(2)]
        psV0 = pst(st, "psV0", [128, 512])
        psV1 = pst(st, "psV1", [128, 512])
        B_wbf = Buf(); B_xbf = [Buf() for _ in range(4)]
        B_psT = [Buf(), Buf()]; B_xT = Buf(); B_psA = [Buf(), Buf()]; B_bias = Buf()
        B_pV = Buf(); B_h = Buf()
        LD(bT[:], bT_d[l], w=[B_bias])
        LD(bV[:], bV_d[l].partition_broadcast(128), w=[B_bias])
        G(lambda e: e.memset(vaug[:, :, :, 64:65], 1.0), w=[B_h])
        wv = w_in_d[l].rearrange("(k p) n -> p k n", p=128)
        for k in range(8):
            LD(wbf[:, k, :], wv[:, k, :], w=[B_wbf], q="pool")
        for tt in range(NT):
            s_ = tt % 2
            x_ = tt % 4
            r0 = b * S + tt * 128
            LD(xbf[x_][:], x_src[r0:r0 + 128, :], w=[B_xbf[x_]], q="pool")
            for k in range(8):
                M(lambda e: e.transpose(out=psT[s_][:, k * 128:(k + 1) * 128], in_=xbf[x_][:, k * 128:(k + 1) * 128],
                                        identity=C["ident_bf"][:]), r=[B_xbf[x_]], w=[B_psT[s_]])
            if tt % 2 == 0:
                V(lambda e: e.tensor_copy(out=xT[:, :, tt * 128:(tt + 1) * 128], in_=psT[s_][:].rearrange("p (k t) -> p k t", k=8)),
                  r=[B_psT[s_]], w=[B_xT])
            else:
                A(lambda e: e.copy(out=xT[:, :, tt * 128:(tt + 1) * 128], in_=psT[s_][:].rearrange("p (k t) -> p k t", k=8)),
                  r=[B_psT[s_]], w=[B_xT])
        i = 0
        for c in range(NTC):
            for tq in range(4):
                s_ = i % 2
                for k in range(8):
                    M(lambda e, s_=s_, k=k, c=c, tq=tq: e.matmul(psA[s_][:], lhsT=wbf[:, k, c * 128:(c + 1) * 128],
                                                                rhs=xT[:, k, tq * 512:(tq + 1) * 512], start=(k == 0), stop=(k == 7)),
                      r=[B_wbf, B_xT], w=[B_psA[s_]])
                if i % 2 == 0:
                    A(lambda e, s_=s_, c=c, tq=tq: e.activation(out=hT[:, c, tq * 512:(tq + 1) * 512], in_=psA[s_][:], func=AF.Identity,
                                                               bias=bT[:, c:c + 1], scale=1.0), r=[B_psA[s_], B_bias], w=[B_h])
                else:
                    V(lambda e, s_=s_, c=c, tq=tq: e.tensor_scalar(out=hT[:, c, tq * 512:(tq + 1) * 512], in0=psA[s_][:],
                                                                  scalar1=bT[:, c:c + 1], scalar2=None, op0=ALU.add),
                      r=[B_psA[s_], B_bias], w=[B_h])
                i += 1
        for tt in range(NT):
            for k in range(8):
                M(lambda e, k=k, tt=tt: e.matmul(psV0[:], lhsT=xT[:, k, tt * 128:(tt + 1) * 128], rhs=wbf[:, k, 1920:2432],
                                                 start=(k == 0), stop=(k == 7)), r=[B_wbf, B_xT], w=[B_pV])
            for k in range(8):
                M(lambda e, k=k, tt=tt: e.matmul(psV1[:, 0:150], lhsT=xT[:, k, tt * 128:(tt + 1) * 128], rhs=wbf[:, k, 2432:2582],
                                                 start=(k == 0), stop=(k == 7)), r=[B_wbf, B_xT], w=[B_pV])
            V(lambda e, tt=tt: e.tensor_tensor(out=vaug[:, tt, 0:8, 0:64], in0=psV0[:].rearrange("p (h d) -> p h d", h=8),
                                               in1=bV[:, 0:512].rearrange("p (h d) -> p h d", h=8), op=ALU.add),
              r=[B_pV, B_bias], w=[B_h])
            V(lambda e, tt=tt: e.tensor_tensor(out=vaug[:, tt, 8:10, 0:64], in0=psV1[:, 0:128].rearrange("p (h d) -> p h d", h=2),
                                               in1=bV[:, 512:640].rearrange("p (h d) -> p h d", h=2), op=ALU.add),
              r=[B_pV, B_bias], w=[B_h])
            V(lambda e, tt=tt: e.tensor_tensor(out=fg[:, tt, :], in0=psV1[:, 128:150], in1=bV[:, 640:662], op=ALU.add),
              r=[B_pV, B_bias], w=[B_h])


def phase_B(nc, T, C, l, b, hT, vaug, fg, sinks_d, peT_d, w1_d, w2_d, attn_d, sbt, pst, V, A, G, M, LD):
    with contextlib.ExitStack() as st:
        w1st = sbt(st, "w1st", [128, 2, 32, 64])
        w1bf = sbt(st, "w1bf", [128, 2, 32, 64], BF16)
        w2st = sbt(st, "w2st", [64, 2, 64])
        w2bf = sbt(st, "w2bf", [64, 2, 64], BF16)
        peT = sbt(st, "peT", [64, 2, 32])
        peTb = sbt(st, "peTb", [64, 2, 32], BF16)
        cbc = sbt(st, "cbc", [64, 2])
        sinkb = sbt(st, "sinkb", [128, 6])
        sinkf = sbt(st, "sinkf", [128, 6])
        sgate = sbt(st, "sgate", [128, NT, 18])
        lpos = sbt(st, "lpos", [128, NT, 4])
        tot = sbt(st, "tot", [128, NT, 4])
        Lpre = sbt(st, "Lpre", [128, NT, 4])
        Lc = sbt(st, "Lc", [128, NT, 4])
        KcT = sbt(st, "KcT", [128, 128], BF16)
        VcA = sbt(st, "VcA", [128, 2, 97], BF16)
        gel = [sbt(st, "gel%d" % i, [64, 128]) for i in range(4)]
        Gt = sbt(st, "Gt", [64, 128], BF16)
        psS = [pst(st, "psS%d" % i, [128, 512]) for i in range(3)]
        acc = [pst(st, "acc%d" % i, [128, 512]) for i in range(3)]
        psO = pst(st, "psO", [128, 3, 128])
        psX = pst(st, "psX", [128, 512])
        B_psS = [Buf(), Buf(), Buf()]; B_acc = [Buf(), Buf(), Buf()]; B_psO = Buf(); B_psX = Buf()
        B0 = Buf()
        NE = 6
        Et = [sbt(st, "Et%d" % i, [128, 128], BF16) for i in range(NE)]
        B_Et = [Buf() for _ in range(NE)]
        ei = [0]
        fb = [sbt(st, "fb%d" % i, [128, NT]) for i in range(2)]
        B_fb = [Buf(), Buf()]
        attn_t = [sbt(st, "attn_t%d" % i, [128, D], BF16) for i in range(2)]
        B_at = [Buf(), Buf()]
        sm = [sbt(st, "sm%d" % i, [128, 16]) for i in range(4)]
        B_sm = [Buf() for _ in range(4)]
        smi = [0]
        onsa = [sbt(st, "onsa%d" % i, [128, 64]) for i in range(3)]
        B_on = [Buf(), Buf(), Buf()]
        pslc = sbt(st, "pslc", [128, 32]); score = sbt(st, "score", [128, 32]); m8 = sbt(st, "m8", [128, 8])
        nsel = sbt(st, "nsel", [128, 32]); nselT = sbt(st, "nselT", [32, 3, 128], BF16)
        rdc = sbt(st, "rdc", [128, 4]); gco = sbt(st, "gco", [128, 4])
        B_sel = Buf(); B_nselT = Buf(); B_rdc = Buf()

        w1v = w1_d[l].rearrange("w (l d) j -> d w l j", d=64)
        LD(w1st[0:64], w1v, w=[B0])
        LD(w1st[64:128], w1v, w=[B0])
        LD(w2st[:], w2_d[l].rearrange("w j k -> j w k"), w=[B0])
        LD(peT[:], peT_d[l], w=[B0])
        LD(sinkb[:], sinks_d[l].partition_broadcast(128), w=[B0])
        V(lambda e: e.tensor_copy(out=w1bf[:], in_=w1st[:]), r=[B0], w=[B0])
        V(lambda e: e.tensor_copy(out=w2bf[:], in_=w2st[:]), r=[B0], w=[B0])
        V(lambda e: e.tensor_copy(out=peTb[:], in_=peT[:]), r=[B0], w=[B0])
        V(lambda e: e.tensor_tensor(out=sinkb[:], in0=sinkb[:], in1=C["al_i"][:], op=ALU.add), r=[B0], w=[B0])
        A(lambda e: e.activation(out=sinkf[:], in_=sinkb[:], func=AF.Exp), r=[B0], w=[B0])
        A(lambda e: e.activation(out=sgate[:], in_=fg[:, :, 4:22], func=AF.Sigmoid), r=[B0], w=[B0])
        A(lambda e: e.activation(out=lpos[:], in_=fg[:, :, 0:4], func=AF.Exp, scale=-1.0), r=[B0], w=[B0])
        A(lambda e: e.activation(out=lpos[:], in_=lpos[:], func=AF.Ln, bias=1.0, scale=1.0), r=[B0], w=[B0])
        lp2 = lpos[:].rearrange("p a h -> p (a h)")
        M(lambda e: e.matmul(psX[:, 0:64], lhsT=C["utri_f"][:], rhs=lp2, start=True, stop=True), r=[B0], w=[B_psX])
        M(lambda e: e.matmul(psX[:, 64:128], lhsT=C["ones_f"][:], rhs=lp2, start=True, stop=True), r=[B0], w=[B_psX])
        V(lambda e: e.tensor_copy(out=tot[:].rearrange("p a h -> p (a h)"), in_=psX[:, 64:128]), r=[B_psX], w=[B0])
        V(lambda e: e.memset(Lpre[:, 0, :], 0.0), r=[B0], w=[B0])
        for tt in range(1, NT):
            V(lambda e, tt=tt: e.tensor_tensor(out=Lpre[:, tt, :], in0=Lpre[:, tt - 1, :], in1=tot[:, tt - 1, :], op=ALU.add), r=[B0], w=[B0])
        V(lambda e: e.tensor_tensor(out=Lc[:].rearrange("p a h -> p (a h)"), in0=psX[:, 0:64],
                                    in1=Lpre[:].rearrange("p a h -> p (a h)"), op=ALU.add), r=[B_psX, B0], w=[B0])
        for wh in range(2):
            for ll in range(32):
                M(lambda e, wh=wh, ll=ll: e.matmul(psX[0:64, 200 + wh:201 + wh], lhsT=w1bf[0:64, wh, ll, :], rhs=peTb[0:64, wh, ll:ll + 1],
                                                   start=(ll == 0), stop=(ll == 31)), r=[B0], w=[B_psX])
        V(lambda e: e.tensor_copy(out=cbc[:], in_=psX[0:64, 200:202]), r=[B_psX], w=[B0])
        V(lambda e: e.tensor_copy(out=VcA[:, 0, 65:97], in_=C["ov"][:]), r=[B0], w=[B0])
        V(lambda e: e.tensor_copy(out=VcA[:, 1, 65:97], in_=C["ov"][:]), r=[B0], w=[B0])
        V(lambda e: e.memset(VcA[:, :, 64:65], 1.0), r=[B0], w=[B0])
        for kvh in range(2):
            P = slice(kvh * 64, kvh * 64 + 64)
            for wh in range(2):
                for ll in range(32):
                    M(lambda e, wh=wh, ll=ll, P=P: e.matmul(psX[0:64, 0:127], lhsT=w1bf[P, wh, ll, :],
                                                          rhs=hT[P, 11 + wh, ll:ll + 2017:16], start=(ll == 0), stop=(ll == 31)),
                      r=[B0], w=[B_psX])
                u, x2, inner, sg = gel
                A(lambda e, wh=wh: e.activation(out=u[:, 0:127], in_=psX[0:64, 0:127], func=AF.Identity, bias=cbc[:, wh:wh + 1], scale=1.0),
                  r=[B_psX, B0], w=[B0])
                V(lambda e: e.tensor_tensor(out=x2[:, 0:127], in0=u[:, 0:127], in1=u[:, 0:127], op=ALU.mult), r=[B0], w=[B0])
                V(lambda e: e.tensor_scalar(out=x2[:, 0:127], in0=x2[:, 0:127], scalar1=0.044715, scalar2=1.0, op0=ALU.mult, op1=ALU.add), r=[B0], w=[B0])
                V(lambda e: e.tensor_tensor(out=inner[:, 0:127], in0=x2[:, 0:127], in1=u[:, 0:127], op=ALU.mult), r=[B0], w=[B0])
                A(lambda e: e.activation(out=sg[:, 0:127], in_=inner[:, 0:127], func=AF.Sigmoid, scale=1.5957691216057308), r=[B0], w=[B0])
                V(lambda e: e.tensor_tensor(out=Gt[:, 0:127], in0=u[:, 0:127], in1=sg[:, 0:127], op=ALU.mult), r=[B0], w=[B0])
                if wh == 0:
                    M(lambda e, P=P: e.matmul(psX[P, 256:383], lhsT=w2bf[0:64, 0, :], rhs=Gt[0:64, 0:127], start=True, stop=True),
                      r=[B0], w=[B_psX])
                    V(lambda e, P=P: e.tensor_copy(out=KcT[P, 0:127], in_=psX[P, 256:383]), r=[B_psX], w=[B0])
                else:
                    M(lambda e: e.matmul(psX[0:127, 384:448], lhsT=Gt[0:64, 0:127], rhs=w2bf[0:64, 1, :], start=True, stop=True),
                      r=[B0], w=[B_psX])
                    V(lambda e, kvh=kvh: e.tensor_copy(out=VcA[0:127, kvh, 0:64], in_=psX[0:127, 384:448]), r=[B_psX], w=[B0])

        def new_sm():
            i_ = smi[0] % 4
            smi[0] += 1
            return sm[i_], B_sm[i_]

        LOOK = 2
        queue = []

        def drain(limit):
            while sum(1 for k_, _ in queue if k_ == "ep") > limit:
                queue.pop(0)[1]()

        def flush():
            while queue:
                queue.pop(0)[1]()

        def Q(fn):
            queue.append(("o", fn))

        si = [0]

        def submit(lhsT_ap, rhs_ap, ncol, extra, ep_fn, mparts=128):
            s_ = si[0] % 3
            si[0] += 1
            nmm = 1 + len(extra)
            M(lambda e: e.matmul(psS[s_][0:mparts, 0:ncol], lhsT=lhsT_ap, rhs=rhs_ap, start=True, stop=(nmm == 1)),
              r=[B0, B_nselT], w=[B_psS[s_]])
            for j_, (la, ra) in enumerate(extra):
                M(lambda e: e.matmul(psS[s_][0:mparts, 0:ncol], lhsT=la, rhs=ra, start=False, stop=(j_ == nmm - 2)),
                  r=[B0, B_nselT], w=[B_psS[s_]])
            queue.append(("ep", lambda: ep_fn(s_)))
            drain(LOOK)

        def exp_pv(ps_ap, Bps, bias_ap, acc_i, v_ap, first, last, kparts=128, acc_ap=None, rb=()):
            i_ = ei[0] % NE
            ei[0] += 1
            A(lambda e: e.activation(out=Et[i_][0:kparts, :], in_=ps_ap, func=AF.Exp, bias=bias_ap, scale=SCALE),
              r=[Bps, B0] + list(rb), w=[B_Et[i_]])
            oap = acc[acc_i][:, 0:65] if acc_ap is None else acc_ap
            M(lambda e: e.matmul(oap, lhsT=Et[i_][0:kparts, :], rhs=v_ap, start=first, stop=last),
              r=[B_Et[i_], B0], w=[B_acc[acc_i]] if acc_ap is None else [B_psO])

        def fox_head(qb, h, at, Bat):
            P = slice((h % 2) * 64, (h % 2) * 64 + 64)
            qs = slice(qb * 128, (qb + 1) * 128)
            qc, kc = h // 2, 2 + h // 2
            f_ = fb[h % 2]; Bf = B_fb[h % 2]
            ai = h % 3
            Q(lambda: V(lambda e: e.tensor_scalar(out=f_[:, 0:qb + 1], in0=Lc[:, 0:qb + 1, h], scalar1=Lpre[:, qb, h:h + 1],
                                                  scalar2=None, op0=ALU.subtract), r=[B0], w=[Bf]))
            for kb in range(qb + 1):
                ks = slice(kb * 128, (kb + 1) * 128)
                extra = [(C["ident_bf"][:], C["maskC"][:, 0:128])] if kb == qb else []

                def ep(s_, kb=kb):
                    exp_pv(psS[s_][:, 0:128], B_psS[s_], f_[:, kb:kb + 1], ai, vaug[:, kb, h, :], kb == 0, kb == qb, rb=[Bf])
                submit(hT[P, kc, ks], hT[P, qc, qs], 128, extra, ep)
            t_, Bt = new_sm()

            def norm():
                V(lambda e: e.reciprocal(out=t_[:, 0:1], in_=acc[ai][:, 64:65]), r=[B_acc[ai]], w=[Bt])
                V(lambda e: e.tensor_scalar(out=at[:, h * 64:(h + 1) * 64], in0=acc[ai][:, 0:64], scalar1=t_[:, 0:1],
                                            scalar2=None, op0=ALU.mult), r=[B_acc[ai], Bt], w=[Bat])
            Q(norm)

        def gqa_branch(qb, kvh, qc0, kc, vh, kbs, altab, extra_fn=None):
            P = slice(kvh * 64, kvh * 64 + 64)
            qs = slice(qb * 128, (qb + 1) * 128)
            Qap = hT[P, qc0:qc0 + 3, qs]
            for n_, (kb, mk) in enumerate(kbs):
                ks = slice(kb * 128, (kb + 1) * 128)
                extra = list(extra_fn(kb)) if extra_fn else []
                if mk:
                    extra.append((C["ident_bf"][:], C["mask" + mk][:]))

                def ep(s_, kb=kb, n_=n_):
                    for g in range(3):
                        hh = kvh * 3 + g
                        exp_pv(psS[s_][:, g * 128:(g + 1) * 128], B_psS[s_], altab[:, hh, qb - kb:qb - kb + 1], g,
                               vaug[:, kb, vh, :], n_ == 0, n_ == len(kbs) - 1)
                submit(hT[P, kc, ks], Qap, 384, extra, ep)

        def swa_norm(qb, kvh, at, Bat):
            for g in range(3):
                hh = kvh * 3 + g
                t_, Bt = new_sm()

                def norm(g=g, hh=hh, t_=t_, Bt=Bt):
                    V(lambda e: e.tensor_tensor(out=t_[:, 0:1], in0=acc[g][:, 64:65], in1=sinkf[:, hh:hh + 1], op=ALU.add),
                      r=[B_acc[g], B0], w=[Bt])
                    V(lambda e: e.reciprocal(out=t_[:, 1:2], in_=t_[:, 0:1]), r=[Bt], w=[Bt])
                    V(lambda e: e.tensor_scalar(out=at[:, 256 + hh * 64:256 + (hh + 1) * 64], in0=acc[g][:, 0:64],
                                                scalar1=t_[:, 1:2], scalar2=None, op0=ALU.mult), r=[B_acc[g], Bt], w=[Bat])
                Q(norm)

        def nsa_cmp(qb, kvh):
            P = slice(kvh * 64, kvh * 64 + 64)
            qs = slice(qb * 128, (qb + 1) * 128)
            Qap = hT[P, 8:11, qs]
            ncols = min(127, 8 * qb + 7)
            off = 121 - 8 * qb

            def ep(s_):
                for g in range(3):
                    hh = kvh * 3 + g
                    exp_pv(psS[s_][0:ncols, g * 128:(g + 1) * 128], B_psS[s_], C["cbias"][0:ncols, hh, qb:qb + 1], None,
                           VcA[0:ncols, kvh, :], True, True, kparts=ncols, acc_ap=psO[:, g, 0:97])
            submit(KcT[P, 0:ncols], Qap, 384, [(C["Zc"][0:8, off:off + ncols], C["Rc"][0:8, :])], ep, mparts=ncols)

            def selmask():
                V(lambda e: e.tensor_scalar(out=rdc[:, 0:3], in0=psO[:, :, 64], scalar1=1e-30, scalar2=None, op0=ALU.max),
                  r=[B_psO], w=[B_rdc])
                V(lambda e: e.reciprocal(out=rdc[:, 0:3], in_=rdc[:, 0:3]), r=[B_rdc], w=[B_rdc])
                V(lambda e: e.tensor_scalar(out=pslc[:], in0=psO[:, 0, 65:97], scalar1=rdc[:, 0:1], scalar2=None, op0=ALU.mult),
                  r=[B_psO, B_rdc], w=[B_sel])
                for g in (1, 2):
                    V(lambda e: e.scalar_tensor_tensor(out=pslc[:], in0=psO[:, g, 65:97], scalar=rdc[:, g:g + 1], in1=pslc[:],
                                                       op0=ALU.mult, op1=ALU.add), r=[B_psO, B_rdc, B_sel], w=[B_sel])
                V(lambda e: e.tensor_tensor(out=score[:], in0=pslc[:], in1=C["sel_nf"][:, qb, :], op=ALU.mult), r=[B_sel], w=[B_sel])
                V(lambda e: e.tensor_tensor(out=score[:], in0=score[:], in1=C["sel_ad"][:, qb, :], op=ALU.add), r=[B_sel], w=[B_sel])
                V(lambda e: e.max(out=m8[:], in_=score[:]), r=[B_sel], w=[B_sel])
                V(lambda e: e.tensor_scalar(out=nsel[:], in0=score[:], scalar1=m8[:, 7:8], scalar2=1.0, op0=ALU.is_ge, op1=ALU.subtract),
                  r=[B_sel], w=[B_sel])
                M(lambda e: e.transpose(out=psX[0:32, 0:128], in_=nsel[:], identity=C["ident_f"][:]), r=[B_sel], w=[B_psX])
                for g in range(3):
                    A(lambda e: e.activation(out=nselT[:, g, :], in_=psX[0:32, 0:128], func=AF.Copy, scale=-NEG),
                      r=[B_psX], w=[B_nselT])
            Q(selmask)
            flush()

        def nsa_sel_norm(qb, kvh):
            for g in range(3):
                hh = kvh * 3 + g
                t_, Bt = new_sm()

                def norm(g=g, hh=hh, t_=t_, Bt=Bt):
                    V(lambda e: e.reciprocal(out=t_[:, 0:1], in_=acc[g][:, 64:65]), r=[B_acc[g]], w=[Bt])
                    V(lambda e: e.tensor_tensor(out=t_[:, 1:2], in0=t_[:, 0:1], in1=sgate[:, qb, hh * 3 + 1:hh * 3 + 2], op=ALU.mult),
                      r=[Bt, B0], w=[Bt])
                    V(lambda e: e.tensor_scalar(out=onsa[g][:], in0=acc[g][:, 0:64], scalar1=t_[:, 1:2], scalar2=None, op0=ALU.mult),
                      r=[B_acc[g], Bt], w=[B_on[g]])
                Q(norm)

        def nsa_win_norm(qb, kvh, at, Bat):
            for g in range(3):
                hh = kvh * 3 + g
                t_, Bt = new_sm()

                def norm(g=g, hh=hh, t_=t_, Bt=Bt):
                    V(lambda e: e.reciprocal(out=t_[:, 0:1], in_=acc[g][:, 64:65]), r=[B_acc[g]], w=[Bt])
                    V(lambda e: e.tensor_tensor(out=t_[:, 1:2], in0=t_[:, 0:1], in1=sgate[:, qb, hh * 3 + 2:hh * 3 + 3], op=ALU.mult),
                      r=[Bt, B0], w=[Bt])
                    V(lambda e: e.scalar_tensor_tensor(out=onsa[g][:], in0=acc[g][:, 0:64], scalar=t_[:, 1:2], in1=onsa[g][:],
                                                       op0=ALU.mult, op1=ALU.add), r=[B_acc[g], Bt, B_on[g]], w=[B_on[g]])
                    V(lambda e: e.tensor_tensor(out=t_[:, 2:3], in0=rdc[:, g:g + 1], in1=sgate[:, qb, hh * 3:hh * 3 + 1], op=ALU.mult),
                      r=[B_rdc, B0, Bt], w=[Bt])
                    V(lambda e: e.scalar_tensor_tensor(out=at[:, 640 + hh * 64:640 + (hh + 1) * 64], in0=psO[:, g, 0:64],
                                                       scalar=t_[:, 2:3], in1=onsa[g][:], op0=ALU.mult, op1=ALU.add),
                      r=[B_psO, Bt, B_on[g]], w=[Bat])
                Q(norm)

        for qb in range(NT):
            at = attn_t[qb % 2]
            Bat = B_at[qb % 2]
            for h in range(4):
                fox_head(qb, h, at, Bat)
            for kvh in range(2):
                kbs = ([(qb - 1, "W")] if qb >= 1 else []) + [(qb, "C")]
                gqa_branch(qb, kvh, 4, 7, 4 + kvh, kbs, C["al_swa"])
                swa_norm(qb, kvh, at, Bat)
            for kvh in range(2):
                nsa_cmp(qb, kvh)
                nT = nselT[:].rearrange("j g t -> j (g t)")
                gqa_branch(qb, kvh, 8, 13, 6 + kvh, [(kb, "C" if kb == qb else None) for kb in range(qb + 1)], C["al_sel"],
                           extra_fn=lambda kb: [(C["expand"][:, kb, :], nT)])
                nsa_sel_norm(qb, kvh)
                kbs = ([(qb - 4, "W")] if qb >= 4 else []) + [(kb, None) for kb in range(max(0, qb - 3), qb)] + [(qb, "C")]
                gqa_branch(qb, kvh, 8, 14, 8 + kvh, kbs, C["al_win"])
                nsa_win_norm(qb, kvh, at, Bat)

            def store(qb=qb, at=at, Bat=Bat):
                r0 = b * S + qb * 128
                LD(attn_d[r0:r0 + 128, :], at[:], r=[Bat])
            Q(store)
        flush()


def ln_a(V, A, G, xin, Bx, tmp, Bt, sm, Bsm):
    V(lambda e: e.tensor_tensor(out=sm[:, 2:3], in0=sm[:, 0:1], in1=sm[:, 1:2], op=ALU.add), r=[Bsm], w=[Bsm])
    V(lambda e: e.tensor_scalar(out=sm[:, 3:4], in0=sm[:, 2:3], scalar1=1.0 / D, scalar2=None, op0=ALU.mult), r=[Bsm], w=[Bsm])
    V(lambda e: e.tensor_scalar(out=xin[:], in0=xin[:], scalar1=sm[:, 3:4], scalar2=None, op0=ALU.subtract), r=[Bx, Bsm], w=[Bx])
    V(lambda e: e.memset(sm[:, 4:5], 0.0), r=[Bsm], w=[Bsm])
    A(lambda e: e.activation(out=tmp[:], in_=xin[:], func=AF.Square, accum_out=sm[:, 4:5]), r=[Bx, Bsm], w=[Bt, Bsm])
    A(lambda e: e.activation(out=sm[:, 5:6], in_=sm[:, 4:5], func=AF.Sqrt, bias=LN_EPS, scale=1.0 / D), r=[Bsm], w=[Bsm])


def ln_b(V, A, G, xin, Bx, g_bc, b_bc, xo, Bxo, tmp, Bt, sm, Bsm, Bc):
    V(lambda e: e.reciprocal(out=sm[:, 6:7], in_=sm[:, 5:6]), r=[Bsm], w=[Bsm])
    V(lambda e: e.scalar_tensor_tensor(out=tmp[:], in0=xin[:], scalar=sm[:, 6:7], in1=g_bc[:], op0=ALU.mult, op1=ALU.mult),
      r=[Bx, Bsm, Bc], w=[Bt])
    V(lambda e: e.tensor_tensor(out=xo[:], in0=tmp[:], in1=b_bc[:], op=ALU.add), r=[Bt, Bc], w=[Bxo])


def phase_C(nc, T, C, l, b, x_src, attn_d, wout_d, ln1g_d, ln1b_d, rw_d, rb_d, x1_d, xg_d, meta_dl,
            acum, B_acum, sbt, pst, V, A, G, M, LD):
    with contextlib.ExitStack() as st:
        wobf = sbt(st, "wobf", [128, 8, D], BF16)
        g_bc = sbt(st, "g_bc", [128, D]); b_bc = sbt(st, "b_bc", [128, D])
        rw = sbt(st, "rw", [128, 8, NEXP]); rb = sbt(st, "rb", [128, NEXP])
        att = [sbt(st, "att%d" % i, [128, D], BF16) for i in range(2)]
        attT = [sbt(st, "attT%d" % i, [128, 8, 128], BF16) for i in range(2)]
        xt = [sbt(st, "xt%d" % i, [128, D]) for i in range(2)]
        x1p = [sbt(st, "x1p%d" % i, [128, D]) for i in range(2)]
        x1 = [sbt(st, "x1_%d" % i, [128, D]) for i in range(2)]
        x1b = [sbt(st, "x1b%d" % i, [128, D], BF16) for i in range(2)]
        x1T = [sbt(st, "x1T%d" % i, [128, 8, 128]) for i in range(2)]
        tmp = [sbt(st, "tmpC%d" % i, [128, D]) for i in range(2)]
        sm = [sbt(st, "smC%d" % i, [128, 8]) for i in range(2)]
        rt = [sbt(st, "rt%d" % i, [128, 8, NEXP]) for i in range(2)]
        rtb = [sbt(st, "rtb%d" % i, [128, NEXP], BF16) for i in range(2)]
        m8 = [sbt(st, "m8C%d" % i, [128, 16]) for i in range(2)]
        wk = [sbt(st, "wk%d" % i, [128, 8]) for i in range(2)]
        dsti = [sbt(st, "dsti%d" % i, [128, 4], I32) for i in range(2)]
        meta = [sbt(st, "metaC%d" % i, [128, 4, 2]) for i in range(2)]
        psT = pst(st, "psTc", [128, D], BF16)
        psM = [pst(st, "psM%d" % i, [128, 512]) for i in range(2)]
        psF = [pst(st, "psF%d" % i, [128, 512]) for i in range(2)]
        psR = pst(st, "psR", [128, 512])
        Bw = Buf(); Bwst = [Buf(), Buf()]; Bc = Buf()
        Batt = [Buf(), Buf()]; BattT = [Buf(), Buf()]; Bxt = [Buf(), Buf()]; Bx1p = [Buf(), Buf()]
        Bx1 = [Buf(), Buf()]; Bx1b = [Buf(), Buf()]; Bx1T = [Buf(), Buf()]; Bt = [Buf(), Buf()]; Bsm = [Buf(), Buf()]
        Brt = [Buf(), Buf()]; BpsT = Buf(); BpsM = Buf(); BpsF = Buf(); BpsR = Buf(); Bmeta = [Buf(), Buf()]
        wv = wout_d[l].rearrange("(k p) n -> p k n", p=128)
        for k in range(8):
            LD(wobf[:, k, :], wv[:, k, :], w=[Bw], q="pool")
        LD(g_bc[:], ln1g_d[l].partition_broadcast(128), w=[Bc])
        LD(b_bc[:], ln1b_d[l].partition_broadcast(128), w=[Bc])
        LD(rw[:], rw_d[l].rearrange("(k p) n -> p k n", p=128), w=[Bc])
        LD(rb[:], rb_d[l].partition_broadcast(128), w=[Bc])
        def stage1(tt):
            s_ = tt % 2
            gt = b * NT + tt
            r0 = gt * 128
            LD(att[s_][:], attn_d[r0:r0 + 128, :], w=[Batt[s_]])
            LD(xt[s_][:], x_src[r0:r0 + 128, :], w=[Bxt[s_]])
            for k in range(8):
                M(lambda e, s_=s_, k=k: e.transpose(out=psT[:, k * 128:(k + 1) * 128], in_=att[s_][:, k * 128:(k + 1) * 128],
                                                     identity=C["ident_bf"][:]), r=[Batt[s_]], w=[BpsT])
            A(lambda e, s_=s_: e.copy(out=attT[s_][:], in_=psT[:].rearrange("p (k t) -> p k t", k=8)), r=[BpsT], w=[BattT[s_]])
            for nh in range(2):
                for k in range(8):
                    M(lambda e, s_=s_, k=k, nh=nh: e.matmul(psM[nh][:], lhsT=attT[s_][:, k, :], rhs=wobf[:, k, nh * 512:(nh + 1) * 512],
                                                           start=(k == 0), stop=(k == 7)), r=[BattT[s_], Bw], w=[BpsM])
            V(lambda e, s_=s_: e.memset(sm[s_][:, 0:2], 0.0), w=[Bsm[s_]])
            for nh in range(2):
                V(lambda e, s_=s_, nh=nh: e.scalar_tensor_tensor(out=x1p[s_][:, nh * 512:(nh + 1) * 512], in0=xt[s_][:, nh * 512:(nh + 1) * 512],
                                                                scalar=ALPHA, in1=psM[nh][:], op0=ALU.mult, op1=ALU.add,
                                                                accum_out=sm[s_][:, nh:nh + 1]),
                  r=[Bxt[s_], BpsM, Bsm[s_]], w=[Bx1p[s_], Bsm[s_]])
            ln_a(V, A, G, x1p[s_], Bx1p[s_], tmp[s_], Bt[s_], sm[s_], Bsm[s_])

        def stage1b(tt):
            s_ = tt % 2
            gt = b * NT + tt
            r0 = gt * 128
            ln_b(V, A, G, x1p[s_], Bx1p[s_], g_bc, b_bc, x1[s_], Bx1[s_], tmp[s_], Bt[s_], sm[s_], Bsm[s_], Bc)
            LD(x1_d[r0:r0 + 128, :], x1[s_][:], r=[Bx1[s_]])
            A(lambda e, s_=s_: e.copy(out=x1b[s_][:], in_=x1[s_][:]), r=[Bx1[s_]], w=[Bx1b[s_]])

        def stage2(tt):
            s_ = tt % 2
            gt = b * NT + tt
            r0 = gt * 128
            for k in range(8):
                M(lambda e, s_=s_, k=k: e.transpose(out=psF[k // 4][:, (k % 4) * 128:(k % 4 + 1) * 128], in_=x1[s_][:, k * 128:(k + 1) * 128],
                                                     identity=C["ident_f"][:]), r=[Bx1[s_]], w=[BpsF])
            V(lambda e, s_=s_: e.tensor_copy(out=x1T[s_][:, 0:4, :], in_=psF[0][:].rearrange("p (k t) -> p k t", k=4)), r=[BpsF], w=[Bx1T[s_]])
            V(lambda e, s_=s_: e.tensor_copy(out=x1T[s_][:, 4:8, :], in_=psF[1][:].rearrange("p (k t) -> p k t", k=4)), r=[BpsF], w=[Bx1T[s_]])
            for k in range(8):
                M(lambda e, s_=s_, k=k: e.matmul(psR[:, 0:32], lhsT=x1T[s_][:, k, :], rhs=rw[:, k, :], start=(k == 0), stop=(k == 7)),
                  r=[Bx1T[s_], Bc], w=[BpsR])
            R_ = rt[s_]; Br = Brt[s_]
            lg, Am, ex, gg, dv, junk = (R_[:, i, :] for i in range(6))
            m_ = m8[s_]
            V(lambda e: e.tensor_tensor(out=lg, in0=psR[:, 0:32], in1=rb[:], op=ALU.add), r=[BpsR, Bc], w=[Br])
            V(lambda e: e.max(out=m_[:, 0:8], in_=lg), r=[Br], w=[Br])
            V(lambda e: e.tensor_scalar(out=Am, in0=lg, scalar1=m_[:, 3:4], scalar2=None, op0=ALU.is_ge), r=[Br], w=[Br])
            V(lambda e: e.tensor_scalar(out=m_[:, 8:9], in0=m_[:, 0:1], scalar1=-1.0, scalar2=None, op0=ALU.mult), r=[Br], w=[Br])
            A(lambda e: e.activation(out=ex, in_=lg, func=AF.Exp, bias=m_[:, 8:9], scale=1.0), r=[Br], w=[Br])
            V(lambda e: e.tensor_tensor(out=ex, in0=ex, in1=Am, op=ALU.mult), r=[Br], w=[Br])
            V(lambda e: e.tensor_reduce(out=m_[:, 9:10], in_=ex, axis=AX.X, op=ALU.add), r=[Br], w=[Br])
            V(lambda e: e.reciprocal(out=m_[:, 10:11], in_=m_[:, 9:10]), r=[Br], w=[Br])
            V(lambda e: e.tensor_scalar(out=gg, in0=ex, scalar1=m_[:, 10:11], scalar2=None, op0=ALU.mult), r=[Br], w=[Br])
            V(lambda e, s_=s_: e.tensor_copy(out=rtb[s_][:], in_=Am), r=[Br], w=[Br])
            M(lambda e, s_=s_: e.matmul(psR[:, 64:96], lhsT=C["ltri"][:], rhs=rtb[s_][:], start=True, stop=False), r=[Br, B_acum], w=[BpsR])
            M(lambda e: e.matmul(psR[:, 64:96], lhsT=C["ones_bf"][:], rhs=acum[:], start=False, stop=True), r=[Br, B_acum], w=[BpsR])
            G(lambda e, s_=s_: e.tensor_tensor(out=acum[:], in0=acum[:], in1=rtb[s_][:], op=ALU.add), r=[Br, B_acum], w=[B_acum])
            V(lambda e: e.tensor_scalar(out=dv, in0=psR[:, 64:96], scalar1=float(CAP), scalar2=None, op0=ALU.is_lt), r=[BpsR, Br], w=[Br])
            V(lambda e: e.tensor_tensor(out=dv, in0=dv, in1=Am, op=ALU.mult), r=[Br], w=[Br])
            V(lambda e: e.tensor_tensor(out=junk, in0=psR[:, 64:96], in1=C["iota_e"][:], op=ALU.add), r=[BpsR, Br], w=[Br])
            V(lambda e: e.tensor_tensor(out=dv, in0=dv, in1=junk, op=ALU.mult), r=[Br], w=[Br])
            V(lambda e: e.max(out=m_[:, 0:8], in_=dv), r=[Br], w=[Br])
            V(lambda e, s_=s_: e.tensor_copy(out=dsti[s_][:], in_=m_[:, 0:4]), r=[Br, Bmeta[s_]], w=[Bmeta[s_]])
            V(lambda e, s_=s_: e.memset(wk[s_][:], 0.0), r=[Bmeta[s_]], w=[Bmeta[s_]])
            for k in range(4):
                V(lambda e, s_=s_, k=k: e.scalar_tensor_tensor(out=junk, in0=dv, scalar=m_[:, k:k + 1], in1=gg, op0=ALU.is_equal, op1=ALU.mult,
                                                              accum_out=wk[s_][:, k:k + 1]), r=[Br, Bmeta[s_]], w=[Br, Bmeta[s_]])
            mi = meta[s_][:].bitcast(I32)
            for k in range(4):
                V(lambda e, s_=s_, k=k, gt=gt: e.tensor_copy(out=mi[:, k, 0:1], in_=C["tokid"][:, gt:gt + 1]), r=[Bmeta[s_]], w=[Bmeta[s_]])
                V(lambda e, s_=s_, k=k: e.tensor_copy(out=meta[s_][:, k, 1:2], in_=wk[s_][:, k:k + 1]), r=[Bmeta[s_]], w=[Bmeta[s_]])
            for k in range(4):
                T.dma("pool", lambda e, s_=s_, k=k: e.indirect_dma_start(
                    out=xg_d, out_offset=bass.IndirectOffsetOnAxis(ap=dsti[s_][:, k:k + 1], axis=0), in_=x1b[s_][:], in_offset=None),
                    reads=[Bx1b[s_], Bmeta[s_]])
                T.dma("pool", lambda e, s_=s_, k=k: e.indirect_dma_start(
                    out=meta_dl, out_offset=bass.IndirectOffsetOnAxis(ap=dsti[s_][:, k:k + 1], axis=0), in_=meta[s_][:, k, :], in_offset=None),
                    reads=[Bmeta[s_]])

        for tt in range(NT + 2):
            if tt < NT:
                stage1(tt)
            if 1 <= tt <= NT:
                stage1b(tt - 1)
            if tt >= 2:
                stage2(tt - 2)


def phase_D(nc, T, C, l, wgu_d, bgu_d, wdn_d, bdn_d, xg_d, meta_dl, yacc_dl, sbt, pst, V, A, G, M, LD):
    NST = CAP // 128
    HN = CAP // 2
    with contextlib.ExitStack() as st:
        wgu = [sbt(st, "wgu%d" % i, [128, 8, 2 * D], BF16) for i in range(2)]
        wdn = [sbt(st, "wdn%d" % i, [128, 8, D], BF16) for i in range(2)]
        NSTG = 3
        stg = [sbt(st, "stg%d" % i, [128, 2 * D]) for i in range(NSTG)]
        bgu = [sbt(st, "bgu%d" % i, [128, 16]) for i in range(2)]
        bdn1 = sbt(st, "bdn1", [1, D])
        Bbdn1 = Buf()
        bdnb = [sbt(st, "bdnb%d" % i, [1, D], BF16) for i in range(2)]
        meta = [sbt(st, "metaD%d" % i, [128, NST, 2]) for i in range(2)]
        NXG = 6
        xgr = [sbt(st, "xgr%d" % i, [128, D], BF16) for i in range(NXG)]
        xgT = sbt(st, "xgT", [128, 8, CAP], BF16)
        actT = sbt(st, "actT", [128, 8, CAP], BF16)
        NW = 2
        ew = [[sbt(st, "ew%d_%d" % (i, w_), [128, HN]) for i in range(4)] for w_ in range(NW)]
        yt = [sbt(st, "yt%d" % i, [128, D]) for i in range(3)]
        psT = [pst(st, "psTd%d" % i, [128, D], BF16) for i in range(2)]
        psG = [pst(st, "psG%d" % i, [128, 512]) for i in range(2)]
        psU = [pst(st, "psU%d" % i, [128, 512]) for i in range(2)]
        psY = [pst(st, "psY%d" % i, [128, 512]) for i in range(2)]
        Bwgu = [Buf(), Buf()]; Bwdn = [Buf(), Buf()]; Bsm = [Buf(), Buf()]; Bstg = [Buf() for _ in range(NSTG)]
        Bxgr = [Buf() for _ in range(NXG)]; BxgT = Buf(); BactT = Buf(); Byt = [Buf(), Buf(), Buf()]
        Bew = [[Buf() for _ in range(4)] for _ in range(NW)]
        BpsT = [Buf(), Buf()]; BpsG = [Buf(), Buf()]; BpsU = [Buf(), Buf()]; BpsY = [Buf(), Buf()]; Byacc = Buf()
        gstep = [0]

        def weight_steps(e_):
            p = e_ % 2
            wv = wgu_d[l, e_].rearrange("(k p) n -> p k n", p=128)
            wv2 = wdn_d[l, e_].rearrange("(k p) n -> p k n", p=128)
            loads, casts = [], []
            for i in range(12):
                g_ = gstep[0]
                gstep[0] += 1
                s_ = g_ % NSTG
                if i < 8:
                    src = wv[:, i, :]
                    dst = wgu[p][:, i, :]
                    stv = stg[s_][:]
                    Bd = Bwgu[p]
                else:
                    k2 = i - 8
                    src = wv2[:, 2 * k2:2 * k2 + 2, :]
                    dst = wdn[p][:, 2 * k2:2 * k2 + 2, :]
                    stv = stg[s_][:].rearrange("p (k n) -> p k n", k=2)
                    Bd = Bwdn[p]
                loads.append(lambda src=src, stv=stv, s_=s_: LD(stv, src, w=[Bstg[s_]]))
                if g_ % 2 == 0:
                    casts.append(lambda dst=dst, stv=stv, s_=s_, Bd=Bd: V(lambda e: e.tensor_copy(out=dst, in_=stv), r=[Bstg[s_]], w=[Bd]))
                else:
                    casts.append(lambda dst=dst, stv=stv, s_=s_, Bd=Bd: A(lambda e: e.copy(out=dst, in_=stv), r=[Bstg[s_]], w=[Bd]))
            steps = []
            PF = NSTG - 1
            for i in range(12):
                def step(i=i):
                    if i == 0:
                        for j_ in range(PF):
                            loads[j_]()
                    if i + PF < 12:
                        loads[i + PF]()
                    casts[i]()
                steps.append(step)
            return steps

        def small_loads(e_):
            p = e_ % 2
            LD(bgu[p][:], bgu_d[l, e_], w=[Bsm[p]])
            LD(bdn1[:], bdn_d[l, e_:e_ + 1, :], w=[Bbdn1])
            G(lambda e: e.tensor_copy(out=bdnb[p][:], in_=bdn1[:]), r=[Bbdn1, Bsm[p]], w=[Bsm[p]])
            s0 = 1 + e_ * CAP
            LD(meta[p][:], meta_dl[s0:s0 + CAP, :].rearrange("(s p) c -> p s c", p=128), w=[Bsm[p]])
            V(lambda e: e.tensor_scalar(out=bgu[p][:, 8:16], in0=bgu[p][:, 8:16], scalar1=1.0, scalar2=None, op0=ALU.add),
              r=[Bsm[p]], w=[Bsm[p]])

        xi = [0]

        def xg_load(e_, stl):
            x_ = xi[0] % NXG
            xi[0] += 1
            s0 = 1 + e_ * CAP
            LD(xgr[x_][:], xg_d[s0 + stl * 128:s0 + (stl + 1) * 128, :], w=[Bxgr[x_]], q="pool")
            return x_

        small_loads(0)
        for stp in weight_steps(0):
            stp()
        it = 0
        prev_toks = []

        def xg_transpose(e_, stl, x_):
            t_ = stl % 2
            for k in range(8):
                M(lambda e: e.transpose(out=psT[t_][:, k * 128:(k + 1) * 128], in_=xgr[x_][:, k * 128:(k + 1) * 128],
                                        identity=C["ident_bf"][:]), r=[Bxgr[x_]], w=[BpsT[t_]])
            A(lambda e: e.copy(out=xgT[:, :, stl * 128:(stl + 1) * 128], in_=psT[t_][:].rearrange("p (k t) -> p k t", k=8)),
              r=[BpsT[t_]], w=[BxgT])

        xs0 = [xg_load(0, stl) for stl in range(NST)]
        for stl in range(NST):
            xg_transpose(0, stl, xs0[stl])
        for e_ in range(NEXP):
            p = e_ % 2
            pend = []
            if e_ + 1 < NEXP:
                small_loads(e_ + 1)
                pend = weight_steps(e_ + 1)
            deferred = None
            for hn in range(2):
                for j in range(8):
                    if pend:
                        pend.pop(0)()
                    cs = slice(hn * HN, (hn + 1) * HN)
                    b_ = it % 2
                    w_ = it % NW
                    it += 1
                    for k in range(8):
                        M(lambda e: e.matmul(psG[b_][:, 0:HN], lhsT=wgu[p][:, k, j * 128:(j + 1) * 128], rhs=xgT[:, k, cs],
                                             start=(k == 0), stop=(k == 7)), r=[Bwgu[p], BxgT], w=[BpsG[b_]])
                    for k in range(8):
                        M(lambda e: e.matmul(psU[b_][:, 0:HN], lhsT=wgu[p][:, k, D + j * 128:D + (j + 1) * 128], rhs=xgT[:, k, cs],
                                             start=(k == 0), stop=(k == 7)), r=[Bwgu[p], BxgT], w=[BpsU[b_]])
                    g1, sg, u1, glu = ew[w_]
                    Bg1, Bsg, Bu1, Bglu = Bew[w_]
                    V(lambda e: e.tensor_scalar(out=g1[:], in0=psG[b_][:, 0:HN], scalar1=bgu[p][:, j:j + 1], scalar2=7.0,
                                                op0=ALU.add, op1=ALU.min), r=[BpsG[b_], Bsm[p]], w=[Bg1])
                    A(lambda e: e.activation(out=sg[:], in_=g1[:], func=AF.Sigmoid, scale=1.702), r=[Bg1], w=[Bsg])
                    V(lambda e: e.tensor_scalar(out=u1[:], in0=psU[b_][:, 0:HN], scalar1=bgu[p][:, 8 + j:9 + j], scalar2=8.0,
                                                op0=ALU.add, op1=ALU.min), r=[BpsU[b_], Bsm[p]], w=[Bu1])
                    G(lambda e: e.tensor_tensor(out=glu[:], in0=g1[:], in1=sg[:], op=ALU.mult), r=[Bg1, Bsg], w=[Bglu])
                    if deferred is not None:
                        deferred()

                    def fin(u1=u1, glu=glu, j=j, cs=cs, Bu1=Bu1, Bglu=Bglu):
                        V(lambda e: e.scalar_tensor_tensor(out=actT[:, j, cs], in0=u1[:], scalar=-6.0, in1=glu[:], op0=ALU.max, op1=ALU.mult),
                          r=[Bu1, Bglu], w=[BactT])
                    deferred = fin
            deferred()
            while pend:
                pend.pop(0)()
            xsn = [xg_load(e_ + 1, stl) for stl in range(NST)] if e_ + 1 < NEXP else None
            mi = meta[p][:].bitcast(I32)
            cur_toks = []
            for stl in range(NST):
                s_ = (e_ * NST + stl) % 3
                for nh in range(2):
                    for k in range(8):
                        M(lambda e: e.matmul(psY[nh][:], lhsT=actT[:, k, stl * 128:(stl + 1) * 128],
                                             rhs=wdn[p][:, k, nh * 512:(nh + 1) * 512], start=(k == 0), stop=False),
                          r=[BactT, Bwdn[p]], w=[BpsY[nh]])
                    M(lambda e: e.matmul(psY[nh][:], lhsT=C["ones_bf"][0:1, :], rhs=bdnb[p][0:1, nh * 512:(nh + 1) * 512],
                                         start=False, stop=True), r=[Bsm[p]], w=[BpsY[nh]])
                A(lambda e: e.activation(out=yt[s_][:, 0:512], in_=psY[0][:], func=AF.Copy, scale=meta[p][:, stl, 1:2]),
                  r=[BpsY[0], Bsm[p]], w=[Byt[s_]])
                V(lambda e: e.tensor_scalar(out=yt[s_][:, 512:1024], in0=psY[1][:], scalar1=meta[p][:, stl, 1:2], scalar2=None, op0=ALU.mult),
                  r=[BpsY[1], Bsm[p]], w=[Byt[s_]])
                for tk in prev_toks:
                    T._wait(T.e["pool"], tk)
                prev_toks = []
                cur_toks.append(T.dma("pool", lambda e: e.indirect_dma_start(
                    out=yacc_dl, out_offset=bass.IndirectOffsetOnAxis(ap=mi[:, stl, 0:1], axis=0), in_=yt[s_][:], in_offset=None,
                    compute_op=ALU.add), reads=[Byt[s_], Bsm[p]]))
                if xsn is not None:
                    xg_transpose(e_ + 1, stl, xsn[stl])
            prev_toks = cur_toks


def phase_E(nc, T, C, l, NGT, x1_d, yacc_dl, ln2g_d, ln2b_d, x_dst, sbt, pst, V, A, G, M, LD):
    with contextlib.ExitStack() as st:
        g_bc = sbt(st, "g2_bc", [128, D]); b_bc = sbt(st, "b2_bc", [128, D])
        NB_ = 3
        xt = [sbt(st, "xe%d" % i, [128, D]) for i in range(NB_)]
        yt = [sbt(st, "ye%d" % i, [128, D]) for i in range(NB_)]
        xp = [sbt(st, "xpe%d" % i, [128, D]) for i in range(NB_)]
        xo = [sbt(st, "xoe%d" % i, [128, D]) for i in range(NB_)]
        tmp = [sbt(st, "tmpE%d" % i, [128, D]) for i in range(NB_)]
        sm = [sbt(st, "smE%d" % i, [128, 8]) for i in range(NB_)]
        Bc = Buf()
        Bxt = [Buf() for _ in range(NB_)]; Byt = [Buf() for _ in range(NB_)]; Bxp = [Buf() for _ in range(NB_)]
        Bxo = [Buf() for _ in range(NB_)]; Bt = [Buf() for _ in range(NB_)]; Bsm = [Buf() for _ in range(NB_)]
        LD(g_bc[:], ln2g_d[l].partition_broadcast(128), w=[Bc])
        LD(b_bc[:], ln2b_d[l].partition_broadcast(128), w=[Bc])

        def sa(gt):
            s_ = gt % NB_
            r0 = gt * 128
            LD(xt[s_][:], x1_d[r0:r0 + 128, :], w=[Bxt[s_]])
            LD(yt[s_][:], yacc_dl[r0:r0 + 128, :], w=[Byt[s_]], q="pool")
            V(lambda e: e.memset(sm[s_][:, 0:2], 0.0), w=[Bsm[s_]])
            V(lambda e: e.scalar_tensor_tensor(out=xp[s_][:], in0=xt[s_][:], scalar=ALPHA, in1=yt[s_][:], op0=ALU.mult, op1=ALU.add,
                                               accum_out=sm[s_][:, 0:1]), r=[Bxt[s_], Byt[s_], Bsm[s_]], w=[Bxp[s_], Bsm[s_]])
            ln_a(V, A, G, xp[s_], Bxp[s_], tmp[s_], Bt[s_], sm[s_], Bsm[s_])

        def sb_(gt):
            s_ = gt % NB_
            r0 = gt * 128
            ln_b(V, A, G, xp[s_], Bxp[s_], g_bc, b_bc, xo[s_], Bxo[s_], tmp[s_], Bt[s_], sm[s_], Bsm[s_], Bc)
            LD(x_dst[r0:r0 + 128, :], xo[s_][:], r=[Bxo[s_]])

        for gt in range(NGT + 1):
            if gt < NGT:
                sa(gt)
            if gt >= 1:
                sb_(gt - 1)


def prep_inputs(inputs, core, nseq=2):
    perm = _col_perm()
    f = lambda a: np.ascontiguousarray(np.asarray(a, dtype=np.float32))
    m = {}
    x = np.asarray(inputs["x"])
    m["x"] = f(x[core * 2:core * 2 + nseq].reshape(nseq * S, D))
    w_in = np.asarray(inputs["w_in"])[:, :, perm]
    m["w_in"] = f(w_in)
    b_in = np.asarray(inputs["b_in"])[:, perm]
    m["bT"] = f(b_in[:, :NTC * 128].reshape(2, NTC, 128).transpose(0, 2, 1))
    m["bV"] = f(b_in[:, NTC * 128:].reshape(2, 1, NTOKC))
    m["sinks"] = f(np.asarray(inputs["sinks"]).reshape(2, 1, 6))
    m["peT"] = f(np.asarray(inputs["cmp_pe"]).transpose(0, 3, 1, 2))
    m["cmp_w1"] = f(inputs["cmp_w1"])
    m["cmp_w2"] = f(inputs["cmp_w2"])
    m["w_out"] = f(inputs["w_out"])
    for k in ("ln1_g", "ln1_b", "ln2_g", "ln2_b"):
        m[k] = f(np.asarray(inputs[k]).reshape(2, 1, D))
    m["router_w"] = f(inputs["router_w"])
    m["router_b"] = f(np.asarray(inputs["router_b"]).reshape(2, 1, NEXP))
    wgu = np.asarray(inputs["w_gate_up"], dtype=np.float32).reshape(2, NEXP, D, D, 2)
    m["w_gate_up"] = np.ascontiguousarray(wgu.transpose(0, 1, 2, 4, 3)).reshape(2, NEXP, D, 2 * D)
    bgu = np.asarray(inputs["b_gate_up"]).reshape(2, NEXP, 8, 128, 2)
    m["b_gu"] = f(bgu.transpose(0, 1, 3, 4, 2).reshape(2, NEXP, 128, 16))
    m["w_down"] = f(inputs["w_down"])
    m["b_down"] = f(inputs["b_down"])
    return m


_CACHE = {}


def kernel(**inputs):
    if "nc" not in _CACHE:
        _CACHE["nc"] = build()
    nc, consts = _CACHE["nc"]
    shared = None
    in_maps = []
    for core in range(8):
        m = prep_inputs(inputs, core) if shared is None else dict(shared)
        if shared is None:
            shared = {k: v for k, v in m.items() if k != "x"}
            for k, v in consts.items():
                shared["c_" + k] = v
            m = dict(shared, x=m["x"])
        else:
            x = np.asarray(inputs["x"])
            m["x"] = np.ascontiguousarray(x[core * 2:core * 2 + 2].reshape(2 * S, D), dtype=np.float32)
        in_maps.append(m)
    res = run_bass_kernel_spmd(nc, in_maps, core_ids=list(range(8)))
    out = np.concatenate([np.asarray(r["out"]).reshape(2, S, D) for r in res.results], axis=0)
    return out.astype(np.float32)
```
